# Optimizing a Trainium2 kernel written in Bass

```python
import math
import jax, jax.numpy as jnp
from jax import lax
import numpy as np

D_MODEL = 1024
BATCH = 8
SEQ = 4096
DEPTH = 4

CHUNK = 64
N_MIXERS = 2
SSM_GROUP = 16
SSM_GROUPS = D_MODEL // SSM_GROUP
SSM_STATE = 64
DT_MIN = 1e-3
DT_MAX = 1e-1
POOL_WINDOWS = (2, 4, 8, 16)
POOL_GROUP = D_MODEL // len(POOL_WINDOWS)
D_FF = ((8 * D_MODEL // 3 + 127) // 128) * 128
N_EXPERTS = 8
TOP_K = 2
D_FF_EXPERT = 7 * D_MODEL // 2
N_EVEN = (DEPTH + 1) // 2
N_ODD = DEPTH // 2
EPS = 1e-6

kernel_name = 'hybrid_s5_pool_moe_adaln'


def _rmsnorm(x, g):
    xf = x.astype(jnp.float32)
    y = xf * lax.rsqrt(jnp.mean(xf * xf, axis=-1, keepdims=True) + EPS)
    return (y * g.astype(jnp.float32)).astype(x.dtype)


def _modulate(h, shift, scale):
    return h * (1 + scale[:, None, :]) + shift[:, None, :]


def _scan_combine(e1, e2):
    a1r, a1i, b1r, b1i = e1
    a2r, a2i, b2r, b2i = e2
    return (a2r * a1r - a2i * a1i,
            a2r * a1i + a2i * a1r,
            a2r * b1r - a2i * b1i + b2r,
            a2r * b1i + a2i * b1r + b2i)


def _s5_mixer(h, w_in, log_dt, lam_re, lam_im, b_re, b_im, c_re, c_im, d_skip, w_glu, w_out):
    f32 = jnp.float32
    bsz, seq, _ = h.shape
    n_chunks = seq // CHUNK
    u = (h @ w_in).astype(f32).reshape(bsz, n_chunks, CHUNK, SSM_GROUPS, SSM_GROUP)
    dt = jnp.exp(log_dt.astype(f32))[:, None]
    lr = lam_re.astype(f32)
    li = lam_im.astype(f32)
    mag = jnp.exp(lr * dt)
    a_re = mag * jnp.cos(li * dt)
    a_im = mag * jnp.sin(li * dt)
    den = lr * lr + li * li
    nr = a_re - 1
    coef_re = (nr * lr + a_im * li) / den
    coef_im = (a_im * lr - nr * li) / den
    br = b_re.astype(f32)
    bi = b_im.astype(f32)
    bbar_re = coef_re[..., None] * br - coef_im[..., None] * bi
    bbar_im = coef_re[..., None] * bi + coef_im[..., None] * br
    steps = jnp.arange(1, CHUNK + 1, dtype=f32)[:, None, None]
    pmag = jnp.exp(lr * dt * steps)
    pow_re = pmag * jnp.cos(li * dt * steps)
    pow_im = pmag * jnp.sin(li * dt * steps)
    cr = c_re.astype(f32)
    ci = c_im.astype(f32)
    dd = d_skip.astype(f32)

    def chunk_step(carry, u_c):
        s_re, s_im = carry
        bu_re = jnp.einsum('bkgh,gph->bkgp', u_c, bbar_re)
        bu_im = jnp.einsum('bkgh,gph->bkgp', u_c, bbar_im)
        ar = jnp.broadcast_to(a_re, bu_re.shape)
        ai = jnp.broadcast_to(a_im, bu_re.shape)
        _, _, x_re, x_im = lax.associative_scan(_scan_combine, (ar, ai, bu_re, bu_im), axis=1)
        x_re = x_re + pow_re * s_re[:, None] - pow_im * s_im[:, None]
        x_im = x_im + pow_re * s_im[:, None] + pow_im * s_re[:, None]
        y_c = (jnp.einsum('bkgp,ghp->bkgh', x_re, cr)
               - jnp.einsum('bkgp,ghp->bkgh', x_im, ci)
               + dd * u_c)
        return (x_re[:, -1], x_im[:, -1]), y_c

    init = (jnp.zeros((bsz, SSM_GROUPS, SSM_STATE), f32),
            jnp.zeros((bsz, SSM_GROUPS, SSM_STATE), f32))
    _, ys = lax.scan(chunk_step, init, jnp.moveaxis(u, 1, 0))
    y = jnp.moveaxis(ys, 0, 1).reshape(bsz, seq, D_MODEL)
    z = jax.nn.gelu(y)
    z = z * jax.nn.sigmoid(z @ w_glu.astype(f32))
    return z.astype(h.dtype) @ w_out


def _pool_mixer(h, w_in, w_mix, scale, w_out):
    f32 = jnp.float32
    bsz, seq, _ = h.shape
    u = (h @ w_in).astype(f32)
    cs = jnp.cumsum(u, axis=1)
    t = jnp.arange(1, seq + 1, dtype=f32)[:, None]
    outs = []
    for g, w in enumerate(POOL_WINDOWS):
        sl = slice(g * POOL_GROUP, (g + 1) * POOL_GROUP)
        csg = cs[..., sl]
        lagged = jnp.pad(csg, ((0, 0), (w, 0), (0, 0)))[:, :seq]
        mean = (csg - lagged) / jnp.minimum(t, w)
        outs.append((mean - u[..., sl]) @ w_mix[g].astype(f32))
    z = jnp.concatenate(outs, axis=-1) * scale.astype(f32)
    return z.astype(h.dtype) @ w_out


def _swiglu(h, w1, w3, w2):
    return (jax.nn.silu(h @ w1) * (h @ w3)) @ w2


def _moe(h, router_w, router_b, w1, w3, w2):
    f32 = jnp.float32
    bsz, seq, d = h.shape
    tok = h.reshape(bsz * seq, d)
    logits = (tok @ router_w).astype(f32) + router_b.astype(f32)
    top_v, top_i = lax.top_k(logits, TOP_K)
    gates = jax.nn.softmax(top_v, axis=-1)
    weight = jnp.sum(jax.nn.one_hot(top_i, N_EXPERTS, dtype=f32) * gates[..., None], axis=1)
    out = jnp.zeros((bsz * seq, d), f32)
    for e in range(N_EXPERTS):
        out = out + weight[:, e:e + 1] * _swiglu(tok, w1[e], w3[e], w2[e]).astype(f32)
    return out.astype(h.dtype).reshape(bsz, seq, d)


def setup_inputs(seed: int = 0) -> dict:
    key = jax.random.key(seed)
    ks = jax.random.split(key, 29)
    f32 = jnp.float32
    D = D_MODEL
    G, P, H = SSM_GROUPS, SSM_STATE, SSM_GROUP

    def nrm(k, shape, s):
        return jax.random.normal(k, shape, f32) * s

    n_idx = jnp.arange(P, dtype=f32)
    return {
        'x': nrm(ks[0], (BATCH, SEQ, D), 1.0),
        'c': nrm(ks[1], (BATCH, D), 1.0),
        'ada_w': nrm(ks[2], (DEPTH, D, 6 * D), 0.5 * D ** -0.5),
        'ada_b': nrm(ks[3], (DEPTH, 6 * D), 0.02),
        'norm_g': 1.0 + nrm(ks[4], (DEPTH, 2, D), 0.02),
        'ssm_in': nrm(ks[5], (N_EVEN, D, D), D ** -0.5),
        'ssm_log_dt': jax.random.uniform(ks[6], (N_EVEN, G), f32, math.log(DT_MIN), math.log(DT_MAX)),
        'ssm_lam_re': -0.5 + nrm(ks[7], (N_EVEN, G, P), 0.01),
        'ssm_lam_im': math.pi * n_idx + nrm(ks[8], (N_EVEN, G, P), 0.01),
        'ssm_b_re': nrm(ks[9], (N_EVEN, G, P, H), (2 * H) ** -0.5),
        'ssm_b_im': nrm(ks[10], (N_EVEN, G, P, H), (2 * H) ** -0.5),
        'ssm_c_re': nrm(ks[11], (N_EVEN, G, H, P), (2 * P) ** -0.5),
        'ssm_c_im': nrm(ks[12], (N_EVEN, G, H, P), (2 * P) ** -0.5),
        'ssm_d': nrm(ks[13], (N_EVEN, G, H), 1.0),
        'ssm_glu': nrm(ks[14], (N_EVEN, D, D), D ** -0.5),
        'ssm_out': nrm(ks[15], (N_EVEN, D, D), D ** -0.5),
        'pool_in': nrm(ks[16], (N_ODD, D, D), D ** -0.5),
        'pool_mix': nrm(ks[17], (N_ODD, len(POOL_WINDOWS), POOL_GROUP, POOL_GROUP), POOL_GROUP ** -0.5),
        'pool_scale': 1.0 + nrm(ks[18], (N_ODD, D), 0.1),
        'pool_out': nrm(ks[19], (N_ODD, D, D), D ** -0.5),
        'ffn_w1': nrm(ks[20], (N_EVEN, D, D_FF), D ** -0.5),
        'ffn_w3': nrm(ks[21], (N_EVEN, D, D_FF), D ** -0.5),
        'ffn_w2': nrm(ks[22], (N_EVEN, D_FF, D), D_FF ** -0.5),
        'router_w': nrm(ks[23], (N_ODD, D, N_EXPERTS), D ** -0.5),
        'router_b': nrm(ks[24], (N_ODD, N_EXPERTS), 0.01),
        'moe_w1': nrm(ks[25], (N_ODD, N_EXPERTS, D, D_FF_EXPERT), D ** -0.5),
        'moe_w3': nrm(ks[26], (N_ODD, N_EXPERTS, D, D_FF_EXPERT), D ** -0.5),
        'moe_w2': nrm(ks[27], (N_ODD, N_EXPERTS, D_FF_EXPERT, D), D_FF_EXPERT ** -0.5),
        'final_g': 1.0 + nrm(ks[28], (D,), 0.02),
    }


def reference(x, c, ada_w, ada_b, norm_g, ssm_in, ssm_log_dt, ssm_lam_re, ssm_lam_im,
              ssm_b_re, ssm_b_im, ssm_c_re, ssm_c_im, ssm_d, ssm_glu, ssm_out,
              pool_in, pool_mix, pool_scale, pool_out, ffn_w1, ffn_w3, ffn_w2,
              router_w, router_b, moe_w1, moe_w3, moe_w2, final_g):
    cond = jax.nn.silu(c)
    for i in range(DEPTH):
        j = i // 2
        mod = cond @ ada_w[i] + ada_b[i]
        sh1, sc1, g1, sh2, sc2, g2 = jnp.split(mod, 6, axis=-1)
        h = _modulate(_rmsnorm(x, norm_g[i, 0]), sh1, sc1)
        if i % N_MIXERS == 0:
            y = _s5_mixer(h, ssm_in[j], ssm_log_dt[j], ssm_lam_re[j], ssm_lam_im[j],
                          ssm_b_re[j], ssm_b_im[j], ssm_c_re[j], ssm_c_im[j], ssm_d[j],
                          ssm_glu[j], ssm_out[j])
        else:
            y = _pool_mixer(h, pool_in[j], pool_mix[j], pool_scale[j], pool_out[j])
        x = x + g1[:, None, :] * y
        h = _modulate(_rmsnorm(x, norm_g[i, 1]), sh2, sc2)
        if i % 2 == 0:
            y = _swiglu(h, ffn_w1[j], ffn_w3[j], ffn_w2[j])
        else:
            y = _moe(h, router_w[j], router_b[j], moe_w1[j], moe_w3[j], moe_w2[j])
        x = x + g2[:, None, :] * y
    return _rmsnorm(x, final_g)
```

```python
import math
import numpy as np
from contextlib import ExitStack
import concourse.bass as bass
import concourse.mybir as mybir
from concourse.bass_utils import run_bass_kernel_spmd

F32 = mybir.dt.float32
BF16 = mybir.dt.bfloat16
ALU = mybir.AluOpType
AF = mybir.ActivationFunctionType

D = 1024
SEQ = 4096
NB = 8
KC = 8
DEPTH = 4
DFF = 2816
NFC = DFF // 128
NE = 8
DFE = 3584
EPS = 1e-6
MAG = 12582912.0
TWO_PI = 2.0 * math.pi

ENGS = ("pe", "dve", "act", "pool", "sp")
SAME_ENGINE_SYNC = True
DMA_RING = 8
SEM_CHUNK = 24000


class _Op:
    __slots__ = ("eng", "fn", "deps", "dma", "idx", "qidx", "waits")


class Prog:
    def __init__(self, nc):
        self.nc = nc
        self.ops = []
        self.last_w = {}
        self.readers = {}
        self.eng_count = {e: 0 for e in ENGS}
        self.dma_count = {e: 0 for e in ENGS}

    def op(self, eng, fn, reads=(), writes=(), dma=False):
        o = _Op()
        o.eng = eng
        o.fn = fn
        o.dma = dma
        deps = set()
        for k in reads:
            w = self.last_w.get(k)
            if w is not None:
                deps.add(w)
        for k in writes:
            w = self.last_w.get(k)
            if w is not None:
                deps.add(w)
            for r in self.readers.get(k, ()):
                deps.add(r)
        oid = len(self.ops)
        o.deps = deps
        o.idx = self.eng_count[eng]
        self.eng_count[eng] += 1
        if dma:
            o.qidx = self.dma_count[eng]
            self.dma_count[eng] += 1
        else:
            o.qidx = -1
        self.ops.append(o)
        for k in writes:
            self.last_w[k] = oid
            self.readers[k] = []
        for k in reads:
            if k in writes:
                continue
            self.readers.setdefault(k, []).append(oid)
        return oid

    def barrier(self):
        lasts = {}
        dma_recent = {}
        for i, o in enumerate(self.ops):
            if o.dma:
                dma_recent.setdefault(o.eng, []).append(i)
            else:
                lasts[o.eng] = i
        deps = set(lasts.values())
        for e, lst in dma_recent.items():
            deps.update(lst[-DMA_RING:])
        for e in ENGS:
            o = _Op()
            o.eng = e
            o.fn = None
            o.dma = False
            o.deps = set(deps)
            o.idx = self.eng_count[e]
            self.eng_count[e] += 1
            o.qidx = -1
            self.ops.append(o)
        self.last_w = {}
        self.readers = {}

    def emit(self, stack):
        nc = self.nc
        ops = self.ops
        n = len(ops)
        known = {e: {f: -1 for f in ENGS} for e in ENGS}
        known_dma = {e: set() for e in ENGS}
        vc = [None] * n
        needed = set()
        for i, o in enumerate(ops):
            E = o.eng
            kn = known[E]
            kd = known_dma[E]
            best = {}
            final = []
            for j in sorted(o.deps):
                d = ops[j]
                if d.dma:
                    if j in kd:
                        continue
                    final.append(("dma", j))
                    kd.add(j)
                    vj = vc[j]
                    for f in ENGS:
                        if vj[f] > kn[f]:
                            kn[f] = vj[f]
                else:
                    F = d.eng
                    if F == E and (not SAME_ENGINE_SYNC or E in ("pe", "sp")):
                        continue
                    if d.idx <= kn[F]:
                        continue
                    if F not in best or d.idx > ops[best[F]].idx:
                        best[F] = j
            for F, j in best.items():
                d = ops[j]
                if d.idx <= kn[F]:
                    continue
                final.append(("eng", j))
                needed.add(j)
                vj = vc[j]
                for f in ENGS:
                    if vj[f] > kn[f]:
                        kn[f] = vj[f]
                if d.idx > kn[F]:
                    kn[F] = d.idx
            o.waits = final
            v = dict(kn)
            if not o.dma:
                if (not SAME_ENGINE_SYNC or E in ("pe", "sp")) and o.idx - 1 > v[E]:
                    v[E] = o.idx - 1
                    kn[E] = o.idx - 1
                v[E] = max(v[E], o.idx)
            vc[i] = v
        nsig = {e: 0 for e in ENGS}
        sig = {}
        for i, o in enumerate(ops):
            if i in needed:
                sig[i] = nsig[o.eng]
                nsig[o.eng] += 1
        eng_sems = {}
        for e in ENGS:
            k = (nsig[e] + SEM_CHUNK - 1) // SEM_CHUNK
            eng_sems[e] = [stack.enter_context(nc.semaphore(f"s_{e}_{c}")) for c in range(k)]
        ring = {}
        for e in ENGS:
            if self.dma_count[e] > 0:
                ring[e] = [stack.enter_context(nc.semaphore(f"r_{e}_{c}")) for c in range(DMA_RING)]
        dma_ids = {e: [] for e in ENGS}
        for i, o in enumerate(ops):
            if o.dma:
                dma_ids[o.eng].append(i)
        self.stats = {e: [0, 0] for e in ENGS}

        def sem_wait(h, kind, j):
            d = ops[j]
            if kind == "dma":
                h.wait_ge(ring[d.eng][d.qidx % DMA_RING], 16 * (d.qidx // DMA_RING + 1))
            else:
                sn = sig[j]
                h.wait_ge(eng_sems[d.eng][sn // SEM_CHUNK], (sn % SEM_CHUNK) + 1)

        def emit_engine(e, h):
            for i, o in enumerate(ops):
                if o.eng != e:
                    continue
                for kind, j in o.waits:
                    sem_wait(h, kind, j)
                    self.stats[e][1] += 1
                if o.dma and o.qidx >= DMA_RING:
                    sem_wait(h, "dma", dma_ids[e][o.qidx - DMA_RING])
                if o.fn is None:
                    if i in sig:
                        sn = sig[i]
                        h.nop().then_inc(eng_sems[e][sn // SEM_CHUNK], 1)
                    continue
                ins = o.fn(h)
                self.stats[e][0] += 1
                if o.dma:
                    ins.then_inc(ring[e][o.qidx % DMA_RING], 16)
                elif i in sig:
                    sn = sig[i]
                    ins.then_inc(eng_sems[e][sn // SEM_CHUNK], 1)
            if self.dma_count[e] > 0:
                for pj in dma_ids[e][-DMA_RING:]:
                    sem_wait(h, "dma", pj)

        return emit_engine


class Arena:
    def __init__(self, ap):
        self.ap = ap
        self.n = ap.shape[1]
        self.o = 0

    def reset(self):
        self.o = 0

    def f32(self, *shape):
        n = int(np.prod(shape))
        assert self.o + n <= self.n, ("arena overflow", self.o, n, self.n)
        v = self.ap[:, self.o:self.o + n]
        self.o += n
        if len(shape) == 2:
            v = v.rearrange("p (a b) -> p a b", a=shape[0])
        elif len(shape) == 3:
            v = v.rearrange("p (a b c) -> p a b c", a=shape[0], b=shape[1])
        return v

    def bf16(self, *shape):
        n = int(np.prod(shape))
        assert n % 2 == 0
        assert self.o + n // 2 <= self.n, ("arena overflow", self.o, n, self.n)
        v = self.ap[:, self.o:self.o + n // 2].bitcast(BF16)
        self.o += n // 2
        if len(shape) == 2:
            v = v.rearrange("p (a b) -> p a b", a=shape[0])
        elif len(shape) == 3:
            v = v.rearrange("p (a b c) -> p a b c", a=shape[0], b=shape[1])
        return v


class Ctx:
    pass


def mm_acc(e, out, pairs):
    n = len(pairs)
    ins = None
    for i, (l, r) in enumerate(pairs):
        ins = e.matmul(out, lhsT=l, rhs=r, start=(i == 0), stop=(i == n - 1))
    return ins


def norm_mod(P, C, xt, xkey, h, hkey, gs, sh, bufs, tag, ncols, ps, pskey, h32=None, tmptag=None):
    sqb, tmp, rs = bufs
    if tmptag is None:
        tmptag = tag
    P.op("act", lambda e: e.activation(out=sqb, in_=xt, func=AF.Square), reads=[xkey], writes=[(tag, "sq")])
    P.op("pe", lambda e: mm_acc(e, ps[:, 0:ncols], [(C.ones_bf, sqb[:, k, :]) for k in range(KC)]),
         reads=[(tag, "sq")], writes=[pskey])
    P.op("act", lambda e: e.activation(out=rs, in_=ps[:, 0:ncols], func=AF.Sqrt, bias=C.eps_col, scale=1.0 / D),
         reads=[pskey], writes=[(tag, "rs")])
    P.op("dve", lambda e: e.reciprocal(out=rs, in_=rs), reads=[(tag, "rs")], writes=[(tag, "rs")])
    for k in range(KC):
        P.op("dve", lambda e, k=k: e.scalar_tensor_tensor(out=tmp[:, k, :], in0=xt[:, k, :], scalar=gs[:, k:k + 1],
                                                          in1=rs, op0=ALU.mult, op1=ALU.mult),
             reads=[xkey, (tag, "rs")], writes=[(tmptag, "tmp", k)])
        if h32 is not None:
            P.op("act", lambda e, k=k: e.activation(out=h32[:, k, :], in_=tmp[:, k, :], func=AF.Identity,
                                                    bias=sh[:, k:k + 1], scale=1.0),
                 reads=[(tmptag, "tmp", k)], writes=[(tmptag, "h32", k)])
            P.op("act", lambda e, k=k: e.copy(out=h[:, k, :], in_=h32[:, k, :]),
                 reads=[(tmptag, "h32", k)], writes=[(hkey, k)])
        else:
            P.op("act", lambda e, k=k: e.activation(out=h[:, k, :], in_=tmp[:, k, :], func=AF.Identity,
                                                    bias=sh[:, k:k + 1], scale=1.0),
                 reads=[(tmptag, "tmp", k)], writes=[(hkey, k)])


def frac_round(P, eng, out, in_, tmp, rkeys, wkeys, tkey):
    P.op(eng, lambda e: e.tensor_scalar(out=tmp, in0=in_, scalar1=MAG, scalar2=None, op0=ALU.add),
         reads=rkeys, writes=[tkey])
    P.op(eng, lambda e: e.tensor_scalar(out=tmp, in0=tmp, scalar1=MAG, scalar2=None, op0=ALU.subtract),
         reads=[tkey], writes=[tkey])
    P.op(eng, lambda e: e.tensor_tensor(out=out, in0=in_, in1=tmp, op=ALU.subtract),
         reads=list(rkeys) + [tkey], writes=wkeys)


def xview(ap):
    return ap.rearrange("(kc p) t -> p kc t", p=128)


def adaln_group(P, C, layer, gq, awb, key, psm):
    src = C.d_adaw[layer].rearrange("(kc p) n -> p kc n", p=128)[:, :, gq * 512:(gq + 1) * 512]
    P.op("pool", lambda e: e.dma_start(out=awb, in_=src), writes=[key], dma=True)

    def f(e):
        ins = None
        for jj in range(4):
            col = gq * 4 + jj
            for k in range(KC):
                ins = e.matmul(psm[:, col:col + 1], lhsT=awb[:, k, jj * 128:(jj + 1) * 128],
                               rhs=C.cndb[:, k:k + 1], start=(k == 0), stop=(k == KC - 1))
        return ins
    P.op("pe", f, reads=[key, "cndb"], writes=[("psm", layer)])


def adaln_finish(P, C, layer, psm):
    i = layer
    P.op("dve", lambda e: e.tensor_tensor(out=C.mod[:, i * 48:(i + 1) * 48], in0=psm[:, 0:48], in1=C.adab[:, i * 48:(i + 1) * 48], op=ALU.add),
         reads=[("psm", layer), "adab"], writes=[("mod", i)])
    for s_ in range(2):
        o = i * 48 + 24 * s_
        P.op("dve", lambda e, s_=s_, o=o: e.scalar_tensor_tensor(
            out=C.gs[:, i, s_, :], in0=C.mod[:, o + 8:o + 16], scalar=1.0, in1=C.ng[:, i, s_, :],
            op0=ALU.add, op1=ALU.mult), reads=[("mod", i), "ng"], writes=[("gs", i, s_)])


def phase_prologue(P, C, A):
    A.reset()
    cnd = A.f32(KC)
    tmpc = A.f32(KC)
    awb = [A.bf16(KC, 512) for _ in range(3)]
    P.op("sp", lambda e: e.dma_start(out=cnd, in_=C.d_cond), writes=["cnd"], dma=True)
    P.op("sp", lambda e: e.dma_start(out=C.adab, in_=C.d_adab), writes=["adab"], dma=True)
    P.op("sp", lambda e: e.dma_start(out=C.ng, in_=C.d_ng), writes=["ng"], dma=True)
    P.op("sp", lambda e: e.dma_start(out=C.fing, in_=C.d_fing), writes=["fing"], dma=True)
    P.op("dve", lambda e: e.memset(C.ones_bf, 1.0), writes=["ones"])
    P.op("dve", lambda e: e.memset(C.eps_col, EPS), writes=["eps"])
    P.op("act", lambda e: e.activation(out=tmpc, in_=cnd, func=AF.Silu), reads=["cnd"], writes=["tmpc"])
    P.op("act", lambda e: e.copy(out=C.cndb, in_=tmpc), reads=["tmpc"], writes=["cndb"])
    for gq in range(12):
        adaln_group(P, C, 0, gq, awb[gq % 3], ("awb", gq % 3), C.PS[7])
    adaln_finish(P, C, 0, C.PS[7])
    P.barrier()


def load_w_sq(P, C, dst, src, key):
    v = src.rearrange("(kc p) n -> p kc n", p=128)
    N = v.shape[2]
    step = 512
    for c0 in range(0, N, step):
        c1 = min(N, c0 + step)
        P.op("pool", lambda e, c0=c0, c1=c1: e.dma_start(out=dst[:, :, c0:c1], in_=v[:, :, c0:c1]),
             writes=[(key, c0 // step)], dma=True)
    return [(key, c // step) for c in range(0, N, step)]


def phase_s5(P, C, A, li, src, dst, sid, did):
    nc = C.nc
    j = li // 2
    T = 512
    NT = SEQ // T
    A.reset()
    w_in = A.bf16(KC, D)
    w_glu = A.bf16(KC, D)
    w_out = A.bf16(KC, D)
    BrT = A.bf16(32, 128)
    BiT = A.bf16(32, 128)
    CrT = A.bf16(32, 128)
    nCiT = A.bf16(32, 128)
    kw_in = load_w_sq(P, C, w_in, C.d_ssm_in[j], "w_in")
    kw_glu = load_w_sq(P, C, w_glu, C.d_ssm_glu[j], "w_glu")
    kw_out = load_w_sq(P, C, w_out, C.d_ssm_out[j], "w_out")
    RHO = A.f32(32)
    THF = A.f32(32)
    BASE = A.f32(32, 8)
    ST_R = A.f32(32)
    ST_I = A.f32(32)
    Dt = A.f32(KC)
    pq = A.f32(3, 32)
    t32 = [A.f32(32) for _ in range(3)]
    P.op("sp", lambda e: e.dma_start(out=pq, in_=C.d_ssm_pq[j]), writes=["pq"], dma=True)
    P.op("sp", lambda e: e.dma_start(out=Dt, in_=C.d_ssm_d[j]), writes=["Dt"], dma=True)
    P.op("dve", lambda e: e.memset(ST_R, 0.0), writes=["ST_R"])
    P.op("dve", lambda e: e.memset(ST_I, 0.0), writes=["ST_I"])
    P.op("act", lambda e: e.activation(out=t32[0], in_=pq[:, 2, :], func=AF.Exp), reads=["pq"], writes=["t32_0"])
    P.op("dve", lambda e: e.tensor_tensor(out=t32[1], in0=pq[:, 0, :], in1=t32[0], op=ALU.mult),
         reads=["pq", "t32_0"], writes=["t32_1"])
    P.op("act", lambda e: e.activation(out=RHO, in_=t32[1], func=AF.Exp), reads=["t32_1"], writes=["RHO"])
    P.op("dve", lambda e: e.tensor_tensor(out=t32[1], in0=pq[:, 1, :], in1=t32[0], op=ALU.mult),
         reads=["pq", "t32_0", "RHO"], writes=["t32_1"])
    P.op("dve", lambda e: e.tensor_scalar(out=t32[1], in0=t32[1], scalar1=1.0 / TWO_PI, scalar2=None, op0=ALU.mult),
         reads=["t32_1"], writes=["t32_1"])
    frac_round(P, "dve", THF, t32[1], t32[2], ["t32_1"], ["THF"], "t32_2")
    P.op("dve", lambda e: e.tensor_scalar(out=t32[0], in0=THF, scalar1=512.0, scalar2=None, op0=ALU.mult),
         reads=["THF"], writes=["t32_0"])
    frac_round(P, "dve", t32[1], t32[0], t32[2], ["t32_0"], ["t32_1"], "t32_2")
    for tt in range(NT):
        P.op("dve", lambda e, tt=tt: e.tensor_scalar(out=t32[0], in0=t32[1], scalar1=float(tt), scalar2=None, op0=ALU.mult),
             reads=["t32_1"], writes=["t32_0"])
        frac_round(P, "dve", BASE[:, :, tt], t32[0], t32[2], ["t32_0"], [("BASE", tt)], "t32_2")
    CH = 512
    mark = A.o
    rowp = [A.f32(CH) for _ in range(3)]
    bt = [A.f32(CH) for _ in range(2)]
    ct = [A.f32(CH) for _ in range(2)]
    w = [A.f32(CH) for _ in range(10)]
    for cc in range(8):
        sl = slice(cc * CH, (cc + 1) * CH)
        rk = ("rowp", cc)
        P.op("sp", lambda e, sl=sl: e.dma_start(out=rowp[0], in_=C.d_ssm_row[j][0][:, sl]), writes=["rowp0"], dma=True)
        P.op("sp", lambda e, sl=sl: e.dma_start(out=rowp[1], in_=C.d_ssm_row[j][1][:, sl]), writes=["rowp1"], dma=True)
        P.op("sp", lambda e, sl=sl: e.dma_start(out=rowp[2], in_=C.d_ssm_row[j][2][:, sl]), writes=["rowp2"], dma=True)
        P.op("sp", lambda e, sl=sl: e.dma_start(out=bt[0], in_=C.d_ssm_bt[j][0][:, sl]), writes=["bt0"], dma=True)
        P.op("sp", lambda e, sl=sl: e.dma_start(out=bt[1], in_=C.d_ssm_bt[j][1][:, sl]), writes=["bt1"], dma=True)
        P.op("sp", lambda e, sl=sl: e.dma_start(out=ct[0], in_=C.d_ssm_ct[j][0][:, sl]), writes=["ct0"], dma=True)
        P.op("sp", lambda e, sl=sl: e.dma_start(out=ct[1], in_=C.d_ssm_ct[j][1][:, sl]), writes=["ct1"], dma=True)
        lr, lim, ldt = rowp
        P.op("act", lambda e: e.activation(out=w[0], in_=ldt, func=AF.Exp), reads=["rowp2"], writes=["w0"])
        P.op("dve", lambda e: e.tensor_tensor(out=w[1], in0=lr, in1=w[0], op=ALU.mult), reads=["rowp0", "w0"], writes=["w1"])
        P.op("act", lambda e: e.activation(out=w[2], in_=w[1], func=AF.Exp), reads=["w1"], writes=["w2"])
        P.op("dve", lambda e: e.tensor_tensor(out=w[3], in0=lim, in1=w[0], op=ALU.mult), reads=["rowp1", "w0"], writes=["w3"])
        P.op("dve", lambda e: e.tensor_scalar(out=w[3], in0=w[3], scalar1=1.0 / TWO_PI, scalar2=None, op0=ALU.mult),
             reads=["w3"], writes=["w3"])
        frac_round(P, "dve", w[4], w[3], w[5], ["w3"], ["w4"], "w5")
        P.op("act", lambda e: e.activation(out=w[5], in_=w[4], func=AF.Sin, scale=TWO_PI), reads=["w4"], writes=["w5"])
        P.op("act", lambda e: e.activation(out=w[6], in_=w[4], func=AF.Abs), reads=["w4"], writes=["w6"])
        P.op("act", lambda e: e.activation(out=w[6], in_=w[6], func=AF.Sin, scale=-TWO_PI, bias=C.halfpi_col),
             reads=["w6"], writes=["w6"])
        P.op("dve", lambda e: e.tensor_tensor(out=w[6], in0=w[6], in1=w[2], op=ALU.mult), reads=["w6", "w2"], writes=["w6"])
        P.op("dve", lambda e: e.tensor_tensor(out=w[5], in0=w[5], in1=w[2], op=ALU.mult), reads=["w5", "w2"], writes=["w5"])
        P.op("dve", lambda e: e.tensor_scalar(out=w[6], in0=w[6], scalar1=-1.0, scalar2=None, op0=ALU.add), reads=["w6"], writes=["w6"])
        P.op("dve", lambda e: e.tensor_tensor(out=w[7], in0=lr, in1=lr, op=ALU.mult), reads=["rowp0"], writes=["w7"])
        P.op("dve", lambda e: e.tensor_tensor(out=w[8], in0=lim, in1=lim, op=ALU.mult), reads=["rowp1"], writes=["w8"])
        P.op("dve", lambda e: e.tensor_tensor(out=w[7], in0=w[7], in1=w[8], op=ALU.add), reads=["w7", "w8"], writes=["w7"])
        P.op("dve", lambda e: e.reciprocal(out=w[7], in_=w[7]), reads=["w7"], writes=["w7"])
        P.op("dve", lambda e: e.tensor_tensor(out=w[8], in0=w[6], in1=lr, op=ALU.mult), reads=["w6", "rowp0"], writes=["w8"])
        P.op("dve", lambda e: e.tensor_tensor(out=w[9], in0=w[5], in1=lim, op=ALU.mult), reads=["w5", "rowp1"], writes=["w9"])
        P.op("dve", lambda e: e.tensor_tensor(out=w[8], in0=w[8], in1=w[9], op=ALU.add), reads=["w8", "w9"], writes=["w8"])
        P.op("dve", lambda e: e.tensor_tensor(out=w[8], in0=w[8], in1=w[7], op=ALU.mult), reads=["w8", "w7"], writes=["w8"])
        P.op("dve", lambda e: e.tensor_tensor(out=w[9], in0=w[5], in1=lr, op=ALU.mult), reads=["w5", "rowp0"], writes=["w9"])
        P.op("dve", lambda e: e.tensor_tensor(out=w[0], in0=w[6], in1=lim, op=ALU.mult), reads=["w6", "rowp1"], writes=["w0"])
        P.op("dve", lambda e: e.tensor_tensor(out=w[9], in0=w[9], in1=w[0], op=ALU.subtract), reads=["w9", "w0"], writes=["w9"])
        P.op("dve", lambda e: e.tensor_tensor(out=w[9], in0=w[9], in1=w[7], op=ALU.mult), reads=["w9", "w7"], writes=["w9"])
        q0 = cc * 4
        brv = BrT[:, q0:q0 + 4, :].rearrange("p a b -> p (a b)")
        biv = BiT[:, q0:q0 + 4, :].rearrange("p a b -> p (a b)")
        crv = CrT[:, q0:q0 + 4, :].rearrange("p a b -> p (a b)")
        civ = nCiT[:, q0:q0 + 4, :].rearrange("p a b -> p (a b)")
        P.op("dve", lambda e: e.tensor_tensor(out=w[1], in0=w[8], in1=bt[0], op=ALU.mult), reads=["w8", "bt0"], writes=["w1"])
        P.op("dve", lambda e: e.tensor_tensor(out=w[2], in0=w[9], in1=bt[1], op=ALU.mult), reads=["w9", "bt1"], writes=["w2"])
        P.op("dve", lambda e, brv=brv: e.tensor_tensor(out=brv, in0=w[1], in1=w[2], op=ALU.subtract),
             reads=["w1", "w2"], writes=[("BrT", cc)])
        P.op("dve", lambda e: e.tensor_tensor(out=w[1], in0=w[8], in1=bt[1], op=ALU.mult), reads=["w8", "bt1", ("BrT", cc)], writes=["w1"])
        P.op("dve", lambda e: e.tensor_tensor(out=w[2], in0=w[9], in1=bt[0], op=ALU.mult), reads=["w9", "bt0", ("BrT", cc)], writes=["w2"])
        P.op("dve", lambda e, biv=biv: e.tensor_tensor(out=biv, in0=w[1], in1=w[2], op=ALU.add),
             reads=["w1", "w2"], writes=[("BiT", cc)])
        P.op("act", lambda e, crv=crv: e.copy(out=crv, in_=ct[0]), reads=["ct0"], writes=[("CrT", cc)])
        P.op("act", lambda e, civ=civ: e.mul(out=civ, in_=ct[1], mul=-1.0), reads=["ct1"], writes=[("nCiT", cc)])
    P.barrier()
    A.o = mark
    xt = A.f32(KC, T)
    tmp = A.f32(KC, T)
    sqb = A.bf16(KC, T)
    rs = A.f32(T)
    h = A.bf16(KC, T)
    u = A.bf16(KC, T)
    zb = A.bf16(KC, T)
    z2b = A.bf16(KC, T)
    y32 = tmp
    NPB = 2
    G = [A.f32(T) for _ in range(NPB)]
    FS = [A.f32(T) for _ in range(NPB)]
    COS = [A.f32(T) for _ in range(NPB)]
    SIN = [A.f32(T) for _ in range(NPB)]
    wr = [A.f32(T) for _ in range(NPB)]
    wi = [A.f32(T) for _ in range(NPB)]
    xr = [A.bf16(T) for _ in range(NPB)]
    xi = [A.bf16(T) for _ in range(NPB)]
    RAB = A.f32(T)
    t1 = A.f32(T)
    t2 = A.f32(T)
    cr = A.f32(T)
    ci = A.f32(T)
    p1 = A.f32(T)
    p2 = A.f32(T)
    sg = A.f32(T)
    PS = C.PS
    gs = C.gs[:, li, 0, :]
    sh = C.mod[:, li * 48 + 0: li * 48 + 8]
    g1 = C.mod[:, li * 48 + 16: li * 48 + 24]
    sv = xview(src)
    dv = xview(dst)
    NQ = 32

    def stage_a(t, q):
        b = q % NPB
        P.op("act", lambda e: e.activation(out=G[b], in_=C.tloc, func=AF.Identity, scale=THF[:, q:q + 1], bias=BASE[:, q, t:t + 1]),
             reads=[], writes=[("G", b)])
        P.op("act", lambda e: e.activation(out=RAB, in_=G[b], func=AF.Identity, bias=C.magp_col, scale=1.0),
             reads=[("G", b)], writes=[("RAB",)])
        P.op("act", lambda e: e.activation(out=RAB, in_=RAB, func=AF.Identity, bias=C.magn_col, scale=1.0),
             reads=[("RAB",)], writes=[("RAB",)])
        P.op("dve", lambda e: e.tensor_tensor(out=FS[b], in0=G[b], in1=RAB, op=ALU.subtract),
             reads=[("G", b), ("RAB",)], writes=[("FS", b)])
        P.op("act", lambda e: e.activation(out=SIN[b], in_=FS[b], func=AF.Sin, scale=TWO_PI), reads=[("FS", b)], writes=[("SIN", b)])
        P.op("act", lambda e: e.activation(out=COS[b], in_=FS[b], func=AF.Abs), reads=[("FS", b)], writes=[("COS", b)])
        P.op("act", lambda e: e.activation(out=COS[b], in_=COS[b], func=AF.Sin, scale=-TWO_PI, bias=C.halfpi_col),
             reads=[("COS", b)], writes=[("COS", b)])

    def stage_bproj(q):
        b = q % NPB
        dc = q // 4
        pr = 4 + 2 * b
        pi_ = pr + 1
        P.op("pe", lambda e: e.matmul(PS[pr], lhsT=BrT[:, q, :], rhs=u[:, dc, :], start=True, stop=True),
             reads=[("u", dc)], writes=[("ps", pr)])
        P.op("pe", lambda e: e.matmul(PS[pi_], lhsT=BiT[:, q, :], rhs=u[:, dc, :], start=True, stop=True),
             reads=[("u", dc)], writes=[("ps", pi_)])

    def stage_scan(q):
        b = q % NPB
        pr = 4 + 2 * b
        pi_ = pr + 1
        P.op("dve", lambda e: e.tensor_tensor(out=t1, in0=PS[pr], in1=COS[b], op=ALU.mult), reads=[("ps", pr), ("COS", b)], writes=[("t1",)])
        P.op("dve", lambda e: e.tensor_tensor(out=t2, in0=PS[pi_], in1=SIN[b], op=ALU.mult), reads=[("ps", pi_), ("SIN", b)], writes=[("t2",)])
        P.op("dve", lambda e: e.tensor_tensor(out=cr, in0=t1, in1=t2, op=ALU.add), reads=[("t1",), ("t2",)], writes=[("cr",)])
        P.op("dve", lambda e: e.tensor_tensor(out=t1, in0=PS[pi_], in1=COS[b], op=ALU.mult), reads=[("ps", pi_), ("COS", b), ("cr",)], writes=[("t1",)])
        P.op("dve", lambda e: e.tensor_tensor(out=t2, in0=PS[pr], in1=SIN[b], op=ALU.mult), reads=[("ps", pr), ("SIN", b), ("cr",)], writes=[("t2",)])
        P.op("dve", lambda e: e.tensor_tensor(out=ci, in0=t1, in1=t2, op=ALU.subtract), reads=[("t1",), ("t2",)], writes=[("ci",)])
        rho_b = RHO[:, q:q + 1].to_broadcast([128, T])
        P.op("dve", lambda e: e.tensor_tensor_scan(out=wr[b], data0=rho_b, data1=cr, initial=ST_R[:, q:q + 1], op0=ALU.mult, op1=ALU.add),
             reads=[("cr",), ("ST_R", q)], writes=[("wr", b)])
        P.op("dve", lambda e: e.tensor_tensor_scan(out=wi[b], data0=rho_b, data1=ci, initial=ST_I[:, q:q + 1], op0=ALU.mult, op1=ALU.add),
             reads=[("ci",), ("ST_I", q)], writes=[("wi", b)])

    def stage_rot(q):
        b = q % NPB
        P.op("pool", lambda e: e.tensor_copy(out=ST_R[:, q:q + 1], in_=wr[b][:, T - 1:T]), reads=[("wr", b)], writes=[("ST_R", q)])
        P.op("pool", lambda e: e.tensor_copy(out=ST_I[:, q:q + 1], in_=wi[b][:, T - 1:T]), reads=[("wi", b)], writes=[("ST_I", q)])
        P.op("pool", lambda e: e.tensor_tensor(out=p1, in0=wr[b], in1=COS[b], op=ALU.mult), reads=[("wr", b), ("COS", b)], writes=[("p1",)])
        P.op("pool", lambda e: e.tensor_tensor(out=p2, in0=wi[b], in1=SIN[b], op=ALU.mult), reads=[("wi", b), ("SIN", b)], writes=[("p2",)])
        P.op("pool", lambda e: e.tensor_tensor(out=xr[b], in0=p1, in1=p2, op=ALU.subtract), reads=[("p1",), ("p2",)], writes=[("xr", b)])
        P.op("pool", lambda e: e.tensor_tensor(out=p1, in0=wr[b], in1=SIN[b], op=ALU.mult), reads=[("wr", b), ("SIN", b), ("xr", b)], writes=[("p1",)])
        P.op("pool", lambda e: e.tensor_tensor(out=p2, in0=wi[b], in1=COS[b], op=ALU.mult), reads=[("wi", b), ("COS", b), ("xr", b)], writes=[("p2",)])
        P.op("pool", lambda e: e.tensor_tensor(out=xi[b], in0=p1, in1=p2, op=ALU.add), reads=[("p1",), ("p2",)], writes=[("xi", b)])

    def stage_cproj(q):
        b = q % NPB
        dc = q // 4
        qq = q % 4
        P.op("pe", lambda e: e.matmul(PS[3], lhsT=CrT[:, q, :], rhs=xr[b], start=(qq == 0), stop=False),
             reads=[("xr", b)], writes=[("ps", 3)])
        P.op("pe", lambda e: e.matmul(PS[3], lhsT=nCiT[:, q, :], rhs=xi[b], start=False, stop=(qq == 3)),
             reads=[("xi", b)], writes=[("ps", 3)])
        if qq == 3:
            P.op("dve", lambda e: e.scalar_tensor_tensor(out=y32[:, dc, :], in0=u[:, dc, :], scalar=Dt[:, dc:dc + 1], in1=PS[3],
                                                         op0=ALU.mult, op1=ALU.add),
                 reads=[("ps", 3), ("u", dc)], writes=[("y32", dc)])
            P.op("act", lambda e: e.activation(out=zb[:, dc, :], in_=y32[:, dc, :], func=AF.Gelu_apprx_tanh),
                 reads=[("y32", dc)], writes=[("zb", dc)])

    def head(t):
        P.op("sp", lambda e: e.dma_start(out=xt, in_=sv[:, :, t * T:(t + 1) * T]), reads=[(sid, t)], writes=["xt"], dma=True)
        stage_a(t, 0)
        norm_mod(P, C, xt, "xt", h, "h", gs, sh, (sqb, tmp, rs), "nm", T, PS[0], "ps0")
        for m in range(KC):
            pb = 1 + (m % 2)
            P.op("pe", lambda e, m=m, pb=pb: mm_acc(e, PS[pb], [(w_in[:, k, m * 128:(m + 1) * 128], h[:, k, :]) for k in range(KC)]),
                 reads=[("h", k) for k in range(KC)], writes=[("ps", pb)])
            P.op("act", lambda e, m=m, pb=pb: e.copy(out=u[:, m, :], in_=PS[pb]), reads=[("ps", pb)], writes=[("u", m)])

    def glu_piece(m):
        pb = 1 + (m % 2)
        P.op("pe", lambda e: mm_acc(e, PS[pb], [(w_glu[:, k, m * 128:(m + 1) * 128], zb[:, k, :]) for k in range(KC)]),
             reads=[("zb", k) for k in range(KC)], writes=[("ps", pb)])
        P.op("act", lambda e: e.activation(out=sg, in_=PS[pb], func=AF.Sigmoid), reads=[("ps", pb)], writes=["sg"])
        P.op("dve", lambda e: e.tensor_tensor(out=z2b[:, m, :], in0=zb[:, m, :], in1=sg, op=ALU.mult),
             reads=["sg", ("zb", m)], writes=[("z2b", m)])

    def out_piece(m):
        pb = 1 + (m % 2)
        P.op("pe", lambda e: mm_acc(e, PS[pb], [(w_out[:, k, m * 128:(m + 1) * 128], z2b[:, k, :]) for k in range(KC)]),
             reads=[("z2b", k) for k in range(KC)], writes=[("ps", pb)])
        P.op("dve", lambda e: e.scalar_tensor_tensor(out=xt[:, m, :], in0=PS[pb], scalar=g1[:, m:m + 1], in1=xt[:, m, :],
                                                     op0=ALU.mult, op1=ALU.add),
             reads=[("ps", pb), "xt"], writes=["xt"])

    def tail_piece(t, i, reload):
        if i == 0 and reload:
            P.op("sp", lambda e: e.dma_start(out=xt, in_=sv[:, :, t * T:(t + 1) * T]), reads=[(sid, t)], writes=["xt"], dma=True)
        if i < 3:
            for m in range(3 * i, min(KC, 3 * i + 3)):
                glu_piece(m)
        elif i < 11:
            out_piece(i - 3)
            if i == 10:
                P.op("sp", lambda e: e.dma_start(out=dv[:, :, t * T:(t + 1) * T], in_=xt), reads=["xt"], writes=[(did, t)], dma=True)

    def core(t, extra=None):
        stage_bproj(0)
        for q in range(NQ):
            if q + 1 < NQ:
                stage_a(t, q + 1)
                stage_bproj(q + 1)
            stage_scan(q)
            stage_rot(q)
            if q >= 1:
                stage_cproj(q - 1)
            if extra is not None and q < 11:
                extra(q)
        stage_cproj(NQ - 1)

    head(0)
    core(0)
    for t in range(1, NT):
        head(t)
        core(t, extra=lambda i, tt=t - 1: tail_piece(tt, i, True))
    for i in range(11):
        tail_piece(NT - 1, i, True)
    P.barrier()


def phase_ffn(P, C, A, li, src, dst, sid, did):
    j = li // 2
    T = 256
    NT = SEQ // T
    A.reset()
    w1 = A.bf16(KC, DFF)
    w3 = A.bf16(KC, DFF)
    w2 = A.bf16(NFC, D)
    load_w_sq(P, C, w1, C.d_ffn_w1[j], "w1")
    load_w_sq(P, C, w3, C.d_ffn_w3[j], "w3")
    v2 = C.d_ffn_w2[j].rearrange("(fc p) n -> p fc n", p=128)
    for f0 in range(0, NFC, 4):
        f1 = min(NFC, f0 + 4)
        P.op("pool", lambda e, f0=f0, f1=f1: e.dma_start(out=w2[:, f0:f1, :], in_=v2[:, f0:f1, :]), writes=[("w2", f0)], dma=True)
    P.barrier()
    xt = [A.f32(KC, T) for _ in range(2)]
    sqb = [A.bf16(KC, T) for _ in range(2)]
    rs = [A.f32(T) for _ in range(2)]
    h = [A.bf16(KC, T) for _ in range(2)]
    tmp = A.f32(KC, T)
    a = A.bf16(NFC, T)
    sg = [A.f32(T) for _ in range(2)]
    awb = A.bf16(KC, 512)
    PS = C.PS
    gs = C.gs[:, li, 1, :]
    sh = C.mod[:, li * 48 + 24: li * 48 + 32]
    g2 = C.mod[:, li * 48 + 40: li * 48 + 48]
    sv = xview(src)
    dv = xview(dst)
    nxt = li + 1
    for t in range(NT):
        bb = t % 2
        xk = ("xt", bb)
        hk = ("h", bb)
        P.op("sp", lambda e, t=t, bb=bb: e.dma_start(out=xt[bb], in_=sv[:, :, t * T:(t + 1) * T]), reads=[(sid, t)], writes=[xk], dma=True)
        norm_mod(P, C, xt[bb], xk, h[bb], hk, gs, sh, (sqb[bb], tmp, rs[bb]), ("nm", bb), T, PS[0], "ps0", tmptag="nmT")
        if nxt < DEPTH and t < 12:
            adaln_group(P, C, nxt, t, awb, "awb", PS[7])
            if t == 11:
                adaln_finish(P, C, nxt, PS[7])
        for fc in range(NFC):
            pg = 1 + 2 * (fc % 2)
            pu = pg + 1
            b = fc % 2
            P.op("pe", lambda e, fc=fc, pg=pg, bb=bb: mm_acc(e, PS[pg][:, 0:T], [(w1[:, k, fc * 128:(fc + 1) * 128], h[bb][:, k, :]) for k in range(KC)]),
                 reads=[(hk, k) for k in range(KC)], writes=[("ps", pg)])
            P.op("pe", lambda e, fc=fc, pu=pu, bb=bb: mm_acc(e, PS[pu][:, 0:T], [(w3[:, k, fc * 128:(fc + 1) * 128], h[bb][:, k, :]) for k in range(KC)]),
                 reads=[(hk, k) for k in range(KC)], writes=[("ps", pu)])
            P.op("act", lambda e, pg=pg, b=b: e.activation(out=sg[b], in_=PS[pg][:, 0:T], func=AF.Silu), reads=[("ps", pg)], writes=[("sg", b)])
            P.op("dve", lambda e, fc=fc, pu=pu, b=b: e.tensor_tensor(out=a[:, fc, :], in0=PS[pu][:, 0:T], in1=sg[b], op=ALU.mult),
                 reads=[("ps", pu), ("sg", b)], writes=[("a", fc)])
        for m in range(KC):
            pb = 5 + (m % 2)
            P.op("pe", lambda e, m=m, pb=pb: mm_acc(e, PS[pb][:, 0:T], [(w2[:, fc, m * 128:(m + 1) * 128], a[:, fc, :]) for fc in range(NFC)]),
                 reads=[("a", fc) for fc in range(NFC)], writes=[("ps", pb)])
            P.op("dve", lambda e, m=m, pb=pb, bb=bb: e.scalar_tensor_tensor(out=xt[bb][:, m, :], in0=PS[pb][:, 0:T], scalar=g2[:, m:m + 1], in1=xt[bb][:, m, :],
                                                                          op0=ALU.mult, op1=ALU.add),
                 reads=[("ps", pb), xk], writes=[xk])
        P.op("sp", lambda e, t=t, bb=bb: e.dma_start(out=dv[:, :, t * T:(t + 1) * T], in_=xt[bb]), reads=[xk], writes=[(did, t)], dma=True)
    P.barrier()


def phase_pool(P, C, A, li, src, dst, sid, did):
    j = li // 2
    T = 512
    NT = SEQ // T
    HALO = 16
    A.reset()
    w_in = A.bf16(KC, D)
    w_out = A.bf16(KC, D)
    wmix = A.bf16(4, 2, 256)
    psc = A.f32(KC)
    load_w_sq(P, C, w_in, C.d_pool_in[j], "w_in")
    load_w_sq(P, C, w_out, C.d_pool_out[j], "w_out")
    for wg in range(4):
        P.op("pool", lambda e, wg=wg: e.dma_start(out=wmix[:, wg, :, :], in_=C.d_pool_mix[j][wg].rearrange("(kc p) n -> p kc n", p=128)),
             writes=[("wmix", wg)], dma=True)
    P.op("sp", lambda e: e.dma_start(out=psc, in_=C.d_pool_scale[j]), writes=["psc"], dma=True)
    P.barrier()
    xt = [A.f32(KC, T) for _ in range(2)]
    sqb = [A.bf16(KC, T) for _ in range(2)]
    rs = [A.f32(T) for _ in range(2)]
    h = [A.bf16(KC, T) for _ in range(2)]
    dm = [A.bf16(KC, T) for _ in range(2)]
    tmp = A.f32(KC, T)
    U = A.f32(KC, HALO + T)
    SA = A.f32(HALO + T)
    SB = A.f32(HALO + T)
    zb = A.bf16(KC, T)
    inv0 = A.f32(4, T)
    awb = A.bf16(KC, 512)
    P.op("sp", lambda e: e.dma_start(out=inv0, in_=C.d_inv0), writes=["inv0"], dma=True)
    P.op("dve", lambda e: e.memset(U, 0.0), writes=[("U", k) for k in range(KC)])
    PS = C.PS
    gs = C.gs[:, li, 0, :]
    sh = C.mod[:, li * 48 + 0: li * 48 + 8]
    g1 = C.mod[:, li * 48 + 16: li * 48 + 24]
    sv = xview(src)
    dv = xview(dst)
    W = (2, 4, 8, 16)
    nxt = li + 1
    for t in range(NT):
        bb = t % 2
        xk = ("xt", bb)
        hk = ("h", bb)
        P.op("sp", lambda e, t=t, bb=bb: e.dma_start(out=xt[bb], in_=sv[:, :, t * T:(t + 1) * T]), reads=[(sid, t)], writes=[xk], dma=True)
        norm_mod(P, C, xt[bb], xk, h[bb], hk, gs, sh, (sqb[bb], tmp, rs[bb]), ("nm", bb), T, PS[0], "ps0", tmptag="nmT")
        if nxt < DEPTH and t < 6:
            for gq in (2 * t, 2 * t + 1):
                adaln_group(P, C, nxt, gq, awb, "awb", PS[7])
            if t == 5:
                adaln_finish(P, C, nxt, PS[7])
        for m in range(KC):
            pb = 1 + (m % 2)
            wgi = m // 2
            wsz = W[wgi]
            P.op("pe", lambda e, m=m, pb=pb, bb=bb: mm_acc(e, PS[pb], [(w_in[:, k, m * 128:(m + 1) * 128], h[bb][:, k, :]) for k in range(KC)]),
                 reads=[(hk, k) for k in range(KC)], writes=[("ps", pb)])
            if t > 0:
                P.op("act", lambda e, m=m: e.copy(out=U[:, m, 0:HALO], in_=U[:, m, T:T + HALO]), reads=[("U", m)], writes=[("U", m)])
            P.op("act", lambda e, m=m, pb=pb: e.copy(out=U[:, m, HALO:HALO + T], in_=PS[pb]), reads=[("ps", pb)], writes=[("U", m)])
            cur = U[:, m, :]
            curk = ("U", m)
            sh_ = 1
            bufs = [SA, SB]
            bi = 0
            while sh_ < wsz:
                nb = bufs[bi]
                nk = ("S", bi)
                P.op("dve", lambda e, cur=cur, nb=nb, sh_=sh_: e.tensor_tensor(out=nb[:, sh_:HALO + T], in0=cur[:, sh_:HALO + T],
                                                                               in1=cur[:, 0:HALO + T - sh_], op=ALU.add),
                     reads=[curk], writes=[nk])
                cur = nb
                curk = nk
                bi ^= 1
                sh_ *= 2
            dmk = ("dm", bb, m)
            if t == 0:
                P.op("dve", lambda e, cur=cur, wgi=wgi: e.tensor_tensor(out=cur[:, HALO:HALO + T], in0=cur[:, HALO:HALO + T],
                                                                          in1=inv0[:, wgi, :], op=ALU.mult),
                     reads=[curk, "inv0"], writes=[curk])
                P.op("dve", lambda e, cur=cur, m=m, bb=bb: e.tensor_tensor(out=dm[bb][:, m, :], in0=cur[:, HALO:HALO + T],
                                                                             in1=U[:, m, HALO:HALO + T], op=ALU.subtract),
                     reads=[curk, ("U", m)], writes=[dmk])
            else:
                P.op("dve", lambda e, cur=cur, m=m, wsz=wsz, bb=bb: e.scalar_tensor_tensor(out=dm[bb][:, m, :], in0=cur[:, HALO:HALO + T],
                                                                                             scalar=1.0 / wsz, in1=U[:, m, HALO:HALO + T],
                                                                                             op0=ALU.mult, op1=ALU.subtract),
                     reads=[curk, ("U", m)], writes=[dmk])
        for wgi in range(4):
            for mo in range(2):
                m = 2 * wgi + mo
                pb = 3 + (m % 2)
                P.op("pe", lambda e, wgi=wgi, mo=mo, pb=pb, bb=bb: mm_acc(e, PS[pb], [(wmix[:, wgi, ki, mo * 128:(mo + 1) * 128], dm[bb][:, 2 * wgi + ki, :])
                                                                                    for ki in range(2)]),
                     reads=[("dm", bb, 2 * wgi), ("dm", bb, 2 * wgi + 1)], writes=[("ps", pb)])
                P.op("act", lambda e, m=m, pb=pb: e.activation(out=zb[:, m, :], in_=PS[pb], func=AF.Identity, scale=psc[:, m:m + 1]),
                     reads=[("ps", pb)], writes=[("zb", m)])
        for m in range(KC):
            pb = 5 + (m % 2)
            P.op("pe", lambda e, m=m, pb=pb: mm_acc(e, PS[pb], [(w_out[:, k, m * 128:(m + 1) * 128], zb[:, k, :]) for k in range(KC)]),
                 reads=[("zb", k) for k in range(KC)], writes=[("ps", pb)])
            P.op("dve", lambda e, m=m, pb=pb, bb=bb: e.scalar_tensor_tensor(out=xt[bb][:, m, :], in0=PS[pb], scalar=g1[:, m:m + 1], in1=xt[bb][:, m, :],
                                                                          op0=ALU.mult, op1=ALU.add),
                 reads=[("ps", pb), xk], writes=[xk])
        P.op("sp", lambda e, t=t, bb=bb: e.dma_start(out=dv[:, :, t * T:(t + 1) * T], in_=xt[bb]), reads=[xk], writes=[(did, t)], dma=True)
    P.barrier()


def phase_moe(P, C, A, li, src, dst, sid, did):
    j = li // 2
    T = 512
    HT = 4
    HN = HT * T
    PS = C.PS
    gs = C.gs[:, li, 1, :]
    sh = C.mod[:, li * 48 + 24: li * 48 + 32]
    g2 = C.mod[:, li * 48 + 40: li * 48 + 48]
    sv = xview(src)
    dv = xview(dst)
    FG = 512
    NFG = DFE // FG
    for half in range(2):
        A.reset()
        hH = A.bf16(KC, HN)
        GW = A.f32(16, NE)
        rw = A.f32(KC, NE)
        rbb = A.f32(NE)
        yacc = A.f32(KC, HN)
        mark = A.o
        xt = yacc[:, 0:2, :].rearrange("p a (b c) -> p (a b) c", b=4)
        tmp = yacc[:, 2:4, :].rearrange("p a (b c) -> p (a b) c", b=4)
        h32 = yacc[:, 4:6, :].rearrange("p a (b c) -> p (a b) c", b=4)
        sqb = A.bf16(KC, T)
        rs = A.f32(T)
        L = A.f32(NE)
        L2 = A.f32(NE)
        eq1 = A.f32(NE)
        eq2 = A.f32(NE)
        sm = A.f32(8)
        P.op("sp", lambda e: e.dma_start(out=rw, in_=C.d_router_w[j]), writes=["rw"], dma=True)
        P.op("sp", lambda e: e.dma_start(out=rbb, in_=C.d_router_b[j]), writes=["rbb"], dma=True)
        for tt in range(HT):
            t = half * HT + tt
            hv = hH[:, :, tt * T:(tt + 1) * T]
            P.op("sp", lambda e, t=t: e.dma_start(out=xt, in_=sv[:, :, t * T:(t + 1) * T]), reads=[(sid, t)], writes=["xt"], dma=True)
            norm_mod(P, C, xt, "xt", hv, ("hH", tt), gs, sh, (sqb, tmp, rs), "nm", T, PS[0], "ps0", h32=h32)
            for sub in range(4):
                si = tt * 4 + sub
                P.op("pe", lambda e, sub=sub: mm_acc(e, PS[1][:, 0:NE], [(h32[:, k, sub * 128:(sub + 1) * 128], rw[:, k, :]) for k in range(KC)]),
                     reads=[("nm", "h32", k) for k in range(KC)] + ["rw"], writes=[("ps", 1)])
                P.op("dve", lambda e: e.tensor_tensor(out=L, in0=PS[1][:, 0:NE], in1=rbb, op=ALU.add), reads=[("ps", 1), "rbb"], writes=["L"])
                P.op("dve", lambda e: e.reduce_max(out=sm[:, 0:1], in_=L, axis=mybir.AxisListType.X), reads=["L"], writes=["m1"])
                P.op("dve", lambda e: e.tensor_scalar(out=eq1, in0=L, scalar1=sm[:, 0:1], scalar2=None, op0=ALU.is_equal),
                     reads=["L", "m1"], writes=["eq1"])
                P.op("dve", lambda e: e.scalar_tensor_tensor(out=L2, in0=eq1, scalar=-1e30, in1=L, op0=ALU.mult, op1=ALU.add),
                     reads=["eq1", "L"], writes=["L2"])
                P.op("dve", lambda e: e.reduce_max(out=sm[:, 1:2], in_=L2, axis=mybir.AxisListType.X), reads=["L2"], writes=["m2"])
                P.op("dve", lambda e: e.tensor_scalar(out=eq2, in0=L2, scalar1=sm[:, 1:2], scalar2=None, op0=ALU.is_equal),
                     reads=["L2", "m2"], writes=["eq2"])
                P.op("dve", lambda e: e.tensor_tensor(out=sm[:, 2:3], in0=sm[:, 0:1], in1=sm[:, 1:2], op=ALU.subtract),
                     reads=["m1", "m2"], writes=["dd"])
                P.op("act", lambda e: e.activation(out=sm[:, 3:4], in_=sm[:, 2:3], func=AF.Sigmoid), reads=["dd"], writes=["p1"])
                P.op("act", lambda e: e.activation(out=sm[:, 4:5], in_=sm[:, 2:3], func=AF.Sigmoid, scale=-1.0), reads=["dd"], writes=["p2"])
                P.op("dve", lambda e: e.tensor_scalar(out=eq1, in0=eq1, scalar1=sm[:, 3:4], scalar2=None, op0=ALU.mult),
                     reads=["eq1", "p1", "L2"], writes=["eq1"])
                P.op("dve", lambda e, si=si: e.scalar_tensor_tensor(out=GW[:, si, :], in0=eq2, scalar=sm[:, 4:5], in1=eq1, op0=ALU.mult, op1=ALU.add),
                     reads=["eq2", "p2", "eq1"], writes=[("GW", si)])
        P.barrier()
        A.o = mark
        NWB = 2
        w1g = [A.bf16(KC, FG) for _ in range(NWB)]
        w3g = [A.bf16(KC, FG) for _ in range(NWB)]
        w2g = [A.bf16(4, D) for _ in range(NWB)]
        WB = [A.bf16(HN) for _ in range(2)]
        a = [A.bf16(4, T) for _ in range(2)]
        sg = [A.f32(T) for _ in range(2)]
        sgw = [A.f32(T) for _ in range(2)]
        wcount = 0
        acount = 0
        for ex in range(NE):
            wb = WB[ex % 2]
            wbk = ("WB", ex % 2)
            for c4 in range(4):
                def f(e, c4=c4, ex=ex):
                    ins = None
                    for s4 in range(4):
                        si = c4 * 4 + s4
                        ins = e.matmul(PS[0][:, s4 * 128:(s4 + 1) * 128], lhsT=GW[:, si, ex:ex + 1].to_broadcast([128, 128]),
                                       rhs=C.ident32, start=True, stop=True)
                    return ins
                P.op("pe", f, reads=[("GW", c4 * 4 + s4) for s4 in range(4)], writes=[("ps", 0)])
                P.op("act", lambda e, c4=c4, wb=wb: e.copy(out=wb[:, c4 * T:(c4 + 1) * T], in_=PS[0]), reads=[("ps", 0)], writes=[wbk])
            for fg in range(NFG):
                b = wcount % NWB
                wcount += 1
                f0 = fg * FG
                v1 = C.d_moe_w1[j][ex].rearrange("(kc p) n -> p kc n", p=128)[:, :, f0:f0 + FG]
                v3 = C.d_moe_w3[j][ex].rearrange("(kc p) n -> p kc n", p=128)[:, :, f0:f0 + FG]
                v2 = C.d_moe_w2[j][ex][f0:f0 + FG, :].rearrange("(fc p) n -> p fc n", p=128)
                P.op("pool", lambda e, b=b, v1=v1: e.dma_start(out=w1g[b], in_=v1), writes=[("w1g", b)], dma=True)
                P.op("pool", lambda e, b=b, v3=v3: e.dma_start(out=w3g[b], in_=v3), writes=[("w3g", b)], dma=True)
                P.op("pool", lambda e, b=b, v2=v2: e.dma_start(out=w2g[b], in_=v2), writes=[("w2g", b)], dma=True)
                first = (ex == 0 and fg == 0)
                for tt in range(HT):
                    ab = acount % 2
                    acount += 1
                    for fc in range(4):
                        pg = 2 * (fc % 2)
                        pu = pg + 1
                        sb = fc % 2
                        P.op("pe", lambda e, fc=fc, pg=pg, b=b, tt=tt: mm_acc(e, PS[pg], [(w1g[b][:, k, fc * 128:(fc + 1) * 128],
                                                                                         hH[:, k, tt * T:(tt + 1) * T]) for k in range(KC)]),
                             reads=[("w1g", b)], writes=[("ps", pg)])
                        P.op("pe", lambda e, fc=fc, pu=pu, b=b, tt=tt: mm_acc(e, PS[pu], [(w3g[b][:, k, fc * 128:(fc + 1) * 128],
                                                                                         hH[:, k, tt * T:(tt + 1) * T]) for k in range(KC)]),
                             reads=[("w3g", b)], writes=[("ps", pu)])
                        P.op("act", lambda e, pg=pg, sb=sb: e.activation(out=sg[sb], in_=PS[pg], func=AF.Silu),
                             reads=[("ps", pg)], writes=[("sg", sb)])
                        P.op("dve", lambda e, sb=sb, wb=wb, tt=tt: e.tensor_tensor(out=sgw[sb], in0=sg[sb], in1=wb[:, tt * T:(tt + 1) * T], op=ALU.mult),
                             reads=[("sg", sb), wbk], writes=[("sgw", sb)])
                        P.op("dve", lambda e, fc=fc, pu=pu, sb=sb, ab=ab: e.tensor_tensor(out=a[ab][:, fc, :], in0=PS[pu], in1=sgw[sb], op=ALU.mult),
                             reads=[("ps", pu), ("sgw", sb)], writes=[("a", ab, fc)])
                    for m in range(KC):
                        pb = 4 + (m % 4)
                        P.op("pe", lambda e, m=m, pb=pb, b=b, ab=ab: mm_acc(e, PS[pb], [(w2g[b][:, fc, m * 128:(m + 1) * 128], a[ab][:, fc, :])
                                                                                     for fc in range(4)]),
                             reads=[("a", ab, fc) for fc in range(4)] + [("w2g", b)], writes=[("ps", pb)])
                        yv = yacc[:, m, tt * T:(tt + 1) * T]
                        if first:
                            P.op("dve", lambda e, yv=yv, pb=pb: e.tensor_copy(out=yv, in_=PS[pb]), reads=[("ps", pb)], writes=[("yacc", m, tt)])
                        else:
                            P.op("dve", lambda e, yv=yv, pb=pb: e.tensor_tensor(out=yv, in0=PS[pb], in1=yv, op=ALU.add),
                                 reads=[("ps", pb), ("yacc", m, tt)], writes=[("yacc", m, tt)])
        P.barrier()
        A.o = mark
        xt3 = [A.f32(KC, T) for _ in range(2)]
        for tt in range(HT):
            t = half * HT + tt
            xb = xt3[tt % 2]
            xk = ("xt3", tt % 2)
            P.op("sp", lambda e, t=t, xb=xb: e.dma_start(out=xb, in_=sv[:, :, t * T:(t + 1) * T]), reads=[(sid, t)], writes=[xk], dma=True)
            for m in range(KC):
                P.op("dve", lambda e, m=m, xb=xb, tt=tt: e.scalar_tensor_tensor(out=xb[:, m, :], in0=yacc[:, m, tt * T:(tt + 1) * T],
                                                                              scalar=g2[:, m:m + 1], in1=xb[:, m, :], op0=ALU.mult, op1=ALU.add),
                     reads=[xk, ("yacc", m, tt)], writes=[xk])
            P.op("sp", lambda e, t=t, xb=xb: e.dma_start(out=dv[:, :, t * T:(t + 1) * T], in_=xb), reads=[xk], writes=[(did, t)], dma=True)
        P.barrier()


def phase_final(P, C, A, src, dst, sid, did, do_norm=True):
    T = 512
    NT = SEQ // T
    A.reset()
    xt = [A.f32(KC, T) for _ in range(2)]
    sqb = A.bf16(KC, T)
    rs = A.f32(T)
    PS = C.PS
    sv = xview(src)
    dv = xview(dst)
    for t in range(NT):
        xb = xt[t % 2]
        xk = ("xt", t % 2)
        P.op("sp", lambda e, t=t, xb=xb: e.dma_start(out=xb, in_=sv[:, :, t * T:(t + 1) * T]), reads=[(sid, t)], writes=[xk], dma=True)
        if do_norm:
            P.op("act", lambda e, xb=xb: e.activation(out=sqb, in_=xb, func=AF.Square), reads=[xk], writes=["sq"])
            P.op("pe", lambda e: mm_acc(e, PS[0], [(C.ones_bf, sqb[:, k, :]) for k in range(KC)]), reads=["sq"], writes=["ps0"])
            P.op("act", lambda e: e.activation(out=rs, in_=PS[0], func=AF.Sqrt, bias=C.eps_col, scale=1.0 / D), reads=["ps0"], writes=["rs"])
            P.op("dve", lambda e: e.reciprocal(out=rs, in_=rs), reads=["rs"], writes=["rs"])
            for k in range(KC):
                P.op("dve", lambda e, k=k, xb=xb: e.scalar_tensor_tensor(out=xb[:, k, :], in0=xb[:, k, :], scalar=C.fing[:, k:k + 1], in1=rs,
                                                                       op0=ALU.mult, op1=ALU.mult),
                     reads=[xk, "rs"], writes=[xk])
        P.op("sp", lambda e, t=t, xb=xb: e.dma_start(out=dv[:, :, t * T:(t + 1) * T], in_=xb), reads=[xk], writes=[(did, t)], dma=True)
    P.barrier()


def build_program(n_phases=8):
    nc = bass.Bass("TRN2", target_bir_lowering=False)
    C = Ctx()
    C.nc = nc

    def din(name, shape):
        return nc.dram_tensor(name, list(shape), F32, kind="ExternalInput").ap()

    d_x = din("xT", (D, SEQ))
    C.d_cond = din("condT", (128, KC))
    C.d_adaw = din("ada_w", (DEPTH, D, 6 * D))
    C.d_adab = din("adab", (128, DEPTH * 48))
    C.d_ng = din("ng", (128, DEPTH, 2, KC))
    C.d_fing = din("fing", (128, KC))
    C.d_ssm_in = din("ssm_in", (2, D, D))
    C.d_ssm_glu = din("ssm_glu", (2, D, D))
    C.d_ssm_out = din("ssm_out", (2, D, D))
    C.d_ssm_pq = din("ssm_pq", (2, 128, 3, 32))
    C.d_ssm_d = din("ssm_dT", (2, 128, KC))
    C.d_ssm_row = din("ssm_row", (2, 3, 128, 4096))
    C.d_ssm_bt = din("ssm_bt", (2, 2, 128, 4096))
    C.d_ssm_ct = din("ssm_ct", (2, 2, 128, 4096))
    C.d_pool_in = din("pool_in", (2, D, D))
    C.d_pool_mix = din("pool_mix", (2, 4, 256, 256))
    C.d_pool_scale = din("pool_scaleT", (2, 128, KC))
    C.d_pool_out = din("pool_out", (2, D, D))
    C.d_ffn_w1 = din("ffn_w1", (2, D, DFF))
    C.d_ffn_w3 = din("ffn_w3", (2, D, DFF))
    C.d_ffn_w2 = din("ffn_w2", (2, DFF, D))
    C.d_router_w = din("router_wT", (2, 128, KC, NE))
    C.d_router_b = din("router_bb", (2, 128, NE))
    C.d_moe_w1 = din("moe_w1", (2, NE, D, DFE))
    C.d_moe_w3 = din("moe_w3", (2, NE, D, DFE))
    C.d_moe_w2 = din("moe_w2", (2, NE, DFE, D))
    C.d_inv0 = din("inv0", (128, 4, 512))
    d_tloc = din("tloc", (128, 512))
    d_ident = din("ident", (128, 128))
    d_out = nc.dram_tensor("outT", [D, SEQ], F32, kind="ExternalOutput").ap()
    xa = nc.dram_tensor("xa", [D, SEQ], F32, kind="Internal").ap()
    xb = nc.dram_tensor("xb", [D, SEQ], F32, kind="Internal").ap()

    with ExitStack() as st:
        P = Prog(nc)
        sb = lambda name, shape, dt=F32: st.enter_context(nc.sbuf_tensor(name, list(shape), dt))
        C.ones_bf = sb("ones_bf", (128, 128), BF16)[:]
        C.eps_col = sb("eps_col", (128, 1))[:]
        C.halfpi_col = sb("halfpi", (128, 1))[:]
        C.magp_col = sb("magp", (128, 1))[:]
        C.magn_col = sb("magn", (128, 1))[:]
        C.ident32 = sb("ident32", (128, 128))[:]
        C.tloc = sb("tloc_sb", (128, 512))[:]
        C.mod = sb("mod", (128, DEPTH * 48))[:]
        C.adab = sb("adab_sb", (128, DEPTH * 48))[:]
        C.ng = sb("ng_sb", (128, DEPTH, 2, KC))[:]
        C.gs = sb("gs_sb", (128, DEPTH, 2, KC))[:]
        C.fing = sb("fing_sb", (128, KC))[:]
        C.cndb = sb("cndb", (128, KC), BF16)[:]
        arena = sb("arena", (128, 50 * 1024 + 512))[:]
        A = Arena(arena)
        C.PS = [st.enter_context(nc.psum_tensor(f"ps{i}", [128, 512], F32))[:] for i in range(8)]

        P.op("sp", lambda e: e.dma_start(out=C.tloc, in_=d_tloc), writes=["tloc"], dma=True)
        P.op("sp", lambda e: e.dma_start(out=C.ident32, in_=d_ident), writes=["ident"], dma=True)
        P.op("dve", lambda e: e.memset(C.halfpi_col, math.pi / 2.0), writes=["halfpi"])
        P.op("dve", lambda e: e.memset(C.magp_col, MAG), writes=["magp"])
        P.op("dve", lambda e: e.memset(C.magn_col, -MAG), writes=["magn"])
        phase_prologue(P, C, A)

        seqs = []
        for li in range(DEPTH):
            seqs.append(("mix", li))
            seqs.append(("ffn", li))
        seqs = seqs[:n_phases]
        cur, cur_id = d_x, "xin"
        nxt = [(xa, "xa"), (xb, "xb")]
        for pi, (kind, li) in enumerate(seqs):
            dstap, dst_id = nxt[pi % 2]
            if kind == "mix":
                if li % 2 == 0:
                    phase_s5(P, C, A, li, cur, dstap, cur_id, dst_id)
                else:
                    phase_pool(P, C, A, li, cur, dstap, cur_id, dst_id)
            else:
                if li % 2 == 0:
                    phase_ffn(P, C, A, li, cur, dstap, cur_id, dst_id)
                else:
                    phase_moe(P, C, A, li, cur, dstap, cur_id, dst_id)
            cur, cur_id = dstap, dst_id
        phase_final(P, C, A, cur, d_out, cur_id, "out", do_norm=(n_phases == 8))

        emit = P.emit(st)
        with nc.Block() as block:
            @block.tensor
            def _(e):
                emit("pe", e)

            @block.vector
            def _(e):
                emit("dve", e)

            @block.scalar
            def _(e):
                emit("act", e)

            @block.gpsimd
            def _(e):
                emit("pool", e)

            @block.sync
            def _(e):
                emit("sp", e)
        C.stats = P.stats
    return nc, C


def _colT(v):
    return np.ascontiguousarray(np.asarray(v, np.float32).reshape(KC, 128).T)


def prepare_inputs(inp, cores):
    f = lambda a: np.ascontiguousarray(np.asarray(a, np.float32))
    shared = {}
    shared["ada_w"] = f(inp["ada_w"])
    shared["adab"] = np.ascontiguousarray(
        np.stack([f(inp["ada_b"][i]).reshape(48, 128).T for i in range(DEPTH)], axis=1).reshape(128, DEPTH * 48))
    ng = np.zeros((128, DEPTH, 2, KC), np.float32)
    for i in range(DEPTH):
        for s in range(2):
            ng[:, i, s, :] = _colT(inp["norm_g"][i, s])
    shared["ng"] = ng
    shared["fing"] = _colT(inp["final_g"])
    for k in ("ssm_in", "ssm_glu", "ssm_out", "pool_in", "pool_mix", "pool_out", "ffn_w1", "ffn_w3", "ffn_w2",
              "moe_w1", "moe_w3", "moe_w2"):
        shared[k] = f(inp[k])
    pq = np.zeros((2, 128, 3, 32), np.float32)
    row = np.zeros((2, 3, 128, 4096), np.float32)
    bt = np.zeros((2, 2, 128, 4096), np.float32)
    ct = np.zeros((2, 2, 128, 4096), np.float32)
    dT = np.zeros((2, 128, KC), np.float32)
    for j in range(2):
        lr = f(inp["ssm_lam_re"][j])
        lim = f(inp["ssm_lam_im"][j])
        ldt = f(inp["ssm_log_dt"][j])
        ldt_gp = np.repeat(ldt[:, None], 64, axis=1)
        for a_i, arr in enumerate((lr, lim, ldt_gp)):
            pq[j, :, a_i, :] = arr.reshape(32, 2, 64).transpose(1, 2, 0).reshape(128, 32)
            row[j, a_i] = np.broadcast_to(arr.reshape(1, 4096), (128, 4096))
        br = f(inp["ssm_b_re"][j])
        bi = f(inp["ssm_b_im"][j])
        cr = f(inp["ssm_c_re"][j])
        ci = f(inp["ssm_c_im"][j])
        btr = bt[j].reshape(2, 128, 32, 2, 64)
        ctr = ct[j].reshape(2, 128, 32, 128)
        for q in range(32):
            for g2 in range(2):
                g = 2 * q + g2
                gl = g % 8
                btr[0, gl * 16:(gl + 1) * 16, q, g2, :] = br[g].T
                btr[1, gl * 16:(gl + 1) * 16, q, g2, :] = bi[g].T
                ctr[0, g2 * 64:(g2 + 1) * 64, q, gl * 16:(gl + 1) * 16] = cr[g].T
                ctr[1, g2 * 64:(g2 + 1) * 64, q, gl * 16:(gl + 1) * 16] = ci[g].T
        dT[j] = _colT(f(inp["ssm_d"][j]).reshape(-1))
    shared["ssm_pq"] = pq
    shared["ssm_row"] = row
    shared["ssm_bt"] = bt
    shared["ssm_ct"] = ct
    shared["ssm_dT"] = dT
    shared["pool_scaleT"] = np.stack([_colT(inp["pool_scale"][j]) for j in range(2)])
    shared["router_wT"] = np.ascontiguousarray(
        np.stack([f(inp["router_w"][j]).reshape(KC, 128, NE).transpose(1, 0, 2) for j in range(2)]))
    shared["router_bb"] = np.ascontiguousarray(
        np.stack([np.broadcast_to(f(inp["router_b"][j])[None, :], (128, NE)) for j in range(2)]))
    inv0 = np.zeros((128, 4, 512), np.float32)
    tpos = np.arange(1, 513, dtype=np.float32)
    for wg, w in enumerate((2, 4, 8, 16)):
        inv0[:, wg, :] = (1.0 / np.minimum(tpos, float(w)))[None, :]
    shared["inv0"] = inv0
    shared["tloc"] = np.ascontiguousarray(np.broadcast_to(np.arange(512, dtype=np.float32)[None, :], (128, 512)))
    shared["ident"] = np.eye(128, dtype=np.float32)
    maps = []
    x = np.asarray(inp["x"], np.float32)
    c = np.asarray(inp["c"], np.float32)
    for b in cores:
        m = dict(shared)
        m["xT"] = np.ascontiguousarray(x[b].T)
        m["condT"] = _colT(c[b])
        maps.append(m)
    return maps


_CACHE = {}


def kernel(**inputs):
    if "nc" not in _CACHE:
        _CACHE["nc"] = build_program(8)[0]
    nc = _CACHE["nc"]
    maps = prepare_inputs(inputs, list(range(NB)))
    res = run_bass_kernel_spmd(nc, maps, core_ids=list(range(NB)))
    out = np.stack([np.ascontiguousarray(r["outT"].T) for r in res.results], axis=0)
    return out.astype(np.float32)
```

```python
import math
import numpy as np
from contextlib import ExitStack
import concourse.bass as bass
import concourse.mybir as mybir
from concourse.bass_utils import run_bass_kernel_spmd

F32 = mybir.dt.float32
BF16 = mybir.dt.bfloat16
ALU = mybir.AluOpType
AF = mybir.ActivationFunctionType

D = 1024
SEQ = 4096
NB = 8
KC = 8
DEPTH = 4
DFF = 2816
NFC = DFF // 128
NE = 8
DFE = 3584
EPS = 1e-6
MAG = 12582912.0
TWO_PI = 2.0 * math.pi

ENGS = ("pe", "dve", "act", "pool", "sp")
SAME_ENGINE_SYNC = True
DMA_RING = 8
SEM_CHUNK = 24000


class _Op:
    __slots__ = ("eng", "fn", "deps", "dma", "idx", "qidx", "waits")


class Prog:
    def __init__(self, nc):
        self.nc = nc
        self.ops = []
        self.last_w = {}
        self.readers = {}
        self.eng_count = {e: 0 for e in ENGS}
        self.dma_count = {e: 0 for e in ENGS}

    def op(self, eng, fn, reads=(), writes=(), dma=False):
        o = _Op()
        o.eng = eng
        o.fn = fn
        o.dma = dma
        deps = set()
        for k in reads:
            w = self.last_w.get(k)
            if w is not None:
                deps.add(w)
        for k in writes:
            w = self.last_w.get(k)
            if w is not None:
                deps.add(w)
            for r in self.readers.get(k, ()):
                deps.add(r)
        oid = len(self.ops)
        o.deps = deps
        o.idx = self.eng_count[eng]
        self.eng_count[eng] += 1
        if dma:
            o.qidx = self.dma_count[eng]
            self.dma_count[eng] += 1
        else:
            o.qidx = -1
        self.ops.append(o)
        for k in writes:
            self.last_w[k] = oid
            self.readers[k] = []
        for k in reads:
            if k in writes:
                continue
            self.readers.setdefault(k, []).append(oid)
        return oid

    def barrier(self):
        lasts = {}
        dma_recent = {}
        for i, o in enumerate(self.ops):
            if o.dma:
                dma_recent.setdefault(o.eng, []).append(i)
            else:
                lasts[o.eng] = i
        deps = set(lasts.values())
        for e, lst in dma_recent.items():
            deps.update(lst[-DMA_RING:])
        for e in ENGS:
            o = _Op()
            o.eng = e
            o.fn = None
            o.dma = False
            o.deps = set(deps)
            o.idx = self.eng_count[e]
            self.eng_count[e] += 1
            o.qidx = -1
            self.ops.append(o)
        self.last_w = {}
        self.readers = {}

    def emit(self, stack):
        nc = self.nc
        ops = self.ops
        n = len(ops)
        known = {e: {f: -1 for f in ENGS} for e in ENGS}
        known_dma = {e: set() for e in ENGS}
        vc = [None] * n
        needed = set()
        for i, o in enumerate(ops):
            E = o.eng
            kn = known[E]
            kd = known_dma[E]
            best = {}
            final = []
            for j in sorted(o.deps):
                d = ops[j]
                if d.dma:
                    if j in kd:
                        continue
                    final.append(("dma", j))
                    kd.add(j)
                    vj = vc[j]
                    for f in ENGS:
                        if vj[f] > kn[f]:
                            kn[f] = vj[f]
                else:
                    F = d.eng
                    if F == E and (not SAME_ENGINE_SYNC or E in ("pe", "sp")):
                        continue
                    if d.idx <= kn[F]:
                        continue
                    if F not in best or d.idx > ops[best[F]].idx:
                        best[F] = j
            for F, j in best.items():
                d = ops[j]
                if d.idx <= kn[F]:
                    continue
                final.append(("eng", j))
                needed.add(j)
                vj = vc[j]
                for f in ENGS:
                    if vj[f] > kn[f]:
                        kn[f] = vj[f]
                if d.idx > kn[F]:
                    kn[F] = d.idx
            o.waits = final
            v = dict(kn)
            if not o.dma:
                if (not SAME_ENGINE_SYNC or E in ("pe", "sp")) and o.idx - 1 > v[E]:
                    v[E] = o.idx - 1
                    kn[E] = o.idx - 1
                v[E] = max(v[E], o.idx)
            vc[i] = v
        nsig = {e: 0 for e in ENGS}
        sig = {}
        for i, o in enumerate(ops):
            if i in needed:
                sig[i] = nsig[o.eng]
                nsig[o.eng] += 1
        eng_sems = {}
        for e in ENGS:
            k = (nsig[e] + SEM_CHUNK - 1) // SEM_CHUNK
            eng_sems[e] = [stack.enter_context(nc.semaphore(f"s_{e}_{c}")) for c in range(k)]
        ring = {}
        for e in ENGS:
            if self.dma_count[e] > 0:
                ring[e] = [stack.enter_context(nc.semaphore(f"r_{e}_{c}")) for c in range(DMA_RING)]
        dma_ids = {e: [] for e in ENGS}
        for i, o in enumerate(ops):
            if o.dma:
                dma_ids[o.eng].append(i)
        self.stats = {e: [0, 0] for e in ENGS}

        def sem_wait(h, kind, j):
            d = ops[j]
            if kind == "dma":
                h.wait_ge(ring[d.eng][d.qidx % DMA_RING], 16 * (d.qidx // DMA_RING + 1))
            else:
                sn = sig[j]
                h.wait_ge(eng_sems[d.eng][sn // SEM_CHUNK], (sn % SEM_CHUNK) + 1)

        def emit_engine(e, h):
            for i, o in enumerate(ops):
                if o.eng != e:
                    continue
                for kind, j in o.waits:
                    sem_wait(h, kind, j)
                    self.stats[e][1] += 1
                if o.dma and o.qidx >= DMA_RING:
                    sem_wait(h, "dma", dma_ids[e][o.qidx - DMA_RING])
                if o.fn is None:
                    if i in sig:
                        sn = sig[i]
                        h.nop().then_inc(eng_sems[e][sn // SEM_CHUNK], 1)
                    continue
                ins = o.fn(h)
                self.stats[e][0] += 1
                if o.dma:
                    ins.then_inc(ring[e][o.qidx % DMA_RING], 16)
                elif i in sig:
                    sn = sig[i]
                    ins.then_inc(eng_sems[e][sn // SEM_CHUNK], 1)
            if self.dma_count[e] > 0:
                for pj in dma_ids[e][-DMA_RING:]:
                    sem_wait(h, "dma", pj)

        return emit_engine


class Arena:
    def __init__(self, ap):
        self.ap = ap
        self.n = ap.shape[1]
        self.o = 0

    def reset(self):
        self.o = 0

    def f32(self, *shape):
        n = int(np.prod(shape))
        assert self.o + n <= self.n, ("arena overflow", self.o, n, self.n)
        v = self.ap[:, self.o:self.o + n]
        self.o += n
        if len(shape) == 2:
            v = v.rearrange("p (a b) -> p a b", a=shape[0])
        elif len(shape) == 3:
            v = v.rearrange("p (a b c) -> p a b c", a=shape[0], b=shape[1])
        return v

    def bf16(self, *shape):
        n = int(np.prod(shape))
        assert n % 2 == 0
        assert self.o + n // 2 <= self.n, ("arena overflow", self.o, n, self.n)
        v = self.ap[:, self.o:self.o + n // 2].bitcast(BF16)
        self.o += n // 2
        if len(shape) == 2:
            v = v.rearrange("p (a b) -> p a b", a=shape[0])
        elif len(shape) == 3:
            v = v.rearrange("p (a b c) -> p a b c", a=shape[0], b=shape[1])
        return v


class Ctx:
    pass


def mm_acc(e, out, pairs):
    n = len(pairs)
    ins = None
    for i, (l, r) in enumerate(pairs):
        ins = e.matmul(out, lhsT=l, rhs=r, start=(i == 0), stop=(i == n - 1))
    return ins


def norm_mod(P, C, xt, xkey, h, hkey, gs, sh, bufs, tag, ncols, ps, pskey, h32=None, tmptag=None):
    sqb, tmp, rs = bufs
    if tmptag is None:
        tmptag = tag
    P.op("act", lambda e: e.activation(out=sqb, in_=xt, func=AF.Square), reads=[xkey], writes=[(tag, "sq")])
    P.op("pe", lambda e: mm_acc(e, ps[:, 0:ncols], [(C.ones_bf, sqb[:, k, :]) for k in range(KC)]),
         reads=[(tag, "sq")], writes=[pskey])
    P.op("act", lambda e: e.activation(out=rs, in_=ps[:, 0:ncols], func=AF.Sqrt, bias=C.eps_col, scale=1.0 / D),
         reads=[pskey], writes=[(tag, "rs")])
    P.op("dve", lambda e: e.reciprocal(out=rs, in_=rs), reads=[(tag, "rs")], writes=[(tag, "rs")])
    for k in range(KC):
        P.op("dve", lambda e, k=k: e.scalar_tensor_tensor(out=tmp[:, k, :], in0=xt[:, k, :], scalar=gs[:, k:k + 1],
                                                          in1=rs, op0=ALU.mult, op1=ALU.mult),
             reads=[xkey, (tag, "rs")], writes=[(tmptag, "tmp", k)])
        if h32 is not None:
            P.op("act", lambda e, k=k: e.activation(out=h32[:, k, :], in_=tmp[:, k, :], func=AF.Identity,
                                                    bias=sh[:, k:k + 1], scale=1.0),
                 reads=[(tmptag, "tmp", k)], writes=[(tmptag, "h32", k)])
            P.op("act", lambda e, k=k: e.copy(out=h[:, k, :], in_=h32[:, k, :]),
                 reads=[(tmptag, "h32", k)], writes=[(hkey, k)])
        else:
            P.op("act", lambda e, k=k: e.activation(out=h[:, k, :], in_=tmp[:, k, :], func=AF.Identity,
                                                    bias=sh[:, k:k + 1], scale=1.0),
                 reads=[(tmptag, "tmp", k)], writes=[(hkey, k)])


def frac_round(P, eng, out, in_, tmp, rkeys, wkeys, tkey):
    P.op(eng, lambda e: e.tensor_scalar(out=tmp, in0=in_, scalar1=MAG, scalar2=None, op0=ALU.add),
         reads=rkeys, writes=[tkey])
    P.op(eng, lambda e: e.tensor_scalar(out=tmp, in0=tmp, scalar1=MAG, scalar2=None, op0=ALU.subtract),
         reads=[tkey], writes=[tkey])
    P.op(eng, lambda e: e.tensor_tensor(out=out, in0=in_, in1=tmp, op=ALU.subtract),
         reads=list(rkeys) + [tkey], writes=wkeys)


def xview(ap):
    return ap.rearrange("(kc p) t -> p kc t", p=128)


def adaln_group(P, C, layer, gq, awb, key, psm):
    src = C.d_adaw[layer].rearrange("(kc p) n -> p kc n", p=128)[:, :, gq * 512:(gq + 1) * 512]
    P.op("pool", lambda e: e.dma_start(out=awb, in_=src), writes=[key], dma=True)

    def f(e):
        ins = None
        for jj in range(4):
            col = gq * 4 + jj
            for k in range(KC):
                ins = e.matmul(psm[:, col:col + 1], lhsT=awb[:, k, jj * 128:(jj + 1) * 128],
                               rhs=C.cndb[:, k:k + 1], start=(k == 0), stop=(k == KC - 1))
        return ins
    P.op("pe", f, reads=[key, "cndb"], writes=[("psm", layer)])


def adaln_finish(P, C, layer, psm):
    i = layer
    P.op("dve", lambda e: e.tensor_tensor(out=C.mod[:, i * 48:(i + 1) * 48], in0=psm[:, 0:48], in1=C.adab[:, i * 48:(i + 1) * 48], op=ALU.add),
         reads=[("psm", layer), "adab"], writes=[("mod", i)])
    for s_ in range(2):
        o = i * 48 + 24 * s_
        P.op("dve", lambda e, s_=s_, o=o: e.scalar_tensor_tensor(
            out=C.gs[:, i, s_, :], in0=C.mod[:, o + 8:o + 16], scalar=1.0, in1=C.ng[:, i, s_, :],
            op0=ALU.add, op1=ALU.mult), reads=[("mod", i), "ng"], writes=[("gs", i, s_)])


def phase_prologue(P, C, A):
    A.reset()
    cnd = A.f32(KC)
    tmpc = A.f32(KC)
    awb = [A.bf16(KC, 512) for _ in range(3)]
    P.op("sp", lambda e: e.dma_start(out=cnd, in_=C.d_cond), writes=["cnd"], dma=True)
    P.op("sp", lambda e: e.dma_start(out=C.adab, in_=C.d_adab), writes=["adab"], dma=True)
    P.op("sp", lambda e: e.dma_start(out=C.ng, in_=C.d_ng), writes=["ng"], dma=True)
    P.op("sp", lambda e: e.dma_start(out=C.fing, in_=C.d_fing), writes=["fing"], dma=True)
    P.op("dve", lambda e: e.memset(C.ones_bf, 1.0), writes=["ones"])
    P.op("dve", lambda e: e.memset(C.eps_col, EPS), writes=["eps"])
    P.op("act", lambda e: e.activation(out=tmpc, in_=cnd, func=AF.Silu), reads=["cnd"], writes=["tmpc"])
    P.op("act", lambda e: e.copy(out=C.cndb, in_=tmpc), reads=["tmpc"], writes=["cndb"])
    for gq in range(12):
        adaln_group(P, C, 0, gq, awb[gq % 3], ("awb", gq % 3), C.PS[7])
    adaln_finish(P, C, 0, C.PS[7])
    P.barrier()


def load_w_sq(P, C, dst, src, key):
    v = src.rearrange("(kc p) n -> p kc n", p=128)
    N = v.shape[2]
    step = 512
    for c0 in range(0, N, step):
        c1 = min(N, c0 + step)
        P.op("pool", lambda e, c0=c0, c1=c1: e.dma_start(out=dst[:, :, c0:c1], in_=v[:, :, c0:c1]),
             writes=[(key, c0 // step)], dma=True)
    return [(key, c // step) for c in range(0, N, step)]


def phase_s5(P, C, A, li, src, dst, sid, did):
    nc = C.nc
    j = li // 2
    T = 512
    NT = SEQ // T
    A.reset()
    w_in = A.bf16(KC, D)
    w_glu = A.bf16(KC, D)
    w_out = A.bf16(KC, D)
    BrT = A.bf16(32, 128)
    BiT = A.bf16(32, 128)
    CrT = A.bf16(32, 128)
    nCiT = A.bf16(32, 128)
    kw_in = load_w_sq(P, C, w_in, C.d_ssm_in[j], "w_in")
    kw_glu = load_w_sq(P, C, w_glu, C.d_ssm_glu[j], "w_glu")
    kw_out = load_w_sq(P, C, w_out, C.d_ssm_out[j], "w_out")
    RHO = A.f32(32)
    THF = A.f32(32)
    BASE = A.f32(32, 8)
    ST_R = A.f32(32)
    ST_I = A.f32(32)
    Dt = A.f32(KC)
    pq = A.f32(3, 32)
    t32 = [A.f32(32) for _ in range(3)]
    P.op("sp", lambda e: e.dma_start(out=pq, in_=C.d_ssm_pq[j]), writes=["pq"], dma=True)
    P.op("sp", lambda e: e.dma_start(out=Dt, in_=C.d_ssm_d[j]), writes=["Dt"], dma=True)
    P.op("dve", lambda e: e.memset(ST_R, 0.0), writes=["ST_R"])
    P.op("dve", lambda e: e.memset(ST_I, 0.0), writes=["ST_I"])
    P.op("act", lambda e: e.activation(out=t32[0], in_=pq[:, 2, :], func=AF.Exp), reads=["pq"], writes=["t32_0"])
    P.op("dve", lambda e: e.tensor_tensor(out=t32[1], in0=pq[:, 0, :], in1=t32[0], op=ALU.mult),
         reads=["pq", "t32_0"], writes=["t32_1"])
    P.op("act", lambda e: e.activation(out=RHO, in_=t32[1], func=AF.Exp), reads=["t32_1"], writes=["RHO"])
    P.op("dve", lambda e: e.tensor_tensor(out=t32[1], in0=pq[:, 1, :], in1=t32[0], op=ALU.mult),
         reads=["pq", "t32_0", "RHO"], writes=["t32_1"])
    P.op("dve", lambda e: e.tensor_scalar(out=t32[1], in0=t32[1], scalar1=1.0 / TWO_PI, scalar2=None, op0=ALU.mult),
         reads=["t32_1"], writes=["t32_1"])
    frac_round(P, "dve", THF, t32[1], t32[2], ["t32_1"], ["THF"], "t32_2")
    P.op("dve", lambda e: e.tensor_scalar(out=t32[0], in0=THF, scalar1=512.0, scalar2=None, op0=ALU.mult),
         reads=["THF"], writes=["t32_0"])
    frac_round(P, "dve", t32[1], t32[0], t32[2], ["t32_0"], ["t32_1"], "t32_2")
    for tt in range(NT):
        P.op("dve", lambda e, tt=tt: e.tensor_scalar(out=t32[0], in0=t32[1], scalar1=float(tt), scalar2=None, op0=ALU.mult),
             reads=["t32_1"], writes=["t32_0"])
        frac_round(P, "dve", BASE[:, :, tt], t32[0], t32[2], ["t32_0"], [("BASE", tt)], "t32_2")
    CH = 512
    mark = A.o
    rowp = [A.f32(CH) for _ in range(3)]
    bt = [A.f32(CH) for _ in range(2)]
    ct = [A.f32(CH) for _ in range(2)]
    w = [A.f32(CH) for _ in range(10)]
    for cc in range(8):
        sl = slice(cc * CH, (cc + 1) * CH)
        rk = ("rowp", cc)
        P.op("sp", lambda e, sl=sl: e.dma_start(out=rowp[0], in_=C.d_ssm_row[j][0][:, sl]), writes=["rowp0"], dma=True)
        P.op("sp", lambda e, sl=sl: e.dma_start(out=rowp[1], in_=C.d_ssm_row[j][1][:, sl]), writes=["rowp1"], dma=True)
        P.op("sp", lambda e, sl=sl: e.dma_start(out=rowp[2], in_=C.d_ssm_row[j][2][:, sl]), writes=["rowp2"], dma=True)
        P.op("sp", lambda e, sl=sl: e.dma_start(out=bt[0], in_=C.d_ssm_bt[j][0][:, sl]), writes=["bt0"], dma=True)
        P.op("sp", lambda e, sl=sl: e.dma_start(out=bt[1], in_=C.d_ssm_bt[j][1][:, sl]), writes=["bt1"], dma=True)
        P.op("sp", lambda e, sl=sl: e.dma_start(out=ct[0], in_=C.d_ssm_ct[j][0][:, sl]), writes=["ct0"], dma=True)
        P.op("sp", lambda e, sl=sl: e.dma_start(out=ct[1], in_=C.d_ssm_ct[j][1][:, sl]), writes=["ct1"], dma=True)
        lr, lim, ldt = rowp
        P.op("act", lambda e: e.activation(out=w[0], in_=ldt, func=AF.Exp), reads=["rowp2"], writes=["w0"])
        P.op("dve", lambda e: e.tensor_tensor(out=w[1], in0=lr, in1=w[0], op=ALU.mult), reads=["rowp0", "w0"], writes=["w1"])
        P.op("act", lambda e: e.activation(out=w[2], in_=w[1], func=AF.Exp), reads=["w1"], writes=["w2"])
        P.op("dve", lambda e: e.tensor_tensor(out=w[3], in0=lim, in1=w[0], op=ALU.mult), reads=["rowp1", "w0"], writes=["w3"])
        P.op("dve", lambda e: e.tensor_scalar(out=w[3], in0=w[3], scalar1=1.0 / TWO_PI, scalar2=None, op0=ALU.mult),
             reads=["w3"], writes=["w3"])
        frac_round(P, "dve", w[4], w[3], w[5], ["w3"], ["w4"], "w5")
        P.op("act", lambda e: e.activation(out=w[5], in_=w[4], func=AF.Sin, scale=TWO_PI), reads=["w4"], writes=["w5"])
        P.op("act", lambda e: e.activation(out=w[6], in_=w[4], func=AF.Abs), reads=["w4"], writes=["w6"])
        P.op("act", lambda e: e.activation(out=w[6], in_=w[6], func=AF.Sin, scale=-TWO_PI, bias=C.halfpi_col),
             reads=["w6"], writes=["w6"])
        P.op("dve", lambda e: e.tensor_tensor(out=w[6], in0=w[6], in1=w[2], op=ALU.mult), reads=["w6", "w2"], writes=["w6"])
        P.op("dve", lambda e: e.tensor_tensor(out=w[5], in0=w[5], in1=w[2], op=ALU.mult), reads=["w5", "w2"], writes=["w5"])
        P.op("dve", lambda e: e.tensor_scalar(out=w[6], in0=w[6], scalar1=-1.0, scalar2=None, op0=ALU.add), reads=["w6"], writes=["w6"])
        P.op("dve", lambda e: e.tensor_tensor(out=w[7], in0=lr, in1=lr, op=ALU.mult), reads=["rowp0"], writes=["w7"])
        P.op("dve", lambda e: e.tensor_tensor(out=w[8], in0=lim, in1=lim, op=ALU.mult), reads=["rowp1"], writes=["w8"])
        P.op("dve", lambda e: e.tensor_tensor(out=w[7], in0=w[7], in1=w[8], op=ALU.add), reads=["w7", "w8"], writes=["w7"])
        P.op("dve", lambda e: e.reciprocal(out=w[7], in_=w[7]), reads=["w7"], writes=["w7"])
        P.op("dve", lambda e: e.tensor_tensor(out=w[8], in0=w[6], in1=lr, op=ALU.mult), reads=["w6", "rowp0"], writes=["w8"])
        P.op("dve", lambda e: e.tensor_tensor(out=w[9], in0=w[5], in1=lim, op=ALU.mult), reads=["w5", "rowp1"], writes=["w9"])
        P.op("dve", lambda e: e.tensor_tensor(out=w[8], in0=w[8], in1=w[9], op=ALU.add), reads=["w8", "w9"], writes=["w8"])
        P.op("dve", lambda e: e.tensor_tensor(out=w[8], in0=w[8], in1=w[7], op=ALU.mult), reads=["w8", "w7"], writes=["w8"])
        P.op("dve", lambda e: e.tensor_tensor(out=w[9], in0=w[5], in1=lr, op=ALU.mult), reads=["w5", "rowp0"], writes=["w9"])
        P.op("dve", lambda e: e.tensor_tensor(out=w[0], in0=w[6], in1=lim, op=ALU.mult), reads=["w6", "rowp1"], writes=["w0"])
        P.op("dve", lambda e: e.tensor_tensor(out=w[9], in0=w[9], in1=w[0], op=ALU.subtract), reads=["w9", "w0"], writes=["w9"])
        P.op("dve", lambda e: e.tensor_tensor(out=w[9], in0=w[9], in1=w[7], op=ALU.mult), reads=["w9", "w7"], writes=["w9"])
        q0 = cc * 4
        brv = BrT[:, q0:q0 + 4, :].rearrange("p a b -> p (a b)")
        biv = BiT[:, q0:q0 + 4, :].rearrange("p a b -> p (a b)")
        crv = CrT[:, q0:q0 + 4, :].rearrange("p a b -> p (a b)")
        civ = nCiT[:, q0:q0 + 4, :].rearrange("p a b -> p (a b)")
        P.op("dve", lambda e: e.tensor_tensor(out=w[1], in0=w[8], in1=bt[0], op=ALU.mult), reads=["w8", "bt0"], writes=["w1"])
        P.op("dve", lambda e: e.tensor_tensor(out=w[2], in0=w[9], in1=bt[1], op=ALU.mult), reads=["w9", "bt1"], writes=["w2"])
        P.op("dve", lambda e, brv=brv: e.tensor_tensor(out=brv, in0=w[1], in1=w[2], op=ALU.subtract),
             reads=["w1", "w2"], writes=[("BrT", cc)])
        P.op("dve", lambda e: e.tensor_tensor(out=w[1], in0=w[8], in1=bt[1], op=ALU.mult), reads=["w8", "bt1", ("BrT", cc)], writes=["w1"])
        P.op("dve", lambda e: e.tensor_tensor(out=w[2], in0=w[9], in1=bt[0], op=ALU.mult), reads=["w9", "bt0", ("BrT", cc)], writes=["w2"])
        P.op("dve", lambda e, biv=biv: e.tensor_tensor(out=biv, in0=w[1], in1=w[2], op=ALU.add),
             reads=["w1", "w2"], writes=[("BiT", cc)])
        P.op("act", lambda e, crv=crv: e.copy(out=crv, in_=ct[0]), reads=["ct0"], writes=[("CrT", cc)])
        P.op("act", lambda e, civ=civ: e.mul(out=civ, in_=ct[1], mul=-1.0), reads=["ct1"], writes=[("nCiT", cc)])
    P.barrier()
    A.o = mark
    xt = A.f32(KC, T)
    tmp = A.f32(KC, T)
    sqb = A.bf16(KC, T)
    rs = A.f32(T)
    h = A.bf16(KC, T)
    u = A.bf16(KC, T)
    zb = A.bf16(KC, T)
    z2b = A.bf16(KC, T)
    y32 = tmp
    NPB = 2
    G = [A.f32(T) for _ in range(NPB)]
    FS = [A.f32(T) for _ in range(NPB)]
    COS = [A.f32(T) for _ in range(NPB)]
    SIN = [A.f32(T) for _ in range(NPB)]
    wr = [A.f32(T) for _ in range(NPB)]
    wi = [A.f32(T) for _ in range(NPB)]
    xr = [A.bf16(T) for _ in range(NPB)]
    xi = [A.bf16(T) for _ in range(NPB)]
    RAB = A.f32(T)
    t1 = A.f32(T)
    t2 = A.f32(T)
    cr = A.f32(T)
    ci = A.f32(T)
    p1 = A.f32(T)
    p2 = A.f32(T)
    sg = A.f32(T)
    PS = C.PS
    gs = C.gs[:, li, 0, :]
    sh = C.mod[:, li * 48 + 0: li * 48 + 8]
    g1 = C.mod[:, li * 48 + 16: li * 48 + 24]
    sv = xview(src)
    dv = xview(dst)
    NQ = 32

    def stage_a(t, q):
        b = q % NPB
        P.op("act", lambda e: e.activation(out=G[b], in_=C.tloc, func=AF.Identity, scale=THF[:, q:q + 1], bias=BASE[:, q, t:t + 1]),
             reads=[], writes=[("G", b)])
        P.op("act", lambda e: e.activation(out=RAB, in_=G[b], func=AF.Identity, bias=C.magp_col, scale=1.0),
             reads=[("G", b)], writes=[("RAB",)])
        P.op("act", lambda e: e.activation(out=RAB, in_=RAB, func=AF.Identity, bias=C.magn_col, scale=1.0),
             reads=[("RAB",)], writes=[("RAB",)])
        P.op("dve", lambda e: e.tensor_tensor(out=FS[b], in0=G[b], in1=RAB, op=ALU.subtract),
             reads=[("G", b), ("RAB",)], writes=[("FS", b)])
        P.op("act", lambda e: e.activation(out=SIN[b], in_=FS[b], func=AF.Sin, scale=TWO_PI), reads=[("FS", b)], writes=[("SIN", b)])
        P.op("act", lambda e: e.activation(out=COS[b], in_=FS[b], func=AF.Abs), reads=[("FS", b)], writes=[("COS", b)])
        P.op("act", lambda e: e.activation(out=COS[b], in_=COS[b], func=AF.Sin, scale=-TWO_PI, bias=C.halfpi_col),
             reads=[("COS", b)], writes=[("COS", b)])

    def stage_bproj(q):
        b = q % NPB
        dc = q // 4
        pr = 4 + 2 * b
        pi_ = pr + 1
        P.op("pe", lambda e: e.matmul(PS[pr], lhsT=BrT[:, q, :], rhs=u[:, dc, :], start=True, stop=True),
             reads=[("u", dc)], writes=[("ps", pr)])
        P.op("pe", lambda e: e.matmul(PS[pi_], lhsT=BiT[:, q, :], rhs=u[:, dc, :], start=True, stop=True),
             reads=[("u", dc)], writes=[("ps", pi_)])

    def stage_scan(q):
        b = q % NPB
        pr = 4 + 2 * b
        pi_ = pr + 1
        P.op("dve", lambda e: e.tensor_tensor(out=t1, in0=PS[pr], in1=COS[b], op=ALU.mult), reads=[("ps", pr), ("COS", b)], writes=[("t1",)])
        P.op("dve", lambda e: e.tensor_tensor(out=t2, in0=PS[pi_], in1=SIN[b], op=ALU.mult), reads=[("ps", pi_), ("SIN", b)], writes=[("t2",)])
        P.op("dve", lambda e: e.tensor_tensor(out=cr, in0=t1, in1=t2, op=ALU.add), reads=[("t1",), ("t2",)], writes=[("cr",)])
        P.op("dve", lambda e: e.tensor_tensor(out=t1, in0=PS[pi_], in1=COS[b], op=ALU.mult), reads=[("ps", pi_), ("COS", b), ("cr",)], writes=[("t1",)])
        P.op("dve", lambda e: e.tensor_tensor(out=t2, in0=PS[pr], in1=SIN[b], op=ALU.mult), reads=[("ps", pr), ("SIN", b), ("cr",)], writes=[("t2",)])
        P.op("dve", lambda e: e.tensor_tensor(out=ci, in0=t1, in1=t2, op=ALU.subtract), reads=[("t1",), ("t2",)], writes=[("ci",)])
        rho_b = RHO[:, q:q + 1].to_broadcast([128, T])
        P.op("dve", lambda e: e.tensor_tensor_scan(out=wr[b], data0=rho_b, data1=cr, initial=ST_R[:, q:q + 1], op0=ALU.mult, op1=ALU.add),
             reads=[("cr",), ("ST_R", q)], writes=[("wr", b)])
        P.op("dve", lambda e: e.tensor_tensor_scan(out=wi[b], data0=rho_b, data1=ci, initial=ST_I[:, q:q + 1], op0=ALU.mult, op1=ALU.add),
             reads=[("ci",), ("ST_I", q)], writes=[("wi", b)])

    def stage_rot(q):
        b = q % NPB
        P.op("pool", lambda e: e.tensor_copy(out=ST_R[:, q:q + 1], in_=wr[b][:, T - 1:T]), reads=[("wr", b)], writes=[("ST_R", q)])
        P.op("pool", lambda e: e.tensor_copy(out=ST_I[:, q:q + 1], in_=wi[b][:, T - 1:T]), reads=[("wi", b)], writes=[("ST_I", q)])
        P.op("pool", lambda e: e.tensor_tensor(out=p1, in0=wr[b], in1=COS[b], op=ALU.mult), reads=[("wr", b), ("COS", b)], writes=[("p1",)])
        P.op("pool", lambda e: e.tensor_tensor(out=p2, in0=wi[b], in1=SIN[b], op=ALU.mult), reads=[("wi", b), ("SIN", b)], writes=[("p2",)])
        P.op("pool", lambda e: e.tensor_tensor(out=xr[b], in0=p1, in1=p2, op=ALU.subtract), reads=[("p1",), ("p2",)], writes=[("xr", b)])
        P.op("pool", lambda e: e.tensor_tensor(out=p1, in0=wr[b], in1=SIN[b], op=ALU.mult), reads=[("wr", b), ("SIN", b), ("xr", b)], writes=[("p1",)])
        P.op("pool", lambda e: e.tensor_tensor(out=p2, in0=wi[b], in1=COS[b], op=ALU.mult), reads=[("wi", b), ("COS", b), ("xr", b)], writes=[("p2",)])
        P.op("pool", lambda e: e.tensor_tensor(out=xi[b], in0=p1, in1=p2, op=ALU.add), reads=[("p1",), ("p2",)], writes=[("xi", b)])

    def stage_cproj(q):
        b = q % NPB
        dc = q // 4
        qq = q % 4
        P.op("pe", lambda e: e.matmul(PS[3], lhsT=CrT[:, q, :], rhs=xr[b], start=(qq == 0), stop=False),
             reads=[("xr", b)], writes=[("ps", 3)])
        P.op("pe", lambda e: e.matmul(PS[3], lhsT=nCiT[:, q, :], rhs=xi[b], start=False, stop=(qq == 3)),
             reads=[("xi", b)], writes=[("ps", 3)])
        if qq == 3:
            P.op("dve", lambda e: e.scalar_tensor_tensor(out=y32[:, dc, :], in0=u[:, dc, :], scalar=Dt[:, dc:dc + 1], in1=PS[3],
                                                         op0=ALU.mult, op1=ALU.add),
                 reads=[("ps", 3), ("u", dc)], writes=[("y32", dc)])
            P.op("act", lambda e: e.activation(out=zb[:, dc, :], in_=y32[:, dc, :], func=AF.Gelu_apprx_tanh),
                 reads=[("y32", dc)], writes=[("zb", dc)])

    def head(t):
        P.op("sp", lambda e: e.dma_start(out=xt, in_=sv[:, :, t * T:(t + 1) * T]), reads=[(sid, t)], writes=["xt"], dma=True)
        stage_a(t, 0)
        norm_mod(P, C, xt, "xt", h, "h", gs, sh, (sqb, tmp, rs), "nm", T, PS[0], "ps0")
        for m in range(KC):
            pb = 1 + (m % 2)
            P.op("pe", lambda e, m=m, pb=pb: mm_acc(e, PS[pb], [(w_in[:, k, m * 128:(m + 1) * 128], h[:, k, :]) for k in range(KC)]),
                 reads=[("h", k) for k in range(KC)], writes=[("ps", pb)])
            P.op("act", lambda e, m=m, pb=pb: e.copy(out=u[:, m, :], in_=PS[pb]), reads=[("ps", pb)], writes=[("u", m)])

    def glu_piece(m):
        pb = 1 + (m % 2)
        P.op("pe", lambda e: mm_acc(e, PS[pb], [(w_glu[:, k, m * 128:(m + 1) * 128], zb[:, k, :]) for k in range(KC)]),
             reads=[("zb", k) for k in range(KC)], writes=[("ps", pb)])
        P.op("act", lambda e: e.activation(out=sg, in_=PS[pb], func=AF.Sigmoid), reads=[("ps", pb)], writes=["sg"])
        P.op("dve", lambda e: e.tensor_tensor(out=z2b[:, m, :], in0=zb[:, m, :], in1=sg, op=ALU.mult),
             reads=["sg", ("zb", m)], writes=[("z2b", m)])

    def out_piece(m):
        pb = 1 + (m % 2)
        P.op("pe", lambda e: mm_acc(e, PS[pb], [(w_out[:, k, m * 128:(m + 1) * 128], z2b[:, k, :]) for k in range(KC)]),
             reads=[("z2b", k) for k in range(KC)], writes=[("ps", pb)])
        P.op("dve", lambda e: e.scalar_tensor_tensor(out=xt[:, m, :], in0=PS[pb], scalar=g1[:, m:m + 1], in1=xt[:, m, :],
                                                     op0=ALU.mult, op1=ALU.add),
             reads=[("ps", pb), "xt"], writes=["xt"])

    def tail_piece(t, i, reload):
        if i == 0 and reload:
            P.op("sp", lambda e: e.dma_start(out=xt, in_=sv[:, :, t * T:(t + 1) * T]), reads=[(sid, t)], writes=["xt"], dma=True)
        if i < 3:
            for m in range(3 * i, min(KC, 3 * i + 3)):
                glu_piece(m)
        elif i < 11:
            out_piece(i - 3)
            if i == 10:
                P.op("sp", lambda e: e.dma_start(out=dv[:, :, t * T:(t + 1) * T], in_=xt), reads=["xt"], writes=[(did, t)], dma=True)

    def core(t, extra=None):
        stage_bproj(0)
        for q in range(NQ):
            if q + 1 < NQ:
                stage_a(t, q + 1)
                stage_bproj(q + 1)
            stage_scan(q)
            stage_rot(q)
            if q >= 1:
                stage_cproj(q - 1)
            if extra is not None and q < 11:
                extra(q)
        stage_cproj(NQ - 1)

    head(0)
    core(0)
    for t in range(1, NT):
        head(t)
        core(t, extra=lambda i, tt=t - 1: tail_piece(tt, i, True))
    for i in range(11):
        tail_piece(NT - 1, i, True)
    P.barrier()


def phase_ffn(P, C, A, li, src, dst, sid, did):
    j = li // 2
    T = 256
    NT = SEQ // T
    A.reset()
    w1 = A.bf16(KC, DFF)
    w3 = A.bf16(KC, DFF)
    w2 = A.bf16(NFC, D)
    load_w_sq(P, C, w1, C.d_ffn_w1[j], "w1")
    load_w_sq(P, C, w3, C.d_ffn_w3[j], "w3")
    v2 = C.d_ffn_w2[j].rearrange("(fc p) n -> p fc n", p=128)
    for f0 in range(0, NFC, 4):
        f1 = min(NFC, f0 + 4)
        P.op("pool", lambda e, f0=f0, f1=f1: e.dma_start(out=w2[:, f0:f1, :], in_=v2[:, f0:f1, :]), writes=[("w2", f0)], dma=True)
    P.barrier()
    xt = [A.f32(KC, T) for _ in range(2)]
    sqb = [A.bf16(KC, T) for _ in range(2)]
    rs = [A.f32(T) for _ in range(2)]
    h = [A.bf16(KC, T) for _ in range(2)]
    tmp = A.f32(KC, T)
    a = A.bf16(NFC, T)
    sg = [A.f32(T) for _ in range(2)]
    awb = A.bf16(KC, 512)
    PS = C.PS
    gs = C.gs[:, li, 1, :]
    sh = C.mod[:, li * 48 + 24: li * 48 + 32]
    g2 = C.mod[:, li * 48 + 40: li * 48 + 48]
    sv = xview(src)
    dv = xview(dst)
    nxt = li + 1
    for t in range(NT):
        bb = t % 2
        xk = ("xt", bb)
        hk = ("h", bb)
        P.op("sp", lambda e, t=t, bb=bb: e.dma_start(out=xt[bb], in_=sv[:, :, t * T:(t + 1) * T]), reads=[(sid, t)], writes=[xk], dma=True)
        norm_mod(P, C, xt[bb], xk, h[bb], hk, gs, sh, (sqb[bb], tmp, rs[bb]), ("nm", bb), T, PS[0], "ps0", tmptag="nmT")
        if nxt < DEPTH and t < 12:
            adaln_group(P, C, nxt, t, awb, "awb", PS[7])
            if t == 11:
                adaln_finish(P, C, nxt, PS[7])
        for fc in range(NFC):
            pg = 1 + 2 * (fc % 2)
            pu = pg + 1
            b = fc % 2
            P.op("pe", lambda e, fc=fc, pg=pg, bb=bb: mm_acc(e, PS[pg][:, 0:T], [(w1[:, k, fc * 128:(fc + 1) * 128], h[bb][:, k, :]) for k in range(KC)]),
                 reads=[(hk, k) for k in range(KC)], writes=[("ps", pg)])
            P.op("pe", lambda e, fc=fc, pu=pu, bb=bb: mm_acc(e, PS[pu][:, 0:T], [(w3[:, k, fc * 128:(fc + 1) * 128], h[bb][:, k, :]) for k in range(KC)]),
                 reads=[(hk, k) for k in range(KC)], writes=[("ps", pu)])
            P.op("act", lambda e, pg=pg, b=b: e.activation(out=sg[b], in_=PS[pg][:, 0:T], func=AF.Silu), reads=[("ps", pg)], writes=[("sg", b)])
            P.op("dve", lambda e, fc=fc, pu=pu, b=b: e.tensor_tensor(out=a[:, fc, :], in0=PS[pu][:, 0:T], in1=sg[b], op=ALU.mult),
                 reads=[("ps", pu), ("sg", b)], writes=[("a", fc)])
        for m in range(KC):
            pb = 5 + (m % 2)
            P.op("pe", lambda e, m=m, pb=pb: mm_acc(e, PS[pb][:, 0:T], [(w2[:, fc, m * 128:(m + 1) * 128], a[:, fc, :]) for fc in range(NFC)]),
                 reads=[("a", fc) for fc in range(NFC)], writes=[("ps", pb)])
            P.op("dve", lambda e, m=m, pb=pb, bb=bb: e.scalar_tensor_tensor(out=xt[bb][:, m, :], in0=PS[pb][:, 0:T], scalar=g2[:, m:m + 1], in1=xt[bb][:, m, :],
                                                                          op0=ALU.mult, op1=ALU.add),
                 reads=[("ps", pb), xk], writes=[xk])
        P.op("sp", lambda e, t=t, bb=bb: e.dma_start(out=dv[:, :, t * T:(t + 1) * T], in_=xt[bb]), reads=[xk], writes=[(did, t)], dma=True)
    P.barrier()


def phase_pool(P, C, A, li, src, dst, sid, did):
    j = li // 2
    T = 512
    NT = SEQ // T
    HALO = 16
    A.reset()
    w_in = A.bf16(KC, D)
    w_out = A.bf16(KC, D)
    wmix = A.bf16(4, 2, 256)
    psc = A.f32(KC)
    load_w_sq(P, C, w_in, C.d_pool_in[j], "w_in")
    load_w_sq(P, C, w_out, C.d_pool_out[j], "w_out")
    for wg in range(4):
        P.op("pool", lambda e, wg=wg: e.dma_start(out=wmix[:, wg, :, :], in_=C.d_pool_mix[j][wg].rearrange("(kc p) n -> p kc n", p=128)),
             writes=[("wmix", wg)], dma=True)
    P.op("sp", lambda e: e.dma_start(out=psc, in_=C.d_pool_scale[j]), writes=["psc"], dma=True)
    P.barrier()
    xt = [A.f32(KC, T) for _ in range(2)]
    sqb = [A.bf16(KC, T) for _ in range(2)]
    rs = [A.f32(T) for _ in range(2)]
    h = [A.bf16(KC, T) for _ in range(2)]
    dm = [A.bf16(KC, T) for _ in range(2)]
    tmp = A.f32(KC, T)
    U = A.f32(KC, HALO + T)
    SA = A.f32(HALO + T)
    SB = A.f32(HALO + T)
    zb = A.bf16(KC, T)
    inv0 = A.f32(4, T)
    awb = A.bf16(KC, 512)
    P.op("sp", lambda e: e.dma_start(out=inv0, in_=C.d_inv0), writes=["inv0"], dma=True)
    P.op("dve", lambda e: e.memset(U, 0.0), writes=[("U", k) for k in range(KC)])
    PS = C.PS
    gs = C.gs[:, li, 0, :]
    sh = C.mod[:, li * 48 + 0: li * 48 + 8]
    g1 = C.mod[:, li * 48 + 16: li * 48 + 24]
    sv = xview(src)
    dv = xview(dst)
    W = (2, 4, 8, 16)
    nxt = li + 1
    for t in range(NT):
        bb = t % 2
        xk = ("xt", bb)
        hk = ("h", bb)
        P.op("sp", lambda e, t=t, bb=bb: e.dma_start(out=xt[bb], in_=sv[:, :, t * T:(t + 1) * T]), reads=[(sid, t)], writes=[xk], dma=True)
        norm_mod(P, C, xt[bb], xk, h[bb], hk, gs, sh, (sqb[bb], tmp, rs[bb]), ("nm", bb), T, PS[0], "ps0", tmptag="nmT")
        if nxt < DEPTH and t < 6:
            for gq in (2 * t, 2 * t + 1):
                adaln_group(P, C, nxt, gq, awb, "awb", PS[7])
            if t == 5:
                adaln_finish(P, C, nxt, PS[7])
        for m in range(KC):
            pb = 1 + (m % 2)
            wgi = m // 2
            wsz = W[wgi]
            P.op("pe", lambda e, m=m, pb=pb, bb=bb: mm_acc(e, PS[pb], [(w_in[:, k, m * 128:(m + 1) * 128], h[bb][:, k, :]) for k in range(KC)]),
                 reads=[(hk, k) for k in range(KC)], writes=[("ps", pb)])
            if t > 0:
                P.op("act", lambda e, m=m: e.copy(out=U[:, m, 0:HALO], in_=U[:, m, T:T + HALO]), reads=[("U", m)], writes=[("U", m)])
            P.op("act", lambda e, m=m, pb=pb: e.copy(out=U[:, m, HALO:HALO + T], in_=PS[pb]), reads=[("ps", pb)], writes=[("U", m)])
            cur = U[:, m, :]
            curk = ("U", m)
            sh_ = 1
            bufs = [SA, SB]
            bi = 0
            while sh_ < wsz:
                nb = bufs[bi]
                nk = ("S", bi)
                P.op("dve", lambda e, cur=cur, nb=nb, sh_=sh_: e.tensor_tensor(out=nb[:, sh_:HALO + T], in0=cur[:, sh_:HALO + T],
                                                                               in1=cur[:, 0:HALO + T - sh_], op=ALU.add),
                     reads=[curk], writes=[nk])
                cur = nb
                curk = nk
                bi ^= 1
                sh_ *= 2
            dmk = ("dm", bb, m)
            if t == 0:
                P.op("dve", lambda e, cur=cur, wgi=wgi: e.tensor_tensor(out=cur[:, HALO:HALO + T], in0=cur[:, HALO:HALO + T],
                                                                          in1=inv0[:, wgi, :], op=ALU.mult),
                     reads=[curk, "inv0"], writes=[curk])
                P.op("dve", lambda e, cur=cur, m=m, bb=bb: e.tensor_tensor(out=dm[bb][:, m, :], in0=cur[:, HALO:HALO + T],
                                                                             in1=U[:, m, HALO:HALO + T], op=ALU.subtract),
                     reads=[curk, ("U", m)], writes=[dmk])
            else:
                P.op("dve", lambda e, cur=cur, m=m, wsz=wsz, bb=bb: e.scalar_tensor_tensor(out=dm[bb][:, m, :], in0=cur[:, HALO:HALO + T],
                                                                                             scalar=1.0 / wsz, in1=U[:, m, HALO:HALO + T],
                                                                                             op0=ALU.mult, op1=ALU.subtract),
                     reads=[curk, ("U", m)], writes=[dmk])
        for wgi in range(4):
            for mo in range(2):
                m = 2 * wgi + mo
                pb = 3 + (m % 2)
                P.op("pe", lambda e, wgi=wgi, mo=mo, pb=pb, bb=bb: mm_acc(e, PS[pb], [(wmix[:, wgi, ki, mo * 128:(mo + 1) * 128], dm[bb][:, 2 * wgi + ki, :])
                                                                                    for ki in range(2)]),
                     reads=[("dm", bb, 2 * wgi), ("dm", bb, 2 * wgi + 1)], writes=[("ps", pb)])
                P.op("act", lambda e, m=m, pb=pb: e.activation(out=zb[:, m, :], in_=PS[pb], func=AF.Identity, scale=psc[:, m:m + 1]),
                     reads=[("ps", pb)], writes=[("zb", m)])
        for m in range(KC):
            pb = 5 + (m % 2)
            P.op("pe", lambda e, m=m, pb=pb: mm_acc(e, PS[pb], [(w_out[:, k, m * 128:(m + 1) * 128], zb[:, k, :]) for k in range(KC)]),
                 reads=[("zb", k) for k in range(KC)], writes=[("ps", pb)])
            P.op("dve", lambda e, m=m, pb=pb, bb=bb: e.scalar_tensor_tensor(out=xt[bb][:, m, :], in0=PS[pb], scalar=g1[:, m:m + 1], in1=xt[bb][:, m, :],
                                                                          op0=ALU.mult, op1=ALU.add),
                 reads=[("ps", pb), xk], writes=[xk])
        P.op("sp", lambda e, t=t, bb=bb: e.dma_start(out=dv[:, :, t * T:(t + 1) * T], in_=xt[bb]), reads=[xk], writes=[(did, t)], dma=True)
    P.barrier()


def phase_moe(P, C, A, li, src, dst, sid, did, final_out=None):
    j = li // 2
    T = 512
    HT = 4
    HN = HT * T
    PS = C.PS
    gs = C.gs[:, li, 1, :]
    sh = C.mod[:, li * 48 + 24: li * 48 + 32]
    g2 = C.mod[:, li * 48 + 40: li * 48 + 48]
    sv = xview(src)
    dv = xview(dst)
    FG = 512
    NFG = DFE // FG
    for half in range(2):
        A.reset()
        hH = A.bf16(KC, HN)
        GW = A.f32(16, NE)
        rw = A.f32(KC, NE)
        rbb = A.f32(NE)
        yacc = A.f32(KC, HN)
        mark = A.o
        xt = yacc[:, 0:2, :].rearrange("p a (b c) -> p (a b) c", b=4)
        tmp = yacc[:, 2:4, :].rearrange("p a (b c) -> p (a b) c", b=4)
        h32 = yacc[:, 4:6, :].rearrange("p a (b c) -> p (a b) c", b=4)
        sqb = A.bf16(KC, T)
        rs = A.f32(T)
        L = A.f32(NE)
        L2 = A.f32(NE)
        eq1 = A.f32(NE)
        eq2 = A.f32(NE)
        sm = A.f32(8)
        P.op("sp", lambda e: e.dma_start(out=rw, in_=C.d_router_w[j]), writes=["rw"], dma=True)
        P.op("sp", lambda e: e.dma_start(out=rbb, in_=C.d_router_b[j]), writes=["rbb"], dma=True)
        for tt in range(HT):
            t = half * HT + tt
            hv = hH[:, :, tt * T:(tt + 1) * T]
            P.op("sp", lambda e, t=t: e.dma_start(out=xt, in_=sv[:, :, t * T:(t + 1) * T]), reads=[(sid, t)], writes=["xt"], dma=True)
            norm_mod(P, C, xt, "xt", hv, ("hH", tt), gs, sh, (sqb, tmp, rs), "nm", T, PS[0], "ps0", h32=h32)
            for sub in range(4):
                si = tt * 4 + sub
                P.op("pe", lambda e, sub=sub: mm_acc(e, PS[1][:, 0:NE], [(h32[:, k, sub * 128:(sub + 1) * 128], rw[:, k, :]) for k in range(KC)]),
                     reads=[("nm", "h32", k) for k in range(KC)] + ["rw"], writes=[("ps", 1)])
                P.op("dve", lambda e: e.tensor_tensor(out=L, in0=PS[1][:, 0:NE], in1=rbb, op=ALU.add), reads=[("ps", 1), "rbb"], writes=["L"])
                P.op("dve", lambda e: e.reduce_max(out=sm[:, 0:1], in_=L, axis=mybir.AxisListType.X), reads=["L"], writes=["m1"])
                P.op("dve", lambda e: e.tensor_scalar(out=eq1, in0=L, scalar1=sm[:, 0:1], scalar2=None, op0=ALU.is_equal),
                     reads=["L", "m1"], writes=["eq1"])
                P.op("dve", lambda e: e.scalar_tensor_tensor(out=L2, in0=eq1, scalar=-1e30, in1=L, op0=ALU.mult, op1=ALU.add),
                     reads=["eq1", "L"], writes=["L2"])
                P.op("dve", lambda e: e.reduce_max(out=sm[:, 1:2], in_=L2, axis=mybir.AxisListType.X), reads=["L2"], writes=["m2"])
                P.op("dve", lambda e: e.tensor_scalar(out=eq2, in0=L2, scalar1=sm[:, 1:2], scalar2=None, op0=ALU.is_equal),
                     reads=["L2", "m2"], writes=["eq2"])
                P.op("dve", lambda e: e.tensor_tensor(out=sm[:, 2:3], in0=sm[:, 0:1], in1=sm[:, 1:2], op=ALU.subtract),
                     reads=["m1", "m2"], writes=["dd"])
                P.op("act", lambda e: e.activation(out=sm[:, 3:4], in_=sm[:, 2:3], func=AF.Sigmoid), reads=["dd"], writes=["p1"])
                P.op("act", lambda e: e.activation(out=sm[:, 4:5], in_=sm[:, 2:3], func=AF.Sigmoid, scale=-1.0), reads=["dd"], writes=["p2"])
                P.op("dve", lambda e: e.tensor_scalar(out=eq1, in0=eq1, scalar1=sm[:, 3:4], scalar2=None, op0=ALU.mult),
                     reads=["eq1", "p1", "L2"], writes=["eq1"])
                P.op("dve", lambda e, si=si: e.scalar_tensor_tensor(out=GW[:, si, :], in0=eq2, scalar=sm[:, 4:5], in1=eq1, op0=ALU.mult, op1=ALU.add),
                     reads=["eq2", "p2", "eq1"], writes=[("GW", si)])
        P.barrier()
        A.o = mark
        NWB = 2
        w1g = [A.bf16(KC, FG) for _ in range(NWB)]
        w3g = [A.bf16(KC, FG) for _ in range(NWB)]
        w2g = [A.bf16(4, D) for _ in range(NWB)]
        WB = [A.bf16(HN) for _ in range(2)]
        a = [A.bf16(4, T) for _ in range(2)]
        sg = [A.f32(T) for _ in range(2)]
        sgw = [A.f32(T) for _ in range(2)]
        def pre_expert(ex):
            wb = WB[ex % 2]
            wbk = ("WB", ex % 2)
            for c4 in range(4):
                def f(e, c4=c4):
                    ins = None
                    for s4 in range(4):
                        si = c4 * 4 + s4
                        ins = e.matmul(PS[0][:, s4 * 128:(s4 + 1) * 128], lhsT=GW[:, si, ex:ex + 1].to_broadcast([128, 128]),
                                       rhs=C.ident32, start=True, stop=True)
                    return ins
                P.op("pe", f, reads=[("GW", c4 * 4 + s4) for s4 in range(4)], writes=[("ps", 0)])
                P.op("act", lambda e, c4=c4: e.copy(out=wb[:, c4 * T:(c4 + 1) * T], in_=PS[0]), reads=[("ps", 0)], writes=[wbk])

        def pre_fg(ex, fg, b):
            f0 = fg * FG
            v1 = C.d_moe_w1[j][ex].rearrange("(kc p) n -> p kc n", p=128)[:, :, f0:f0 + FG]
            v3 = C.d_moe_w3[j][ex].rearrange("(kc p) n -> p kc n", p=128)[:, :, f0:f0 + FG]
            v2 = C.d_moe_w2[j][ex][f0:f0 + FG, :].rearrange("(fc p) n -> p fc n", p=128)
            P.op("pool", lambda e: e.dma_start(out=w1g[b], in_=v1), writes=[("w1g", b)], dma=True)
            P.op("pool", lambda e: e.dma_start(out=w3g[b], in_=v3), writes=[("w3g", b)], dma=True)
            P.op("pool", lambda e: e.dma_start(out=w2g[b], in_=v2), writes=[("w2g", b)], dma=True)

        def stage1_fc(ex, tt, b, ab, fc):
            wb = WB[ex % 2]
            wbk = ("WB", ex % 2)
            pg = 2 * (fc % 2)
            pu = pg + 1
            sb = fc % 2
            P.op("pe", lambda e: mm_acc(e, PS[pg], [(w1g[b][:, k, fc * 128:(fc + 1) * 128], hH[:, k, tt * T:(tt + 1) * T]) for k in range(KC)]),
                 reads=[("w1g", b)], writes=[("ps", pg)])
            P.op("pe", lambda e: mm_acc(e, PS[pu], [(w3g[b][:, k, fc * 128:(fc + 1) * 128], hH[:, k, tt * T:(tt + 1) * T]) for k in range(KC)]),
                 reads=[("w3g", b)], writes=[("ps", pu)])
            P.op("act", lambda e: e.activation(out=sg[sb], in_=PS[pg], func=AF.Silu), reads=[("ps", pg)], writes=[("sg", sb)])
            P.op("dve", lambda e: e.tensor_tensor(out=sgw[sb], in0=sg[sb], in1=wb[:, tt * T:(tt + 1) * T], op=ALU.mult),
                 reads=[("sg", sb), wbk], writes=[("sgw", sb)])
            P.op("dve", lambda e: e.tensor_tensor(out=a[ab][:, fc, :], in0=PS[pu], in1=sgw[sb], op=ALU.mult),
                 reads=[("ps", pu), ("sgw", sb)], writes=[("a", ab, fc)])

        def stage2(tt, b, ab, first):
            for m in range(KC):
                pb = 4 + (m % 4)
                P.op("pe", lambda e, m=m, pb=pb: mm_acc(e, PS[pb], [(w2g[b][:, fc, m * 128:(m + 1) * 128], a[ab][:, fc, :]) for fc in range(4)]),
                     reads=[("a", ab, fc) for fc in range(4)] + [("w2g", b)], writes=[("ps", pb)])
                yv = yacc[:, m, tt * T:(tt + 1) * T]
                if first:
                    P.op("dve", lambda e, yv=yv, pb=pb: e.tensor_copy(out=yv, in_=PS[pb]), reads=[("ps", pb)], writes=[("yacc", m, tt)])
                else:
                    P.op("dve", lambda e, yv=yv, pb=pb: e.tensor_tensor(out=yv, in0=PS[pb], in1=yv, op=ALU.add),
                         reads=[("ps", pb), ("yacc", m, tt)], writes=[("yacc", m, tt)])

        wcount = 0
        acount = 0
        pending = None
        for ex in range(NE):
            for fg in range(NFG):
                b = wcount % NWB
                wcount += 1
                first = (ex == 0 and fg == 0)
                for tt in range(HT):
                    ab = acount % 2
                    acount += 1
                    if tt == 0:
                        if fg == 0:
                            pre_expert(ex)
                        pre_fg(ex, fg, b)
                    stage1_fc(ex, tt, b, ab, 0)
                    if pending is not None:
                        stage2(*pending)
                    for fc in range(1, 4):
                        stage1_fc(ex, tt, b, ab, fc)
                    pending = (tt, b, ab, first)
        stage2(*pending)
        P.barrier()
        A.o = mark
        xt3 = [A.f32(KC, T) for _ in range(2)]
        if final_out is not None:
            fsq = A.bf16(KC, T)
            frs = A.f32(T)
        for tt in range(HT):
            t = half * HT + tt
            xb = xt3[tt % 2]
            xk = ("xt3", tt % 2)
            P.op("sp", lambda e, t=t, xb=xb: e.dma_start(out=xb, in_=sv[:, :, t * T:(t + 1) * T]), reads=[(sid, t)], writes=[xk], dma=True)
            for m in range(KC):
                P.op("dve", lambda e, m=m, xb=xb, tt=tt: e.scalar_tensor_tensor(out=xb[:, m, :], in0=yacc[:, m, tt * T:(tt + 1) * T],
                                                                              scalar=g2[:, m:m + 1], in1=xb[:, m, :], op0=ALU.mult, op1=ALU.add),
                     reads=[xk, ("yacc", m, tt)], writes=[xk])
            if final_out is not None:
                sqk = ("fsq", tt % 2)
                P.op("act", lambda e, xb=xb: e.activation(out=fsq, in_=xb, func=AF.Square), reads=[xk], writes=["fsq"])
                P.op("pe", lambda e: mm_acc(e, PS[0], [(C.ones_bf, fsq[:, k, :]) for k in range(KC)]), reads=["fsq"], writes=[("ps", 0)])
                P.op("act", lambda e: e.activation(out=frs, in_=PS[0], func=AF.Sqrt, bias=C.eps_col, scale=1.0 / D), reads=[("ps", 0)], writes=["frs"])
                P.op("dve", lambda e: e.reciprocal(out=frs, in_=frs), reads=["frs"], writes=["frs"])
                for k in range(KC):
                    P.op("dve", lambda e, k=k, xb=xb: e.scalar_tensor_tensor(out=xb[:, k, :], in0=xb[:, k, :], scalar=C.fing[:, k:k + 1], in1=frs,
                                                                           op0=ALU.mult, op1=ALU.mult),
                         reads=[xk, "frs"], writes=[xk])
                fv = xview(final_out)
                P.op("sp", lambda e, t=t, xb=xb, fv=fv: e.dma_start(out=fv[:, :, t * T:(t + 1) * T], in_=xb), reads=[xk], writes=[("out", t)], dma=True)
            else:
                P.op("sp", lambda e, t=t, xb=xb: e.dma_start(out=dv[:, :, t * T:(t + 1) * T], in_=xb), reads=[xk], writes=[(did, t)], dma=True)
        P.barrier()


def phase_final(P, C, A, src, dst, sid, did, do_norm=True):
    T = 512
    NT = SEQ // T
    A.reset()
    xt = [A.f32(KC, T) for _ in range(2)]
    sqb = A.bf16(KC, T)
    rs = A.f32(T)
    PS = C.PS
    sv = xview(src)
    dv = xview(dst)
    for t in range(NT):
        xb = xt[t % 2]
        xk = ("xt", t % 2)
        P.op("sp", lambda e, t=t, xb=xb: e.dma_start(out=xb, in_=sv[:, :, t * T:(t + 1) * T]), reads=[(sid, t)], writes=[xk], dma=True)
        if do_norm:
            P.op("act", lambda e, xb=xb: e.activation(out=sqb, in_=xb, func=AF.Square), reads=[xk], writes=["sq"])
            P.op("pe", lambda e: mm_acc(e, PS[0], [(C.ones_bf, sqb[:, k, :]) for k in range(KC)]), reads=["sq"], writes=["ps0"])
            P.op("act", lambda e: e.activation(out=rs, in_=PS[0], func=AF.Sqrt, bias=C.eps_col, scale=1.0 / D), reads=["ps0"], writes=["rs"])
            P.op("dve", lambda e: e.reciprocal(out=rs, in_=rs), reads=["rs"], writes=["rs"])
            for k in range(KC):
                P.op("dve", lambda e, k=k, xb=xb: e.scalar_tensor_tensor(out=xb[:, k, :], in0=xb[:, k, :], scalar=C.fing[:, k:k + 1], in1=rs,
                                                                       op0=ALU.mult, op1=ALU.mult),
                     reads=[xk, "rs"], writes=[xk])
        P.op("sp", lambda e, t=t, xb=xb: e.dma_start(out=dv[:, :, t * T:(t + 1) * T], in_=xb), reads=[xk], writes=[(did, t)], dma=True)
    P.barrier()


def build_program(n_phases=8):
    nc = bass.Bass("TRN2", target_bir_lowering=False)
    C = Ctx()
    C.nc = nc

    def din(name, shape):
        return nc.dram_tensor(name, list(shape), F32, kind="ExternalInput").ap()

    d_x = din("xT", (D, SEQ))
    C.d_cond = din("condT", (128, KC))
    C.d_adaw = din("ada_w", (DEPTH, D, 6 * D))
    C.d_adab = din("adab", (128, DEPTH * 48))
    C.d_ng = din("ng", (128, DEPTH, 2, KC))
    C.d_fing = din("fing", (128, KC))
    C.d_ssm_in = din("ssm_in", (2, D, D))
    C.d_ssm_glu = din("ssm_glu", (2, D, D))
    C.d_ssm_out = din("ssm_out", (2, D, D))
    C.d_ssm_pq = din("ssm_pq", (2, 128, 3, 32))
    C.d_ssm_d = din("ssm_dT", (2, 128, KC))
    C.d_ssm_row = din("ssm_row", (2, 3, 128, 4096))
    C.d_ssm_bt = din("ssm_bt", (2, 2, 128, 4096))
    C.d_ssm_ct = din("ssm_ct", (2, 2, 128, 4096))
    C.d_pool_in = din("pool_in", (2, D, D))
    C.d_pool_mix = din("pool_mix", (2, 4, 256, 256))
    C.d_pool_scale = din("pool_scaleT", (2, 128, KC))
    C.d_pool_out = din("pool_out", (2, D, D))
    C.d_ffn_w1 = din("ffn_w1", (2, D, DFF))
    C.d_ffn_w3 = din("ffn_w3", (2, D, DFF))
    C.d_ffn_w2 = din("ffn_w2", (2, DFF, D))
    C.d_router_w = din("router_wT", (2, 128, KC, NE))
    C.d_router_b = din("router_bb", (2, 128, NE))
    C.d_moe_w1 = din("moe_w1", (2, NE, D, DFE))
    C.d_moe_w3 = din("moe_w3", (2, NE, D, DFE))
    C.d_moe_w2 = din("moe_w2", (2, NE, DFE, D))
    C.d_inv0 = din("inv0", (128, 4, 512))
    d_tloc = din("tloc", (128, 512))
    d_ident = din("ident", (128, 128))
    d_out = nc.dram_tensor("outT", [D, SEQ], F32, kind="ExternalOutput").ap()
    xa = nc.dram_tensor("xa", [D, SEQ], F32, kind="Internal").ap()
    xb = nc.dram_tensor("xb", [D, SEQ], F32, kind="Internal").ap()

    with ExitStack() as st:
        P = Prog(nc)
        sb = lambda name, shape, dt=F32: st.enter_context(nc.sbuf_tensor(name, list(shape), dt))
        C.ones_bf = sb("ones_bf", (128, 128), BF16)[:]
        C.eps_col = sb("eps_col", (128, 1))[:]
        C.halfpi_col = sb("halfpi", (128, 1))[:]
        C.magp_col = sb("magp", (128, 1))[:]
        C.magn_col = sb("magn", (128, 1))[:]
        C.ident32 = sb("ident32", (128, 128))[:]
        C.tloc = sb("tloc_sb", (128, 512))[:]
        C.mod = sb("mod", (128, DEPTH * 48))[:]
        C.adab = sb("adab_sb", (128, DEPTH * 48))[:]
        C.ng = sb("ng_sb", (128, DEPTH, 2, KC))[:]
        C.gs = sb("gs_sb", (128, DEPTH, 2, KC))[:]
        C.fing = sb("fing_sb", (128, KC))[:]
        C.cndb = sb("cndb", (128, KC), BF16)[:]
        arena = sb("arena", (128, 50 * 1024 + 512))[:]
        A = Arena(arena)
        C.PS = [st.enter_context(nc.psum_tensor(f"ps{i}", [128, 512], F32))[:] for i in range(8)]

        P.op("sp", lambda e: e.dma_start(out=C.tloc, in_=d_tloc), writes=["tloc"], dma=True)
        P.op("sp", lambda e: e.dma_start(out=C.ident32, in_=d_ident), writes=["ident"], dma=True)
        P.op("dve", lambda e: e.memset(C.halfpi_col, math.pi / 2.0), writes=["halfpi"])
        P.op("dve", lambda e: e.memset(C.magp_col, MAG), writes=["magp"])
        P.op("dve", lambda e: e.memset(C.magn_col, -MAG), writes=["magn"])
        phase_prologue(P, C, A)

        seqs = []
        for li in range(DEPTH):
            seqs.append(("mix", li))
            seqs.append(("ffn", li))
        seqs = seqs[:n_phases]
        cur, cur_id = d_x, "xin"
        nxt = [(xa, "xa"), (xb, "xb")]
        for pi, (kind, li) in enumerate(seqs):
            dstap, dst_id = nxt[pi % 2]
            if kind == "mix":
                if li % 2 == 0:
                    phase_s5(P, C, A, li, cur, dstap, cur_id, dst_id)
                else:
                    phase_pool(P, C, A, li, cur, dstap, cur_id, dst_id)
            else:
                if li % 2 == 0:
                    phase_ffn(P, C, A, li, cur, dstap, cur_id, dst_id)
                else:
                    fuse = (n_phases == 8 and pi == 7)
                    phase_moe(P, C, A, li, cur, dstap, cur_id, dst_id, final_out=(d_out if fuse else None))
            cur, cur_id = dstap, dst_id
        if n_phases != 8:
            phase_final(P, C, A, cur, d_out, cur_id, "out", do_norm=False)

        emit = P.emit(st)
        with nc.Block() as block:
            @block.tensor
            def _(e):
                emit("pe", e)

            @block.vector
            def _(e):
                emit("dve", e)

            @block.scalar
            def _(e):
                emit("act", e)

            @block.gpsimd
            def _(e):
                emit("pool", e)

            @block.sync
            def _(e):
                emit("sp", e)
        C.stats = P.stats
    return nc, C


def _colT(v):
    return np.ascontiguousarray(np.asarray(v, np.float32).reshape(KC, 128).T)


def prepare_inputs(inp, cores):
    f = lambda a: np.ascontiguousarray(np.asarray(a, np.float32))
    shared = {}
    shared["ada_w"] = f(inp["ada_w"])
    shared["adab"] = np.ascontiguousarray(
        np.stack([f(inp["ada_b"][i]).reshape(48, 128).T for i in range(DEPTH)], axis=1).reshape(128, DEPTH * 48))
    ng = np.zeros((128, DEPTH, 2, KC), np.float32)
    for i in range(DEPTH):
        for s in range(2):
            ng[:, i, s, :] = _colT(inp["norm_g"][i, s])
    shared["ng"] = ng
    shared["fing"] = _colT(inp["final_g"])
    for k in ("ssm_in", "ssm_glu", "ssm_out", "pool_in", "pool_mix", "pool_out", "ffn_w1", "ffn_w3", "ffn_w2",
              "moe_w1", "moe_w3", "moe_w2"):
        shared[k] = f(inp[k])
    pq = np.zeros((2, 128, 3, 32), np.float32)
    row = np.zeros((2, 3, 128, 4096), np.float32)
    bt = np.zeros((2, 2, 128, 4096), np.float32)
    ct = np.zeros((2, 2, 128, 4096), np.float32)
    dT = np.zeros((2, 128, KC), np.float32)
    for j in range(2):
        lr = f(inp["ssm_lam_re"][j])
        lim = f(inp["ssm_lam_im"][j])
        ldt = f(inp["ssm_log_dt"][j])
        ldt_gp = np.repeat(ldt[:, None], 64, axis=1)
        for a_i, arr in enumerate((lr, lim, ldt_gp)):
            pq[j, :, a_i, :] = arr.reshape(32, 2, 64).transpose(1, 2, 0).reshape(128, 32)
            row[j, a_i] = np.broadcast_to(arr.reshape(1, 4096), (128, 4096))
        br = f(inp["ssm_b_re"][j])
        bi = f(inp["ssm_b_im"][j])
        cr = f(inp["ssm_c_re"][j])
        ci = f(inp["ssm_c_im"][j])
        btr = bt[j].reshape(2, 128, 32, 2, 64)
        ctr = ct[j].reshape(2, 128, 32, 128)
        for q in range(32):
            for g2 in range(2):
                g = 2 * q + g2
                gl = g % 8
                btr[0, gl * 16:(gl + 1) * 16, q, g2, :] = br[g].T
                btr[1, gl * 16:(gl + 1) * 16, q, g2, :] = bi[g].T
                ctr[0, g2 * 64:(g2 + 1) * 64, q, gl * 16:(gl + 1) * 16] = cr[g].T
                ctr[1, g2 * 64:(g2 + 1) * 64, q, gl * 16:(gl + 1) * 16] = ci[g].T
        dT[j] = _colT(f(inp["ssm_d"][j]).reshape(-1))
    shared["ssm_pq"] = pq
    shared["ssm_row"] = row
    shared["ssm_bt"] = bt
    shared["ssm_ct"] = ct
    shared["ssm_dT"] = dT
    shared["pool_scaleT"] = np.stack([_colT(inp["pool_scale"][j]) for j in range(2)])
    shared["router_wT"] = np.ascontiguousarray(
        np.stack([f(inp["router_w"][j]).reshape(KC, 128, NE).transpose(1, 0, 2) for j in range(2)]))
    shared["router_bb"] = np.ascontiguousarray(
        np.stack([np.broadcast_to(f(inp["router_b"][j])[None, :], (128, NE)) for j in range(2)]))
    inv0 = np.zeros((128, 4, 512), np.float32)
    tpos = np.arange(1, 513, dtype=np.float32)
    for wg, w in enumerate((2, 4, 8, 16)):
        inv0[:, wg, :] = (1.0 / np.minimum(tpos, float(w)))[None, :]
    shared["inv0"] = inv0
    shared["tloc"] = np.ascontiguousarray(np.broadcast_to(np.arange(512, dtype=np.float32)[None, :], (128, 512)))
    shared["ident"] = np.eye(128, dtype=np.float32)
    maps = []
    x = np.asarray(inp["x"], np.float32)
    c = np.asarray(inp["c"], np.float32)
    for b in cores:
        m = dict(shared)
        m["xT"] = np.ascontiguousarray(x[b].T)
        m["condT"] = _colT(c[b])
        maps.append(m)
    return maps


_CACHE = {}


def kernel(**inputs):
    if "nc" not in _CACHE:
        _CACHE["nc"] = build_program(8)[0]
    nc = _CACHE["nc"]
    maps = prepare_inputs(inputs, list(range(NB)))
    res = run_bass_kernel_spmd(nc, maps, core_ids=list(range(NB)))
    out = np.stack([np.ascontiguousarray(r["outT"].T) for r in res.results], axis=0)
    return out.astype(np.float32)
```

```python
import math
import numpy as np
from contextlib import ExitStack
import concourse.bass as bass
import concourse.mybir as mybir
from concourse.bass_utils import run_bass_kernel_spmd

F32 = mybir.dt.float32
BF16 = mybir.dt.bfloat16
ALU = mybir.AluOpType
AF = mybir.ActivationFunctionType

D = 1024
SEQ = 4096
NB = 8
KC = 8
DEPTH = 4
DFF = 2816
NFC = DFF // 128
NE = 8
DFE = 3584
EPS = 1e-6
MAG = 12582912.0
TWO_PI = 2.0 * math.pi

ENGS = ("pe", "dve", "act", "pool", "sp")
SAME_ENGINE_SYNC = True
DMA_RING = 8
SEM_CHUNK = 24000


class _Op:
    __slots__ = ("eng", "fn", "deps", "dma", "idx", "qidx", "waits")


class Prog:
    def __init__(self, nc):
        self.nc = nc
        self.ops = []
        self.last_w = {}
        self.readers = {}
        self.eng_count = {e: 0 for e in ENGS}
        self.dma_count = {e: 0 for e in ENGS}

    def op(self, eng, fn, reads=(), writes=(), dma=False):
        o = _Op()
        o.eng = eng
        o.fn = fn
        o.dma = dma
        deps = set()
        for k in reads:
            w = self.last_w.get(k)
            if w is not None:
                deps.add(w)
        for k in writes:
            w = self.last_w.get(k)
            if w is not None:
                deps.add(w)
            for r in self.readers.get(k, ()):
                deps.add(r)
        oid = len(self.ops)
        o.deps = deps
        o.idx = self.eng_count[eng]
        self.eng_count[eng] += 1
        if dma:
            o.qidx = self.dma_count[eng]
            self.dma_count[eng] += 1
        else:
            o.qidx = -1
        self.ops.append(o)
        for k in writes:
            self.last_w[k] = oid
            self.readers[k] = []
        for k in reads:
            if k in writes:
                continue
            self.readers.setdefault(k, []).append(oid)
        return oid

    def barrier(self):
        lasts = {}
        dma_recent = {}
        for i, o in enumerate(self.ops):
            if o.dma:
                dma_recent.setdefault(o.eng, []).append(i)
            else:
                lasts[o.eng] = i
        deps = set(lasts.values())
        for e, lst in dma_recent.items():
            deps.update(lst[-DMA_RING:])
        for e in ENGS:
            o = _Op()
            o.eng = e
            o.fn = None
            o.dma = False
            o.deps = set(deps)
            o.idx = self.eng_count[e]
            self.eng_count[e] += 1
            o.qidx = -1
            self.ops.append(o)
        self.last_w = {}
        self.readers = {}

    def emit(self, stack):
        nc = self.nc
        ops = self.ops
        n = len(ops)
        known = {e: {f: -1 for f in ENGS} for e in ENGS}
        known_dma = {e: set() for e in ENGS}
        vc = [None] * n
        needed = set()
        for i, o in enumerate(ops):
            E = o.eng
            kn = known[E]
            kd = known_dma[E]
            best = {}
            final = []
            for j in sorted(o.deps):
                d = ops[j]
                if d.dma:
                    if j in kd:
                        continue
                    final.append(("dma", j))
                    kd.add(j)
                    vj = vc[j]
                    for f in ENGS:
                        if vj[f] > kn[f]:
                            kn[f] = vj[f]
                else:
                    F = d.eng
                    if F == E and (not SAME_ENGINE_SYNC or E in ("pe", "sp")):
                        continue
                    if d.idx <= kn[F]:
                        continue
                    if F not in best or d.idx > ops[best[F]].idx:
                        best[F] = j
            for F, j in best.items():
                d = ops[j]
                if d.idx <= kn[F]:
                    continue
                final.append(("eng", j))
                needed.add(j)
                vj = vc[j]
                for f in ENGS:
                    if vj[f] > kn[f]:
                        kn[f] = vj[f]
                if d.idx > kn[F]:
                    kn[F] = d.idx
            o.waits = final
            v = dict(kn)
            if not o.dma:
                if (not SAME_ENGINE_SYNC or E in ("pe", "sp")) and o.idx - 1 > v[E]:
                    v[E] = o.idx - 1
                    kn[E] = o.idx - 1
                v[E] = max(v[E], o.idx)
            vc[i] = v
        nsig = {e: 0 for e in ENGS}
        sig = {}
        for i, o in enumerate(ops):
            if i in needed:
                sig[i] = nsig[o.eng]
                nsig[o.eng] += 1
        eng_sems = {}
        for e in ENGS:
            k = (nsig[e] + SEM_CHUNK - 1) // SEM_CHUNK
            eng_sems[e] = [stack.enter_context(nc.semaphore(f"s_{e}_{c}")) for c in range(k)]
        ring = {}
        for e in ENGS:
            if self.dma_count[e] > 0:
                ring[e] = [stack.enter_context(nc.semaphore(f"r_{e}_{c}")) for c in range(DMA_RING)]
        dma_ids = {e: [] for e in ENGS}
        for i, o in enumerate(ops):
            if o.dma:
                dma_ids[o.eng].append(i)
        self.stats = {e: [0, 0] for e in ENGS}

        def sem_wait(h, kind, j):
            d = ops[j]
            if kind == "dma":
                h.wait_ge(ring[d.eng][d.qidx % DMA_RING], 16 * (d.qidx // DMA_RING + 1))
            else:
                sn = sig[j]
                h.wait_ge(eng_sems[d.eng][sn // SEM_CHUNK], (sn % SEM_CHUNK) + 1)

        def emit_engine(e, h):
            for i, o in enumerate(ops):
                if o.eng != e:
                    continue
                for kind, j in o.waits:
                    sem_wait(h, kind, j)
                    self.stats[e][1] += 1
                if o.dma and o.qidx >= DMA_RING:
                    sem_wait(h, "dma", dma_ids[e][o.qidx - DMA_RING])
                if o.fn is None:
                    if i in sig:
                        sn = sig[i]
                        h.nop().then_inc(eng_sems[e][sn // SEM_CHUNK], 1)
                    continue
                ins = o.fn(h)
                self.stats[e][0] += 1
                if o.dma:
                    ins.then_inc(ring[e][o.qidx % DMA_RING], 16)
                elif i in sig:
                    sn = sig[i]
                    ins.then_inc(eng_sems[e][sn // SEM_CHUNK], 1)
            if self.dma_count[e] > 0:
                for pj in dma_ids[e][-DMA_RING:]:
                    sem_wait(h, "dma", pj)

        return emit_engine


class Arena:
    def __init__(self, ap):
        self.ap = ap
        self.n = ap.shape[1]
        self.o = 0

    def reset(self):
        self.o = 0

    def f32(self, *shape):
        n = int(np.prod(shape))
        assert self.o + n <= self.n, ("arena overflow", self.o, n, self.n)
        v = self.ap[:, self.o:self.o + n]
        self.o += n
        if len(shape) == 2:
            v = v.rearrange("p (a b) -> p a b", a=shape[0])
        elif len(shape) == 3:
            v = v.rearrange("p (a b c) -> p a b c", a=shape[0], b=shape[1])
        return v

    def bf16(self, *shape):
        n = int(np.prod(shape))
        assert n % 2 == 0
        assert self.o + n // 2 <= self.n, ("arena overflow", self.o, n, self.n)
        v = self.ap[:, self.o:self.o + n // 2].bitcast(BF16)
        self.o += n // 2
        if len(shape) == 2:
            v = v.rearrange("p (a b) -> p a b", a=shape[0])
        elif len(shape) == 3:
            v = v.rearrange("p (a b c) -> p a b c", a=shape[0], b=shape[1])
        return v


class Ctx:
    pass


def mm_acc(e, out, pairs):
    n = len(pairs)
    ins = None
    for i, (l, r) in enumerate(pairs):
        ins = e.matmul(out, lhsT=l, rhs=r, start=(i == 0), stop=(i == n - 1))
    return ins


def norm_mod(P, C, xt, xkey, h, hkey, gs, sh, bufs, tag, ncols, ps, pskey, h32=None, tmptag=None):
    sqb, tmp, rs = bufs
    if tmptag is None:
        tmptag = tag
    P.op("act", lambda e: e.activation(out=sqb, in_=xt, func=AF.Square), reads=[xkey], writes=[(tag, "sq")])
    P.op("pe", lambda e: mm_acc(e, ps[:, 0:ncols], [(C.ones_bf, sqb[:, k, :]) for k in range(KC)]),
         reads=[(tag, "sq")], writes=[pskey])
    P.op("act", lambda e: e.activation(out=rs, in_=ps[:, 0:ncols], func=AF.Sqrt, bias=C.eps_col, scale=1.0 / D),
         reads=[pskey], writes=[(tag, "rs")])
    P.op("dve", lambda e: e.reciprocal(out=rs, in_=rs), reads=[(tag, "rs")], writes=[(tag, "rs")])
    for k in range(KC):
        P.op("dve", lambda e, k=k: e.scalar_tensor_tensor(out=tmp[:, k, :], in0=xt[:, k, :], scalar=gs[:, k:k + 1],
                                                          in1=rs, op0=ALU.mult, op1=ALU.mult),
             reads=[xkey, (tag, "rs")], writes=[(tmptag, "tmp", k)])
        if h32 is not None:
            P.op("act", lambda e, k=k: e.activation(out=h32[:, k, :], in_=tmp[:, k, :], func=AF.Identity,
                                                    bias=sh[:, k:k + 1], scale=1.0),
                 reads=[(tmptag, "tmp", k)], writes=[(tmptag, "h32", k)])
            P.op("act", lambda e, k=k: e.copy(out=h[:, k, :], in_=h32[:, k, :]),
                 reads=[(tmptag, "h32", k)], writes=[(hkey, k)])
        else:
            P.op("act", lambda e, k=k: e.activation(out=h[:, k, :], in_=tmp[:, k, :], func=AF.Identity,
                                                    bias=sh[:, k:k + 1], scale=1.0),
                 reads=[(tmptag, "tmp", k)], writes=[(hkey, k)])


def frac_round(P, eng, out, in_, tmp, rkeys, wkeys, tkey):
    P.op(eng, lambda e: e.tensor_scalar(out=tmp, in0=in_, scalar1=MAG, scalar2=None, op0=ALU.add),
         reads=rkeys, writes=[tkey])
    P.op(eng, lambda e: e.tensor_scalar(out=tmp, in0=tmp, scalar1=MAG, scalar2=None, op0=ALU.subtract),
         reads=[tkey], writes=[tkey])
    P.op(eng, lambda e: e.tensor_tensor(out=out, in0=in_, in1=tmp, op=ALU.subtract),
         reads=list(rkeys) + [tkey], writes=wkeys)


def xview(ap):
    return ap.rearrange("(kc p) t -> p kc t", p=128)


def adaln_group(P, C, layer, gq, awb, key, psm):
    src = C.d_adaw[layer].rearrange("(kc p) n -> p kc n", p=128)[:, :, gq * 512:(gq + 1) * 512]
    P.op("pool", lambda e: e.dma_start(out=awb, in_=src), writes=[key], dma=True)

    def f(e):
        ins = None
        for jj in range(4):
            col = gq * 4 + jj
            for k in range(KC):
                ins = e.matmul(psm[:, col:col + 1], lhsT=awb[:, k, jj * 128:(jj + 1) * 128],
                               rhs=C.cndb[:, k:k + 1], start=(k == 0), stop=(k == KC - 1))
        return ins
    P.op("pe", f, reads=[key, "cndb"], writes=[("psm", layer)])


def adaln_finish(P, C, layer, psm):
    i = layer
    P.op("dve", lambda e: e.tensor_tensor(out=C.mod[:, i * 48:(i + 1) * 48], in0=psm[:, 0:48], in1=C.adab[:, i * 48:(i + 1) * 48], op=ALU.add),
         reads=[("psm", layer), "adab"], writes=[("mod", i)])
    for s_ in range(2):
        o = i * 48 + 24 * s_
        P.op("dve", lambda e, s_=s_, o=o: e.scalar_tensor_tensor(
            out=C.gs[:, i, s_, :], in0=C.mod[:, o + 8:o + 16], scalar=1.0, in1=C.ng[:, i, s_, :],
            op0=ALU.add, op1=ALU.mult), reads=[("mod", i), "ng"], writes=[("gs", i, s_)])


def phase_prologue(P, C, A):
    A.reset()
    cnd = A.f32(KC)
    tmpc = A.f32(KC)
    awb = [A.bf16(KC, 512) for _ in range(3)]
    P.op("sp", lambda e: e.dma_start(out=cnd, in_=C.d_cond), writes=["cnd"], dma=True)
    P.op("sp", lambda e: e.dma_start(out=C.adab, in_=C.d_adab), writes=["adab"], dma=True)
    P.op("sp", lambda e: e.dma_start(out=C.ng, in_=C.d_ng), writes=["ng"], dma=True)
    P.op("sp", lambda e: e.dma_start(out=C.fing, in_=C.d_fing), writes=["fing"], dma=True)
    P.op("dve", lambda e: e.memset(C.ones_bf, 1.0), writes=["ones"])
    P.op("dve", lambda e: e.memset(C.eps_col, EPS), writes=["eps"])
    P.op("act", lambda e: e.activation(out=tmpc, in_=cnd, func=AF.Silu), reads=["cnd"], writes=["tmpc"])
    P.op("act", lambda e: e.copy(out=C.cndb, in_=tmpc), reads=["tmpc"], writes=["cndb"])
    for gq in range(12):
        adaln_group(P, C, 0, gq, awb[gq % 3], ("awb", gq % 3), C.PS[7])
    adaln_finish(P, C, 0, C.PS[7])
    P.barrier()


def load_w_sq(P, C, dst, src, key):
    v = src.rearrange("(kc p) n -> p kc n", p=128)
    N = v.shape[2]
    step = 512
    for c0 in range(0, N, step):
        c1 = min(N, c0 + step)
        P.op("pool", lambda e, c0=c0, c1=c1: e.dma_start(out=dst[:, :, c0:c1], in_=v[:, :, c0:c1]),
             writes=[(key, c0 // step)], dma=True)
    return [(key, c // step) for c in range(0, N, step)]


def phase_s5(P, C, A, li, src, dst, sid, did):
    nc = C.nc
    j = li // 2
    T = 512
    NT = SEQ // T
    A.reset()
    w_in = A.bf16(KC, D)
    w_glu = A.bf16(KC, D)
    w_out = A.bf16(KC, D)
    BrT = A.bf16(32, 128)
    BiT = A.bf16(32, 128)
    CrT = A.bf16(32, 128)
    nCiT = A.bf16(32, 128)
    kw_in = load_w_sq(P, C, w_in, C.d_ssm_in[j], "w_in")
    kw_glu = load_w_sq(P, C, w_glu, C.d_ssm_glu[j], "w_glu")
    kw_out = load_w_sq(P, C, w_out, C.d_ssm_out[j], "w_out")
    RHO = A.f32(32)
    THF = A.f32(32)
    BASE = A.f32(32, 8)
    ST_R = A.f32(32)
    ST_I = A.f32(32)
    Dt = A.f32(KC)
    pq = A.f32(3, 32)
    t32 = [A.f32(32) for _ in range(3)]
    P.op("sp", lambda e: e.dma_start(out=pq, in_=C.d_ssm_pq[j]), writes=["pq"], dma=True)
    P.op("sp", lambda e: e.dma_start(out=Dt, in_=C.d_ssm_d[j]), writes=["Dt"], dma=True)
    P.op("dve", lambda e: e.memset(ST_R, 0.0), writes=["ST_R"])
    P.op("dve", lambda e: e.memset(ST_I, 0.0), writes=["ST_I"])
    P.op("act", lambda e: e.activation(out=t32[0], in_=pq[:, 2, :], func=AF.Exp), reads=["pq"], writes=["t32_0"])
    P.op("dve", lambda e: e.tensor_tensor(out=t32[1], in0=pq[:, 0, :], in1=t32[0], op=ALU.mult),
         reads=["pq", "t32_0"], writes=["t32_1"])
    P.op("act", lambda e: e.activation(out=RHO, in_=t32[1], func=AF.Exp), reads=["t32_1"], writes=["RHO"])
    P.op("dve", lambda e: e.tensor_tensor(out=t32[1], in0=pq[:, 1, :], in1=t32[0], op=ALU.mult),
         reads=["pq", "t32_0", "RHO"], writes=["t32_1"])
    P.op("dve", lambda e: e.tensor_scalar(out=t32[1], in0=t32[1], scalar1=1.0 / TWO_PI, scalar2=None, op0=ALU.mult),
         reads=["t32_1"], writes=["t32_1"])
    frac_round(P, "dve", THF, t32[1], t32[2], ["t32_1"], ["THF"], "t32_2")
    P.op("dve", lambda e: e.tensor_scalar(out=t32[0], in0=THF, scalar1=512.0, scalar2=None, op0=ALU.mult),
         reads=["THF"], writes=["t32_0"])
    frac_round(P, "dve", t32[1], t32[0], t32[2], ["t32_0"], ["t32_1"], "t32_2")
    for tt in range(NT):
        P.op("dve", lambda e, tt=tt: e.tensor_scalar(out=t32[0], in0=t32[1], scalar1=float(tt), scalar2=None, op0=ALU.mult),
             reads=["t32_1"], writes=["t32_0"])
        frac_round(P, "dve", BASE[:, :, tt], t32[0], t32[2], ["t32_0"], [("BASE", tt)], "t32_2")
    CH = 1024
    mark = A.o
    rowp = [A.f32(CH) for _ in range(3)]
    bt = [A.f32(CH) for _ in range(2)]
    ct = [A.f32(CH) for _ in range(2)]
    w = [A.f32(CH) for _ in range(10)]
    NQC = CH // 128
    for cc in range(4096 // CH):
        sl = slice(cc * CH, (cc + 1) * CH)
        rk = ("rowp", cc)
        P.op("sp", lambda e, sl=sl: e.dma_start(out=rowp[0], in_=C.d_ssm_row[j][0][:, sl]), writes=["rowp0"], dma=True)
        P.op("sp", lambda e, sl=sl: e.dma_start(out=rowp[1], in_=C.d_ssm_row[j][1][:, sl]), writes=["rowp1"], dma=True)
        P.op("sp", lambda e, sl=sl: e.dma_start(out=rowp[2], in_=C.d_ssm_row[j][2][:, sl]), writes=["rowp2"], dma=True)
        P.op("sp", lambda e, sl=sl: e.dma_start(out=bt[0], in_=C.d_ssm_bt[j][0][:, sl]), writes=["bt0"], dma=True)
        P.op("sp", lambda e, sl=sl: e.dma_start(out=bt[1], in_=C.d_ssm_bt[j][1][:, sl]), writes=["bt1"], dma=True)
        P.op("sp", lambda e, sl=sl: e.dma_start(out=ct[0], in_=C.d_ssm_ct[j][0][:, sl]), writes=["ct0"], dma=True)
        P.op("sp", lambda e, sl=sl: e.dma_start(out=ct[1], in_=C.d_ssm_ct[j][1][:, sl]), writes=["ct1"], dma=True)
        lr, lim, ldt = rowp
        P.op("act", lambda e: e.activation(out=w[0], in_=ldt, func=AF.Exp), reads=["rowp2"], writes=["w0"])
        P.op("dve", lambda e: e.tensor_tensor(out=w[1], in0=lr, in1=w[0], op=ALU.mult), reads=["rowp0", "w0"], writes=["w1"])
        P.op("act", lambda e: e.activation(out=w[2], in_=w[1], func=AF.Exp), reads=["w1"], writes=["w2"])
        P.op("dve", lambda e: e.tensor_tensor(out=w[3], in0=lim, in1=w[0], op=ALU.mult), reads=["rowp1", "w0"], writes=["w3"])
        P.op("dve", lambda e: e.tensor_scalar(out=w[3], in0=w[3], scalar1=1.0 / TWO_PI, scalar2=None, op0=ALU.mult),
             reads=["w3"], writes=["w3"])
        frac_round(P, "dve", w[4], w[3], w[5], ["w3"], ["w4"], "w5")
        P.op("act", lambda e: e.activation(out=w[5], in_=w[4], func=AF.Sin, scale=TWO_PI), reads=["w4"], writes=["w5"])
        P.op("act", lambda e: e.activation(out=w[6], in_=w[4], func=AF.Abs), reads=["w4"], writes=["w6"])
        P.op("act", lambda e: e.activation(out=w[6], in_=w[6], func=AF.Sin, scale=-TWO_PI, bias=C.halfpi_col),
             reads=["w6"], writes=["w6"])
        P.op("dve", lambda e: e.tensor_tensor(out=w[6], in0=w[6], in1=w[2], op=ALU.mult), reads=["w6", "w2"], writes=["w6"])
        P.op("dve", lambda e: e.tensor_tensor(out=w[5], in0=w[5], in1=w[2], op=ALU.mult), reads=["w5", "w2"], writes=["w5"])
        P.op("dve", lambda e: e.tensor_scalar(out=w[6], in0=w[6], scalar1=-1.0, scalar2=None, op0=ALU.add), reads=["w6"], writes=["w6"])
        P.op("dve", lambda e: e.tensor_tensor(out=w[7], in0=lr, in1=lr, op=ALU.mult), reads=["rowp0"], writes=["w7"])
        P.op("dve", lambda e: e.tensor_tensor(out=w[8], in0=lim, in1=lim, op=ALU.mult), reads=["rowp1"], writes=["w8"])
        P.op("dve", lambda e: e.tensor_tensor(out=w[7], in0=w[7], in1=w[8], op=ALU.add), reads=["w7", "w8"], writes=["w7"])
        P.op("dve", lambda e: e.reciprocal(out=w[7], in_=w[7]), reads=["w7"], writes=["w7"])
        P.op("dve", lambda e: e.tensor_tensor(out=w[8], in0=w[6], in1=lr, op=ALU.mult), reads=["w6", "rowp0"], writes=["w8"])
        P.op("dve", lambda e: e.tensor_tensor(out=w[9], in0=w[5], in1=lim, op=ALU.mult), reads=["w5", "rowp1"], writes=["w9"])
        P.op("dve", lambda e: e.tensor_tensor(out=w[8], in0=w[8], in1=w[9], op=ALU.add), reads=["w8", "w9"], writes=["w8"])
        P.op("dve", lambda e: e.tensor_tensor(out=w[8], in0=w[8], in1=w[7], op=ALU.mult), reads=["w8", "w7"], writes=["w8"])
        P.op("dve", lambda e: e.tensor_tensor(out=w[9], in0=w[5], in1=lr, op=ALU.mult), reads=["w5", "rowp0"], writes=["w9"])
        P.op("dve", lambda e: e.tensor_tensor(out=w[0], in0=w[6], in1=lim, op=ALU.mult), reads=["w6", "rowp1"], writes=["w0"])
        P.op("dve", lambda e: e.tensor_tensor(out=w[9], in0=w[9], in1=w[0], op=ALU.subtract), reads=["w9", "w0"], writes=["w9"])
        P.op("dve", lambda e: e.tensor_tensor(out=w[9], in0=w[9], in1=w[7], op=ALU.mult), reads=["w9", "w7"], writes=["w9"])
        q0 = cc * NQC
        brv = BrT[:, q0:q0 + NQC, :].rearrange("p a b -> p (a b)")
        biv = BiT[:, q0:q0 + NQC, :].rearrange("p a b -> p (a b)")
        crv = CrT[:, q0:q0 + NQC, :].rearrange("p a b -> p (a b)")
        civ = nCiT[:, q0:q0 + NQC, :].rearrange("p a b -> p (a b)")
        P.op("dve", lambda e: e.tensor_tensor(out=w[1], in0=w[8], in1=bt[0], op=ALU.mult), reads=["w8", "bt0"], writes=["w1"])
        P.op("dve", lambda e: e.tensor_tensor(out=w[2], in0=w[9], in1=bt[1], op=ALU.mult), reads=["w9", "bt1"], writes=["w2"])
        P.op("dve", lambda e, brv=brv: e.tensor_tensor(out=brv, in0=w[1], in1=w[2], op=ALU.subtract),
             reads=["w1", "w2"], writes=[("BrT", cc)])
        P.op("dve", lambda e: e.tensor_tensor(out=w[1], in0=w[8], in1=bt[1], op=ALU.mult), reads=["w8", "bt1", ("BrT", cc)], writes=["w1"])
        P.op("dve", lambda e: e.tensor_tensor(out=w[2], in0=w[9], in1=bt[0], op=ALU.mult), reads=["w9", "bt0", ("BrT", cc)], writes=["w2"])
        P.op("dve", lambda e, biv=biv: e.tensor_tensor(out=biv, in0=w[1], in1=w[2], op=ALU.add),
             reads=["w1", "w2"], writes=[("BiT", cc)])
        P.op("act", lambda e, crv=crv: e.copy(out=crv, in_=ct[0]), reads=["ct0"], writes=[("CrT", cc)])
        P.op("act", lambda e, civ=civ: e.mul(out=civ, in_=ct[1], mul=-1.0), reads=["ct1"], writes=[("nCiT", cc)])
    P.barrier()
    A.o = mark
    xt = A.f32(KC, T)
    tmp = A.f32(KC, T)
    sqb = A.bf16(KC, T)
    rs = A.f32(T)
    h = A.bf16(KC, T)
    u = A.bf16(KC, T)
    zb = A.bf16(KC, T)
    z2b = A.bf16(KC, T)
    y32 = tmp
    NPB = 2
    G = [A.f32(T) for _ in range(NPB)]
    FS = [A.f32(T) for _ in range(NPB)]
    COS = [A.f32(T) for _ in range(NPB)]
    SIN = [A.f32(T) for _ in range(NPB)]
    wr = [A.f32(T) for _ in range(NPB)]
    wi = [A.f32(T) for _ in range(NPB)]
    xr = [A.bf16(T) for _ in range(NPB)]
    xi = [A.bf16(T) for _ in range(NPB)]
    RAB = A.f32(T)
    t1 = A.f32(T)
    t2 = A.f32(T)
    cr = A.f32(T)
    ci = A.f32(T)
    p1 = A.f32(T)
    p2 = A.f32(T)
    sg = A.f32(T)
    PS = C.PS
    gs = C.gs[:, li, 0, :]
    sh = C.mod[:, li * 48 + 0: li * 48 + 8]
    g1 = C.mod[:, li * 48 + 16: li * 48 + 24]
    sv = xview(src)
    dv = xview(dst)
    NQ = 32

    def stage_a(t, q):
        b = q % NPB
        P.op("act", lambda e: e.activation(out=G[b], in_=C.tloc, func=AF.Identity, scale=THF[:, q:q + 1], bias=BASE[:, q, t:t + 1]),
             reads=[], writes=[("G", b)])
        P.op("act", lambda e: e.activation(out=RAB, in_=G[b], func=AF.Identity, bias=C.magp_col, scale=1.0),
             reads=[("G", b)], writes=[("RAB",)])
        P.op("act", lambda e: e.activation(out=RAB, in_=RAB, func=AF.Identity, bias=C.magn_col, scale=1.0),
             reads=[("RAB",)], writes=[("RAB",)])
        P.op("dve", lambda e: e.tensor_tensor(out=FS[b], in0=G[b], in1=RAB, op=ALU.subtract),
             reads=[("G", b), ("RAB",)], writes=[("FS", b)])
        P.op("act", lambda e: e.activation(out=SIN[b], in_=FS[b], func=AF.Sin, scale=TWO_PI), reads=[("FS", b)], writes=[("SIN", b)])
        P.op("act", lambda e: e.activation(out=COS[b], in_=FS[b], func=AF.Abs), reads=[("FS", b)], writes=[("COS", b)])
        P.op("act", lambda e: e.activation(out=COS[b], in_=COS[b], func=AF.Sin, scale=-TWO_PI, bias=C.halfpi_col),
             reads=[("COS", b)], writes=[("COS", b)])

    def stage_bproj(q):
        b = q % NPB
        dc = q // 4
        pr = 4 + 2 * b
        pi_ = pr + 1
        P.op("pe", lambda e: e.matmul(PS[pr], lhsT=BrT[:, q, :], rhs=u[:, dc, :], start=True, stop=True),
             reads=[("u", dc)], writes=[("ps", pr)])
        P.op("pe", lambda e: e.matmul(PS[pi_], lhsT=BiT[:, q, :], rhs=u[:, dc, :], start=True, stop=True),
             reads=[("u", dc)], writes=[("ps", pi_)])

    def stage_scan(q):
        b = q % NPB
        pr = 4 + 2 * b
        pi_ = pr + 1
        P.op("dve", lambda e: e.tensor_tensor(out=t1, in0=PS[pr], in1=COS[b], op=ALU.mult), reads=[("ps", pr), ("COS", b)], writes=[("t1",)])
        P.op("dve", lambda e: e.tensor_tensor(out=t2, in0=PS[pi_], in1=SIN[b], op=ALU.mult), reads=[("ps", pi_), ("SIN", b)], writes=[("t2",)])
        P.op("dve", lambda e: e.tensor_tensor(out=cr, in0=t1, in1=t2, op=ALU.add), reads=[("t1",), ("t2",)], writes=[("cr",)])
        P.op("dve", lambda e: e.tensor_tensor(out=t1, in0=PS[pi_], in1=COS[b], op=ALU.mult), reads=[("ps", pi_), ("COS", b), ("cr",)], writes=[("t1",)])
        P.op("dve", lambda e: e.tensor_tensor(out=t2, in0=PS[pr], in1=SIN[b], op=ALU.mult), reads=[("ps", pr), ("SIN", b), ("cr",)], writes=[("t2",)])
        P.op("dve", lambda e: e.tensor_tensor(out=ci, in0=t1, in1=t2, op=ALU.subtract), reads=[("t1",), ("t2",)], writes=[("ci",)])
        rho_b = RHO[:, q:q + 1].to_broadcast([128, T])
        P.op("dve", lambda e: e.tensor_tensor_scan(out=wr[b], data0=rho_b, data1=cr, initial=ST_R[:, q:q + 1], op0=ALU.mult, op1=ALU.add),
             reads=[("cr",), ("ST_R", q)], writes=[("wr", b)])
        P.op("dve", lambda e: e.tensor_tensor_scan(out=wi[b], data0=rho_b, data1=ci, initial=ST_I[:, q:q + 1], op0=ALU.mult, op1=ALU.add),
             reads=[("ci",), ("ST_I", q)], writes=[("wi", b)])

    def stage_rot(q):
        b = q % NPB
        P.op("pool", lambda e: e.tensor_copy(out=ST_R[:, q:q + 1], in_=wr[b][:, T - 1:T]), reads=[("wr", b)], writes=[("ST_R", q)])
        P.op("pool", lambda e: e.tensor_copy(out=ST_I[:, q:q + 1], in_=wi[b][:, T - 1:T]), reads=[("wi", b)], writes=[("ST_I", q)])
        P.op("pool", lambda e: e.tensor_tensor(out=p1, in0=wr[b], in1=COS[b], op=ALU.mult), reads=[("wr", b), ("COS", b)], writes=[("p1",)])
        P.op("pool", lambda e: e.tensor_tensor(out=p2, in0=wi[b], in1=SIN[b], op=ALU.mult), reads=[("wi", b), ("SIN", b)], writes=[("p2",)])
        P.op("pool", lambda e: e.tensor_tensor(out=xr[b], in0=p1, in1=p2, op=ALU.subtract), reads=[("p1",), ("p2",)], writes=[("xr", b)])
        P.op("pool", lambda e: e.tensor_tensor(out=p1, in0=wr[b], in1=SIN[b], op=ALU.mult), reads=[("wr", b), ("SIN", b), ("xr", b)], writes=[("p1",)])
        P.op("pool", lambda e: e.tensor_tensor(out=p2, in0=wi[b], in1=COS[b], op=ALU.mult), reads=[("wi", b), ("COS", b), ("xr", b)], writes=[("p2",)])
        P.op("pool", lambda e: e.tensor_tensor(out=xi[b], in0=p1, in1=p2, op=ALU.add), reads=[("p1",), ("p2",)], writes=[("xi", b)])

    def stage_cproj(q):
        b = q % NPB
        dc = q // 4
        qq = q % 4
        P.op("pe", lambda e: e.matmul(PS[3], lhsT=CrT[:, q, :], rhs=xr[b], start=(qq == 0), stop=False),
             reads=[("xr", b)], writes=[("ps", 3)])
        P.op("pe", lambda e: e.matmul(PS[3], lhsT=nCiT[:, q, :], rhs=xi[b], start=False, stop=(qq == 3)),
             reads=[("xi", b)], writes=[("ps", 3)])
        if qq == 3:
            P.op("dve", lambda e: e.scalar_tensor_tensor(out=y32[:, dc, :], in0=u[:, dc, :], scalar=Dt[:, dc:dc + 1], in1=PS[3],
                                                         op0=ALU.mult, op1=ALU.add),
                 reads=[("ps", 3), ("u", dc)], writes=[("y32", dc)])
            P.op("act", lambda e: e.activation(out=zb[:, dc, :], in_=y32[:, dc, :], func=AF.Gelu_apprx_tanh),
                 reads=[("y32", dc)], writes=[("zb", dc)])

    def head(t):
        P.op("sp", lambda e: e.dma_start(out=xt, in_=sv[:, :, t * T:(t + 1) * T]), reads=[(sid, t)], writes=["xt"], dma=True)
        stage_a(t, 0)
        norm_mod(P, C, xt, "xt", h, "h", gs, sh, (sqb, tmp, rs), "nm", T, PS[0], "ps0")
        for m in range(KC):
            pb = 1 + (m % 2)
            P.op("pe", lambda e, m=m, pb=pb: mm_acc(e, PS[pb], [(w_in[:, k, m * 128:(m + 1) * 128], h[:, k, :]) for k in range(KC)]),
                 reads=[("h", k) for k in range(KC)], writes=[("ps", pb)])
            P.op("act", lambda e, m=m, pb=pb: e.copy(out=u[:, m, :], in_=PS[pb]), reads=[("ps", pb)], writes=[("u", m)])

    def glu_piece(m):
        pb = 1 + (m % 2)
        P.op("pe", lambda e: mm_acc(e, PS[pb], [(w_glu[:, k, m * 128:(m + 1) * 128], zb[:, k, :]) for k in range(KC)]),
             reads=[("zb", k) for k in range(KC)], writes=[("ps", pb)])
        P.op("act", lambda e: e.activation(out=sg, in_=PS[pb], func=AF.Sigmoid), reads=[("ps", pb)], writes=["sg"])
        P.op("dve", lambda e: e.tensor_tensor(out=z2b[:, m, :], in0=zb[:, m, :], in1=sg, op=ALU.mult),
             reads=["sg", ("zb", m)], writes=[("z2b", m)])

    def out_piece(m):
        pb = 1 + (m % 2)
        P.op("pe", lambda e: mm_acc(e, PS[pb], [(w_out[:, k, m * 128:(m + 1) * 128], z2b[:, k, :]) for k in range(KC)]),
             reads=[("z2b", k) for k in range(KC)], writes=[("ps", pb)])
        P.op("dve", lambda e: e.scalar_tensor_tensor(out=xt[:, m, :], in0=PS[pb], scalar=g1[:, m:m + 1], in1=xt[:, m, :],
                                                     op0=ALU.mult, op1=ALU.add),
             reads=[("ps", pb), "xt"], writes=["xt"])

    def tail_piece(t, i, reload):
        if i == 0 and reload:
            P.op("sp", lambda e: e.dma_start(out=xt, in_=sv[:, :, t * T:(t + 1) * T]), reads=[(sid, t)], writes=["xt"], dma=True)
        if i < 3:
            for m in range(3 * i, min(KC, 3 * i + 3)):
                glu_piece(m)
        elif i < 11:
            out_piece(i - 3)
            if i == 10:
                P.op("sp", lambda e: e.dma_start(out=dv[:, :, t * T:(t + 1) * T], in_=xt), reads=["xt"], writes=[(did, t)], dma=True)

    def core(t, extra=None):
        stage_bproj(0)
        for q in range(NQ):
            if q + 1 < NQ:
                stage_a(t, q + 1)
                stage_bproj(q + 1)
            stage_scan(q)
            stage_rot(q)
            if q >= 1:
                stage_cproj(q - 1)
            if extra is not None and q < 11:
                extra(q)
        stage_cproj(NQ - 1)

    head(0)
    core(0)
    for t in range(1, NT):
        head(t)
        core(t, extra=lambda i, tt=t - 1: tail_piece(tt, i, True))
    for i in range(11):
        tail_piece(NT - 1, i, True)
    P.barrier()


def phase_ffn(P, C, A, li, src, dst, sid, did):
    j = li // 2
    T = 256
    NT = SEQ // T
    A.reset()
    w1 = A.bf16(KC, DFF)
    w3 = A.bf16(KC, DFF)
    w2 = A.bf16(NFC, D)
    load_w_sq(P, C, w1, C.d_ffn_w1[j], "w1")
    load_w_sq(P, C, w3, C.d_ffn_w3[j], "w3")
    v2 = C.d_ffn_w2[j].rearrange("(fc p) n -> p fc n", p=128)
    for f0 in range(0, NFC, 4):
        f1 = min(NFC, f0 + 4)
        P.op("pool", lambda e, f0=f0, f1=f1: e.dma_start(out=w2[:, f0:f1, :], in_=v2[:, f0:f1, :]), writes=[("w2", f0)], dma=True)
    P.barrier()
    xt = [A.f32(KC, T) for _ in range(2)]
    sqb = [A.bf16(KC, T) for _ in range(2)]
    rs = [A.f32(T) for _ in range(2)]
    h = [A.bf16(KC, T) for _ in range(2)]
    tmp = A.f32(KC, T)
    a = A.bf16(NFC, T)
    sg = [A.f32(T) for _ in range(2)]
    awb = A.bf16(KC, 512)
    PS = C.PS
    gs = C.gs[:, li, 1, :]
    sh = C.mod[:, li * 48 + 24: li * 48 + 32]
    g2 = C.mod[:, li * 48 + 40: li * 48 + 48]
    sv = xview(src)
    dv = xview(dst)
    nxt = li + 1
    for t in range(NT):
        bb = t % 2
        xk = ("xt", bb)
        hk = ("h", bb)
        P.op("sp", lambda e, t=t, bb=bb: e.dma_start(out=xt[bb], in_=sv[:, :, t * T:(t + 1) * T]), reads=[(sid, t)], writes=[xk], dma=True)
        norm_mod(P, C, xt[bb], xk, h[bb], hk, gs, sh, (sqb[bb], tmp, rs[bb]), ("nm", bb), T, PS[0], "ps0", tmptag="nmT")
        if nxt < DEPTH and t < 12:
            adaln_group(P, C, nxt, t, awb, "awb", PS[7])
            if t == 11:
                adaln_finish(P, C, nxt, PS[7])
        for fc in range(NFC):
            pg = 1 + 2 * (fc % 2)
            pu = pg + 1
            b = fc % 2
            P.op("pe", lambda e, fc=fc, pg=pg, bb=bb: mm_acc(e, PS[pg][:, 0:T], [(w1[:, k, fc * 128:(fc + 1) * 128], h[bb][:, k, :]) for k in range(KC)]),
                 reads=[(hk, k) for k in range(KC)], writes=[("ps", pg)])
            P.op("pe", lambda e, fc=fc, pu=pu, bb=bb: mm_acc(e, PS[pu][:, 0:T], [(w3[:, k, fc * 128:(fc + 1) * 128], h[bb][:, k, :]) for k in range(KC)]),
                 reads=[(hk, k) for k in range(KC)], writes=[("ps", pu)])
            P.op("act", lambda e, pg=pg, b=b: e.activation(out=sg[b], in_=PS[pg][:, 0:T], func=AF.Silu), reads=[("ps", pg)], writes=[("sg", b)])
            P.op("dve", lambda e, fc=fc, pu=pu, b=b: e.tensor_tensor(out=a[:, fc, :], in0=PS[pu][:, 0:T], in1=sg[b], op=ALU.mult),
                 reads=[("ps", pu), ("sg", b)], writes=[("a", fc)])
        for m in range(KC):
            pb = 5 + (m % 2)
            P.op("pe", lambda e, m=m, pb=pb: mm_acc(e, PS[pb][:, 0:T], [(w2[:, fc, m * 128:(m + 1) * 128], a[:, fc, :]) for fc in range(NFC)]),
                 reads=[("a", fc) for fc in range(NFC)], writes=[("ps", pb)])
            P.op("dve", lambda e, m=m, pb=pb, bb=bb: e.scalar_tensor_tensor(out=xt[bb][:, m, :], in0=PS[pb][:, 0:T], scalar=g2[:, m:m + 1], in1=xt[bb][:, m, :],
                                                                          op0=ALU.mult, op1=ALU.add),
                 reads=[("ps", pb), xk], writes=[xk])
        P.op("sp", lambda e, t=t, bb=bb: e.dma_start(out=dv[:, :, t * T:(t + 1) * T], in_=xt[bb]), reads=[xk], writes=[(did, t)], dma=True)
    P.barrier()


def phase_pool(P, C, A, li, src, dst, sid, did):
    j = li // 2
    T = 512
    NT = SEQ // T
    HALO = 16
    A.reset()
    w_in = A.bf16(KC, D)
    w_out = A.bf16(KC, D)
    wmix = A.bf16(4, 2, 256)
    psc = A.f32(KC)
    load_w_sq(P, C, w_in, C.d_pool_in[j], "w_in")
    load_w_sq(P, C, w_out, C.d_pool_out[j], "w_out")
    for wg in range(4):
        P.op("pool", lambda e, wg=wg: e.dma_start(out=wmix[:, wg, :, :], in_=C.d_pool_mix[j][wg].rearrange("(kc p) n -> p kc n", p=128)),
             writes=[("wmix", wg)], dma=True)
    P.op("sp", lambda e: e.dma_start(out=psc, in_=C.d_pool_scale[j]), writes=["psc"], dma=True)
    P.barrier()
    xt = [A.f32(KC, T) for _ in range(2)]
    sqb = [A.bf16(KC, T) for _ in range(2)]
    rs = [A.f32(T) for _ in range(2)]
    h = [A.bf16(KC, T) for _ in range(2)]
    dm = [A.bf16(KC, T) for _ in range(2)]
    tmp = A.f32(KC, T)
    U = A.f32(KC, HALO + T)
    SA = A.f32(HALO + T)
    SB = A.f32(HALO + T)
    zb = A.bf16(KC, T)
    inv0 = A.f32(4, T)
    awb = A.bf16(KC, 512)
    P.op("sp", lambda e: e.dma_start(out=inv0, in_=C.d_inv0), writes=["inv0"], dma=True)
    P.op("dve", lambda e: e.memset(U, 0.0), writes=[("U", k) for k in range(KC)])
    PS = C.PS
    gs = C.gs[:, li, 0, :]
    sh = C.mod[:, li * 48 + 0: li * 48 + 8]
    g1 = C.mod[:, li * 48 + 16: li * 48 + 24]
    sv = xview(src)
    dv = xview(dst)
    W = (2, 4, 8, 16)
    nxt = li + 1
    for t in range(NT):
        bb = t % 2
        xk = ("xt", bb)
        hk = ("h", bb)
        P.op("sp", lambda e, t=t, bb=bb: e.dma_start(out=xt[bb], in_=sv[:, :, t * T:(t + 1) * T]), reads=[(sid, t)], writes=[xk], dma=True)
        norm_mod(P, C, xt[bb], xk, h[bb], hk, gs, sh, (sqb[bb], tmp, rs[bb]), ("nm", bb), T, PS[0], "ps0", tmptag="nmT")
        if nxt < DEPTH and t < 6:
            for gq in (2 * t, 2 * t + 1):
                adaln_group(P, C, nxt, gq, awb, "awb", PS[7])
            if t == 5:
                adaln_finish(P, C, nxt, PS[7])
        for m in range(KC):
            pb = 1 + (m % 2)
            wgi = m // 2
            wsz = W[wgi]
            P.op("pe", lambda e, m=m, pb=pb, bb=bb: mm_acc(e, PS[pb], [(w_in[:, k, m * 128:(m + 1) * 128], h[bb][:, k, :]) for k in range(KC)]),
                 reads=[(hk, k) for k in range(KC)], writes=[("ps", pb)])
            if t > 0:
                P.op("act", lambda e, m=m: e.copy(out=U[:, m, 0:HALO], in_=U[:, m, T:T + HALO]), reads=[("U", m)], writes=[("U", m)])
            P.op("act", lambda e, m=m, pb=pb: e.copy(out=U[:, m, HALO:HALO + T], in_=PS[pb]), reads=[("ps", pb)], writes=[("U", m)])
            cur = U[:, m, :]
            curk = ("U", m)
            sh_ = 1
            bufs = [SA, SB]
            bi = 0
            while sh_ < wsz:
                nb = bufs[bi]
                nk = ("S", bi)
                P.op("dve", lambda e, cur=cur, nb=nb, sh_=sh_: e.tensor_tensor(out=nb[:, sh_:HALO + T], in0=cur[:, sh_:HALO + T],
                                                                               in1=cur[:, 0:HALO + T - sh_], op=ALU.add),
                     reads=[curk], writes=[nk])
                cur = nb
                curk = nk
                bi ^= 1
                sh_ *= 2
            dmk = ("dm", bb, m)
            if t == 0:
                P.op("dve", lambda e, cur=cur, wgi=wgi: e.tensor_tensor(out=cur[:, HALO:HALO + T], in0=cur[:, HALO:HALO + T],
                                                                          in1=inv0[:, wgi, :], op=ALU.mult),
                     reads=[curk, "inv0"], writes=[curk])
                P.op("dve", lambda e, cur=cur, m=m, bb=bb: e.tensor_tensor(out=dm[bb][:, m, :], in0=cur[:, HALO:HALO + T],
                                                                             in1=U[:, m, HALO:HALO + T], op=ALU.subtract),
                     reads=[curk, ("U", m)], writes=[dmk])
            else:
                P.op("dve", lambda e, cur=cur, m=m, wsz=wsz, bb=bb: e.scalar_tensor_tensor(out=dm[bb][:, m, :], in0=cur[:, HALO:HALO + T],
                                                                                             scalar=1.0 / wsz, in1=U[:, m, HALO:HALO + T],
                                                                                             op0=ALU.mult, op1=ALU.subtract),
                     reads=[curk, ("U", m)], writes=[dmk])
        for wgi in range(4):
            for mo in range(2):
                m = 2 * wgi + mo
                pb = 3 + (m % 2)
                P.op("pe", lambda e, wgi=wgi, mo=mo, pb=pb, bb=bb: mm_acc(e, PS[pb], [(wmix[:, wgi, ki, mo * 128:(mo + 1) * 128], dm[bb][:, 2 * wgi + ki, :])
                                                                                    for ki in range(2)]),
                     reads=[("dm", bb, 2 * wgi), ("dm", bb, 2 * wgi + 1)], writes=[("ps", pb)])
                P.op("act", lambda e, m=m, pb=pb: e.activation(out=zb[:, m, :], in_=PS[pb], func=AF.Identity, scale=psc[:, m:m + 1]),
                     reads=[("ps", pb)], writes=[("zb", m)])
        for m in range(KC):
            pb = 5 + (m % 2)
            P.op("pe", lambda e, m=m, pb=pb: mm_acc(e, PS[pb], [(w_out[:, k, m * 128:(m + 1) * 128], zb[:, k, :]) for k in range(KC)]),
                 reads=[("zb", k) for k in range(KC)], writes=[("ps", pb)])
            P.op("dve", lambda e, m=m, pb=pb, bb=bb: e.scalar_tensor_tensor(out=xt[bb][:, m, :], in0=PS[pb], scalar=g1[:, m:m + 1], in1=xt[bb][:, m, :],
                                                                          op0=ALU.mult, op1=ALU.add),
                 reads=[("ps", pb), xk], writes=[xk])
        P.op("sp", lambda e, t=t, bb=bb: e.dma_start(out=dv[:, :, t * T:(t + 1) * T], in_=xt[bb]), reads=[xk], writes=[(did, t)], dma=True)
    P.barrier()


def phase_moe(P, C, A, li, src, dst, sid, did, final_out=None):
    j = li // 2
    T = 512
    HT = 4
    HN = HT * T
    PS = C.PS
    gs = C.gs[:, li, 1, :]
    sh = C.mod[:, li * 48 + 24: li * 48 + 32]
    g2 = C.mod[:, li * 48 + 40: li * 48 + 48]
    sv = xview(src)
    dv = xview(dst)
    FG = 512
    NFG = DFE // FG
    for half in range(2):
        A.reset()
        hH = A.bf16(KC, HN)
        GW = A.f32(16, NE)
        rw = A.f32(KC, NE)
        rbb = A.f32(NE)
        yacc = A.f32(KC, HN)
        mark = A.o
        xt = yacc[:, 0:2, :].rearrange("p a (b c) -> p (a b) c", b=4)
        tmp = yacc[:, 2:4, :].rearrange("p a (b c) -> p (a b) c", b=4)
        h32 = yacc[:, 4:6, :].rearrange("p a (b c) -> p (a b) c", b=4)
        sqb = A.bf16(KC, T)
        rs = A.f32(T)
        L = A.f32(NE)
        L2 = A.f32(NE)
        eq1 = A.f32(NE)
        eq2 = A.f32(NE)
        sm = A.f32(8)
        P.op("sp", lambda e: e.dma_start(out=rw, in_=C.d_router_w[j]), writes=["rw"], dma=True)
        P.op("sp", lambda e: e.dma_start(out=rbb, in_=C.d_router_b[j]), writes=["rbb"], dma=True)
        for tt in range(HT):
            t = half * HT + tt
            hv = hH[:, :, tt * T:(tt + 1) * T]
            P.op("sp", lambda e, t=t: e.dma_start(out=xt, in_=sv[:, :, t * T:(t + 1) * T]), reads=[(sid, t)], writes=["xt"], dma=True)
            norm_mod(P, C, xt, "xt", hv, ("hH", tt), gs, sh, (sqb, tmp, rs), "nm", T, PS[0], "ps0", h32=h32)
            for sub in range(4):
                si = tt * 4 + sub
                P.op("pe", lambda e, sub=sub: mm_acc(e, PS[1][:, 0:NE], [(h32[:, k, sub * 128:(sub + 1) * 128], rw[:, k, :]) for k in range(KC)]),
                     reads=[("nm", "h32", k) for k in range(KC)] + ["rw"], writes=[("ps", 1)])
                P.op("dve", lambda e: e.tensor_tensor(out=L, in0=PS[1][:, 0:NE], in1=rbb, op=ALU.add), reads=[("ps", 1), "rbb"], writes=["L"])
                P.op("dve", lambda e: e.reduce_max(out=sm[:, 0:1], in_=L, axis=mybir.AxisListType.X), reads=["L"], writes=["m1"])
                P.op("dve", lambda e: e.tensor_scalar(out=eq1, in0=L, scalar1=sm[:, 0:1], scalar2=None, op0=ALU.is_equal),
                     reads=["L", "m1"], writes=["eq1"])
                P.op("dve", lambda e: e.scalar_tensor_tensor(out=L2, in0=eq1, scalar=-1e30, in1=L, op0=ALU.mult, op1=ALU.add),
                     reads=["eq1", "L"], writes=["L2"])
                P.op("dve", lambda e: e.reduce_max(out=sm[:, 1:2], in_=L2, axis=mybir.AxisListType.X), reads=["L2"], writes=["m2"])
                P.op("dve", lambda e: e.tensor_scalar(out=eq2, in0=L2, scalar1=sm[:, 1:2], scalar2=None, op0=ALU.is_equal),
                     reads=["L2", "m2"], writes=["eq2"])
                P.op("dve", lambda e: e.tensor_tensor(out=sm[:, 2:3], in0=sm[:, 0:1], in1=sm[:, 1:2], op=ALU.subtract),
                     reads=["m1", "m2"], writes=["dd"])
                P.op("act", lambda e: e.activation(out=sm[:, 3:4], in_=sm[:, 2:3], func=AF.Sigmoid), reads=["dd"], writes=["p1"])
                P.op("act", lambda e: e.activation(out=sm[:, 4:5], in_=sm[:, 2:3], func=AF.Sigmoid, scale=-1.0), reads=["dd"], writes=["p2"])
                P.op("dve", lambda e: e.tensor_scalar(out=eq1, in0=eq1, scalar1=sm[:, 3:4], scalar2=None, op0=ALU.mult),
                     reads=["eq1", "p1", "L2"], writes=["eq1"])
                P.op("dve", lambda e, si=si: e.scalar_tensor_tensor(out=GW[:, si, :], in0=eq2, scalar=sm[:, 4:5], in1=eq1, op0=ALU.mult, op1=ALU.add),
                     reads=["eq2", "p2", "eq1"], writes=[("GW", si)])
        P.barrier()
        A.o = mark
        NWB = 2
        w1g = [A.bf16(KC, FG) for _ in range(NWB)]
        w3g = [A.bf16(KC, FG) for _ in range(NWB)]
        w2g = [A.bf16(4, D) for _ in range(NWB)]
        WB = [A.bf16(HN) for _ in range(2)]
        a = [A.bf16(4, T) for _ in range(2)]
        sg = [A.f32(T) for _ in range(2)]
        sgw = [A.f32(T) for _ in range(2)]
        def pre_expert(ex):
            wb = WB[ex % 2]
            wbk = ("WB", ex % 2)
            for c4 in range(4):
                def f(e, c4=c4):
                    ins = None
                    for s4 in range(4):
                        si = c4 * 4 + s4
                        ins = e.matmul(PS[0][:, s4 * 128:(s4 + 1) * 128], lhsT=GW[:, si, ex:ex + 1].to_broadcast([128, 128]),
                                       rhs=C.ident32, start=True, stop=True)
                    return ins
                P.op("pe", f, reads=[("GW", c4 * 4 + s4) for s4 in range(4)], writes=[("ps", 0)])
                P.op("act", lambda e, c4=c4: e.copy(out=wb[:, c4 * T:(c4 + 1) * T], in_=PS[0]), reads=[("ps", 0)], writes=[wbk])

        def pre_fg(ex, fg, b):
            f0 = fg * FG
            v1 = C.d_moe_w1[j][ex].rearrange("(kc p) n -> p kc n", p=128)[:, :, f0:f0 + FG]
            v3 = C.d_moe_w3[j][ex].rearrange("(kc p) n -> p kc n", p=128)[:, :, f0:f0 + FG]
            v2 = C.d_moe_w2[j][ex][f0:f0 + FG, :].rearrange("(fc p) n -> p fc n", p=128)
            P.op("pool", lambda e: e.dma_start(out=w1g[b], in_=v1), writes=[("w1g", b)], dma=True)
            P.op("pool", lambda e: e.dma_start(out=w3g[b], in_=v3), writes=[("w3g", b)], dma=True)
            P.op("pool", lambda e: e.dma_start(out=w2g[b], in_=v2), writes=[("w2g", b)], dma=True)

        def stage1_fc(ex, tt, b, ab, fc):
            wb = WB[ex % 2]
            wbk = ("WB", ex % 2)
            pg = 2 * (fc % 2)
            pu = pg + 1
            sb = fc % 2
            P.op("pe", lambda e: mm_acc(e, PS[pg], [(w1g[b][:, k, fc * 128:(fc + 1) * 128], hH[:, k, tt * T:(tt + 1) * T]) for k in range(KC)]),
                 reads=[("w1g", b)], writes=[("ps", pg)])
            P.op("pe", lambda e: mm_acc(e, PS[pu], [(w3g[b][:, k, fc * 128:(fc + 1) * 128], hH[:, k, tt * T:(tt + 1) * T]) for k in range(KC)]),
                 reads=[("w3g", b)], writes=[("ps", pu)])
            P.op("act", lambda e: e.activation(out=sg[sb], in_=PS[pg], func=AF.Silu), reads=[("ps", pg)], writes=[("sg", sb)])
            P.op("dve", lambda e: e.tensor_tensor(out=sgw[sb], in0=sg[sb], in1=wb[:, tt * T:(tt + 1) * T], op=ALU.mult),
                 reads=[("sg", sb), wbk], writes=[("sgw", sb)])
            P.op("dve", lambda e: e.tensor_tensor(out=a[ab][:, fc, :], in0=PS[pu], in1=sgw[sb], op=ALU.mult),
                 reads=[("ps", pu), ("sgw", sb)], writes=[("a", ab, fc)])

        def stage2(tt, b, ab, first):
            for m in range(KC):
                pb = 4 + (m % 4)
                P.op("pe", lambda e, m=m, pb=pb: mm_acc(e, PS[pb], [(w2g[b][:, fc, m * 128:(m + 1) * 128], a[ab][:, fc, :]) for fc in range(4)]),
                     reads=[("a", ab, fc) for fc in range(4)] + [("w2g", b)], writes=[("ps", pb)])
                yv = yacc[:, m, tt * T:(tt + 1) * T]
                if first:
                    P.op("dve", lambda e, yv=yv, pb=pb: e.tensor_copy(out=yv, in_=PS[pb]), reads=[("ps", pb)], writes=[("yacc", m, tt)])
                else:
                    P.op("dve", lambda e, yv=yv, pb=pb: e.tensor_tensor(out=yv, in0=PS[pb], in1=yv, op=ALU.add),
                         reads=[("ps", pb), ("yacc", m, tt)], writes=[("yacc", m, tt)])

        wcount = 0
        acount = 0
        pending = None
        for ex in range(NE):
            for fg in range(NFG):
                b = wcount % NWB
                wcount += 1
                first = (ex == 0 and fg == 0)
                for tt in range(HT):
                    ab = acount % 2
                    acount += 1
                    if tt == 0:
                        if fg == 0:
                            pre_expert(ex)
                        pre_fg(ex, fg, b)
                    stage1_fc(ex, tt, b, ab, 0)
                    if pending is not None:
                        stage2(*pending)
                    for fc in range(1, 4):
                        stage1_fc(ex, tt, b, ab, fc)
                    pending = (tt, b, ab, first)
        stage2(*pending)
        P.barrier()
        A.o = mark
        xt3 = [A.f32(KC, T) for _ in range(2)]
        if final_out is not None:
            fsq = A.bf16(KC, T)
            frs = A.f32(T)
        for tt in range(HT):
            t = half * HT + tt
            xb = xt3[tt % 2]
            xk = ("xt3", tt % 2)
            P.op("sp", lambda e, t=t, xb=xb: e.dma_start(out=xb, in_=sv[:, :, t * T:(t + 1) * T]), reads=[(sid, t)], writes=[xk], dma=True)
            for m in range(KC):
                P.op("dve", lambda e, m=m, xb=xb, tt=tt: e.scalar_tensor_tensor(out=xb[:, m, :], in0=yacc[:, m, tt * T:(tt + 1) * T],
                                                                              scalar=g2[:, m:m + 1], in1=xb[:, m, :], op0=ALU.mult, op1=ALU.add),
                     reads=[xk, ("yacc", m, tt)], writes=[xk])
            if final_out is not None:
                sqk = ("fsq", tt % 2)
                P.op("act", lambda e, xb=xb: e.activation(out=fsq, in_=xb, func=AF.Square), reads=[xk], writes=["fsq"])
                P.op("pe", lambda e: mm_acc(e, PS[0], [(C.ones_bf, fsq[:, k, :]) for k in range(KC)]), reads=["fsq"], writes=[("ps", 0)])
                P.op("act", lambda e: e.activation(out=frs, in_=PS[0], func=AF.Sqrt, bias=C.eps_col, scale=1.0 / D), reads=[("ps", 0)], writes=["frs"])
                P.op("dve", lambda e: e.reciprocal(out=frs, in_=frs), reads=["frs"], writes=["frs"])
                for k in range(KC):
                    P.op("dve", lambda e, k=k, xb=xb: e.scalar_tensor_tensor(out=xb[:, k, :], in0=xb[:, k, :], scalar=C.fing[:, k:k + 1], in1=frs,
                                                                           op0=ALU.mult, op1=ALU.mult),
                         reads=[xk, "frs"], writes=[xk])
                fv = xview(final_out)
                P.op("sp", lambda e, t=t, xb=xb, fv=fv: e.dma_start(out=fv[:, :, t * T:(t + 1) * T], in_=xb), reads=[xk], writes=[("out", t)], dma=True)
            else:
                P.op("sp", lambda e, t=t, xb=xb: e.dma_start(out=dv[:, :, t * T:(t + 1) * T], in_=xb), reads=[xk], writes=[(did, t)], dma=True)
        P.barrier()


def phase_final(P, C, A, src, dst, sid, did, do_norm=True):
    T = 512
    NT = SEQ // T
    A.reset()
    xt = [A.f32(KC, T) for _ in range(2)]
    sqb = A.bf16(KC, T)
    rs = A.f32(T)
    PS = C.PS
    sv = xview(src)
    dv = xview(dst)
    for t in range(NT):
        xb = xt[t % 2]
        xk = ("xt", t % 2)
        P.op("sp", lambda e, t=t, xb=xb: e.dma_start(out=xb, in_=sv[:, :, t * T:(t + 1) * T]), reads=[(sid, t)], writes=[xk], dma=True)
        if do_norm:
            P.op("act", lambda e, xb=xb: e.activation(out=sqb, in_=xb, func=AF.Square), reads=[xk], writes=["sq"])
            P.op("pe", lambda e: mm_acc(e, PS[0], [(C.ones_bf, sqb[:, k, :]) for k in range(KC)]), reads=["sq"], writes=["ps0"])
            P.op("act", lambda e: e.activation(out=rs, in_=PS[0], func=AF.Sqrt, bias=C.eps_col, scale=1.0 / D), reads=["ps0"], writes=["rs"])
            P.op("dve", lambda e: e.reciprocal(out=rs, in_=rs), reads=["rs"], writes=["rs"])
            for k in range(KC):
                P.op("dve", lambda e, k=k, xb=xb: e.scalar_tensor_tensor(out=xb[:, k, :], in0=xb[:, k, :], scalar=C.fing[:, k:k + 1], in1=rs,
                                                                       op0=ALU.mult, op1=ALU.mult),
                     reads=[xk, "rs"], writes=[xk])
        P.op("sp", lambda e, t=t, xb=xb: e.dma_start(out=dv[:, :, t * T:(t + 1) * T], in_=xb), reads=[xk], writes=[(did, t)], dma=True)
    P.barrier()


def build_program(n_phases=8):
    nc = bass.Bass("TRN2", target_bir_lowering=False)
    C = Ctx()
    C.nc = nc

    def din(name, shape):
        return nc.dram_tensor(name, list(shape), F32, kind="ExternalInput").ap()

    d_x = din("xT", (D, SEQ))
    C.d_cond = din("condT", (128, KC))
    C.d_adaw = din("ada_w", (DEPTH, D, 6 * D))
    C.d_adab = din("adab", (128, DEPTH * 48))
    C.d_ng = din("ng", (128, DEPTH, 2, KC))
    C.d_fing = din("fing", (128, KC))
    C.d_ssm_in = din("ssm_in", (2, D, D))
    C.d_ssm_glu = din("ssm_glu", (2, D, D))
    C.d_ssm_out = din("ssm_out", (2, D, D))
    C.d_ssm_pq = din("ssm_pq", (2, 128, 3, 32))
    C.d_ssm_d = din("ssm_dT", (2, 128, KC))
    C.d_ssm_row = din("ssm_row", (2, 3, 128, 4096))
    C.d_ssm_bt = din("ssm_bt", (2, 2, 128, 4096))
    C.d_ssm_ct = din("ssm_ct", (2, 2, 128, 4096))
    C.d_pool_in = din("pool_in", (2, D, D))
    C.d_pool_mix = din("pool_mix", (2, 4, 256, 256))
    C.d_pool_scale = din("pool_scaleT", (2, 128, KC))
    C.d_pool_out = din("pool_out", (2, D, D))
    C.d_ffn_w1 = din("ffn_w1", (2, D, DFF))
    C.d_ffn_w3 = din("ffn_w3", (2, D, DFF))
    C.d_ffn_w2 = din("ffn_w2", (2, DFF, D))
    C.d_router_w = din("router_wT", (2, 128, KC, NE))
    C.d_router_b = din("router_bb", (2, 128, NE))
    C.d_moe_w1 = din("moe_w1", (2, NE, D, DFE))
    C.d_moe_w3 = din("moe_w3", (2, NE, D, DFE))
    C.d_moe_w2 = din("moe_w2", (2, NE, DFE, D))
    C.d_inv0 = din("inv0", (128, 4, 512))
    d_tloc = din("tloc", (128, 512))
    d_ident = din("ident", (128, 128))
    d_out = nc.dram_tensor("outT", [D, SEQ], F32, kind="ExternalOutput").ap()
    xa = nc.dram_tensor("xa", [D, SEQ], F32, kind="Internal").ap()
    xb = nc.dram_tensor("xb", [D, SEQ], F32, kind="Internal").ap()

    with ExitStack() as st:
        P = Prog(nc)
        sb = lambda name, shape, dt=F32: st.enter_context(nc.sbuf_tensor(name, list(shape), dt))
        C.ones_bf = sb("ones_bf", (128, 128), BF16)[:]
        C.eps_col = sb("eps_col", (128, 1))[:]
        C.halfpi_col = sb("halfpi", (128, 1))[:]
        C.magp_col = sb("magp", (128, 1))[:]
        C.magn_col = sb("magn", (128, 1))[:]
        C.ident32 = sb("ident32", (128, 128))[:]
        C.tloc = sb("tloc_sb", (128, 512))[:]
        C.mod = sb("mod", (128, DEPTH * 48))[:]
        C.adab = sb("adab_sb", (128, DEPTH * 48))[:]
        C.ng = sb("ng_sb", (128, DEPTH, 2, KC))[:]
        C.gs = sb("gs_sb", (128, DEPTH, 2, KC))[:]
        C.fing = sb("fing_sb", (128, KC))[:]
        C.cndb = sb("cndb", (128, KC), BF16)[:]
        arena = sb("arena", (128, 50 * 1024 + 512))[:]
        A = Arena(arena)
        C.PS = [st.enter_context(nc.psum_tensor(f"ps{i}", [128, 512], F32))[:] for i in range(8)]

        P.op("sp", lambda e: e.dma_start(out=C.tloc, in_=d_tloc), writes=["tloc"], dma=True)
        P.op("sp", lambda e: e.dma_start(out=C.ident32, in_=d_ident), writes=["ident"], dma=True)
        P.op("dve", lambda e: e.memset(C.halfpi_col, math.pi / 2.0), writes=["halfpi"])
        P.op("dve", lambda e: e.memset(C.magp_col, MAG), writes=["magp"])
        P.op("dve", lambda e: e.memset(C.magn_col, -MAG), writes=["magn"])
        phase_prologue(P, C, A)

        seqs = []
        for li in range(DEPTH):
            seqs.append(("mix", li))
            seqs.append(("ffn", li))
        seqs = seqs[:n_phases]
        cur, cur_id = d_x, "xin"
        nxt = [(xa, "xa"), (xb, "xb")]
        for pi, (kind, li) in enumerate(seqs):
            dstap, dst_id = nxt[pi % 2]
            if kind == "mix":
                if li % 2 == 0:
                    phase_s5(P, C, A, li, cur, dstap, cur_id, dst_id)
                else:
                    phase_pool(P, C, A, li, cur, dstap, cur_id, dst_id)
            else:
                if li % 2 == 0:
                    phase_ffn(P, C, A, li, cur, dstap, cur_id, dst_id)
                else:
                    fuse = (n_phases == 8 and pi == 7)
                    phase_moe(P, C, A, li, cur, dstap, cur_id, dst_id, final_out=(d_out if fuse else None))
            cur, cur_id = dstap, dst_id
        if n_phases != 8:
            phase_final(P, C, A, cur, d_out, cur_id, "out", do_norm=False)

        emit = P.emit(st)
        with nc.Block() as block:
            @block.tensor
            def _(e):
                emit("pe", e)

            @block.vector
            def _(e):
                emit("dve", e)

            @block.scalar
            def _(e):
                emit("act", e)

            @block.gpsimd
            def _(e):
                emit("pool", e)

            @block.sync
            def _(e):
                emit("sp", e)
        C.stats = P.stats
    return nc, C


def _colT(v):
    return np.ascontiguousarray(np.asarray(v, np.float32).reshape(KC, 128).T)


def prepare_inputs(inp, cores):
    f = lambda a: np.ascontiguousarray(np.asarray(a, np.float32))
    shared = {}
    shared["ada_w"] = f(inp["ada_w"])
    shared["adab"] = np.ascontiguousarray(
        np.stack([f(inp["ada_b"][i]).reshape(48, 128).T for i in range(DEPTH)], axis=1).reshape(128, DEPTH * 48))
    ng = np.zeros((128, DEPTH, 2, KC), np.float32)
    for i in range(DEPTH):
        for s in range(2):
            ng[:, i, s, :] = _colT(inp["norm_g"][i, s])
    shared["ng"] = ng
    shared["fing"] = _colT(inp["final_g"])
    for k in ("ssm_in", "ssm_glu", "ssm_out", "pool_in", "pool_mix", "pool_out", "ffn_w1", "ffn_w3", "ffn_w2",
              "moe_w1", "moe_w3", "moe_w2"):
        shared[k] = f(inp[k])
    pq = np.zeros((2, 128, 3, 32), np.float32)
    row = np.zeros((2, 3, 128, 4096), np.float32)
    bt = np.zeros((2, 2, 128, 4096), np.float32)
    ct = np.zeros((2, 2, 128, 4096), np.float32)
    dT = np.zeros((2, 128, KC), np.float32)
    for j in range(2):
        lr = f(inp["ssm_lam_re"][j])
        lim = f(inp["ssm_lam_im"][j])
        ldt = f(inp["ssm_log_dt"][j])
        ldt_gp = np.repeat(ldt[:, None], 64, axis=1)
        for a_i, arr in enumerate((lr, lim, ldt_gp)):
            pq[j, :, a_i, :] = arr.reshape(32, 2, 64).transpose(1, 2, 0).reshape(128, 32)
            row[j, a_i] = np.broadcast_to(arr.reshape(1, 4096), (128, 4096))
        br = f(inp["ssm_b_re"][j])
        bi = f(inp["ssm_b_im"][j])
        cr = f(inp["ssm_c_re"][j])
        ci = f(inp["ssm_c_im"][j])
        btr = bt[j].reshape(2, 128, 32, 2, 64)
        ctr = ct[j].reshape(2, 128, 32, 128)
        for q in range(32):
            for g2 in range(2):
                g = 2 * q + g2
                gl = g % 8
                btr[0, gl * 16:(gl + 1) * 16, q, g2, :] = br[g].T
                btr[1, gl * 16:(gl + 1) * 16, q, g2, :] = bi[g].T
                ctr[0, g2 * 64:(g2 + 1) * 64, q, gl * 16:(gl + 1) * 16] = cr[g].T
                ctr[1, g2 * 64:(g2 + 1) * 64, q, gl * 16:(gl + 1) * 16] = ci[g].T
        dT[j] = _colT(f(inp["ssm_d"][j]).reshape(-1))
    shared["ssm_pq"] = pq
    shared["ssm_row"] = row
    shared["ssm_bt"] = bt
    shared["ssm_ct"] = ct
    shared["ssm_dT"] = dT
    shared["pool_scaleT"] = np.stack([_colT(inp["pool_scale"][j]) for j in range(2)])
    shared["router_wT"] = np.ascontiguousarray(
        np.stack([f(inp["router_w"][j]).reshape(KC, 128, NE).transpose(1, 0, 2) for j in range(2)]))
    shared["router_bb"] = np.ascontiguousarray(
        np.stack([np.broadcast_to(f(inp["router_b"][j])[None, :], (128, NE)) for j in range(2)]))
    inv0 = np.zeros((128, 4, 512), np.float32)
    tpos = np.arange(1, 513, dtype=np.float32)
    for wg, w in enumerate((2, 4, 8, 16)):
        inv0[:, wg, :] = (1.0 / np.minimum(tpos, float(w)))[None, :]
    shared["inv0"] = inv0
    shared["tloc"] = np.ascontiguousarray(np.broadcast_to(np.arange(512, dtype=np.float32)[None, :], (128, 512)))
    shared["ident"] = np.eye(128, dtype=np.float32)
    maps = []
    x = np.asarray(inp["x"], np.float32)
    c = np.asarray(inp["c"], np.float32)
    for b in cores:
        m = dict(shared)
        m["xT"] = np.ascontiguousarray(x[b].T)
        m["condT"] = _colT(c[b])
        maps.append(m)
    return maps


_CACHE = {}


def kernel(**inputs):
    if "nc" not in _CACHE:
        _CACHE["nc"] = build_program(8)[0]
    nc = _CACHE["nc"]
    maps = prepare_inputs(inputs, list(range(NB)))
    res = run_bass_kernel_spmd(nc, maps, core_ids=list(range(NB)))
    out = np.stack([np.ascontiguousarray(r["outT"].T) for r in res.results], axis=0)
    return out.astype(np.float32)
```
